# Optimizing a Trainium2 kernel written in Bass

```python
import jax, jax.numpy as jnp
from jax import lax
import numpy as np

D_MODEL = 1024
BATCH = 1
SEQ = 16384
DEPTH = 4

EPS = 1e-6
BLOCK = 128
CONV_CH = 512
CONV_K = 3
POOL_WINDOWS = (2, 4, 8, 16)
POOL_GROUP_CH = 128
POOL_CH = POOL_GROUP_CH * len(POOL_WINDOWS)
EVEN_IN = 3 * CONV_CH + POOL_CH
EVEN_MIX = CONV_CH + POOL_CH
SWA_HEADS = 8
SWA_KV_HEADS = 2
SWA_GROUP = SWA_HEADS // SWA_KV_HEADS
HEAD_DIM = 64
WINDOW = 128
SWA_Q_W = SWA_HEADS * HEAD_DIM
SWA_KV_W = SWA_KV_HEADS * HEAD_DIM
MLA_HEADS = 8
MLA_NOPE = 64
MLA_ROPE = 32
MLA_QK = MLA_NOPE + MLA_ROPE
MLA_V = 64
Q_LORA = 384
KV_LORA = 256
ROPE_THETA = 10000.0
ODD_SPLITS = (SWA_Q_W, SWA_KV_W, SWA_KV_W, Q_LORA, KV_LORA, MLA_ROPE)
ODD_IN = sum(ODD_SPLITS)
ODD_MIX = SWA_HEADS * HEAD_DIM + MLA_HEADS * MLA_V
N_GROUPS = 4
EXPERTS_PER_GROUP = 8
N_EXPERTS = N_GROUPS * EXPERTS_PER_GROUP
EXPERT_FF = 256
TOP_K = 2
N_EVEN = (DEPTH + 1) // 2
N_ODD = DEPTH // 2

kernel_name = "hybrid_conv_pool_swa_mla_hmoe_adaln"


def rms_norm(x, g):
    xf = x.astype(jnp.float32)
    y = xf * lax.rsqrt(jnp.mean(xf * xf, axis=-1, keepdims=True) + EPS)
    return (y * g.astype(jnp.float32)).astype(x.dtype)


def apply_rope(x, positions):
    half = x.shape[-1] // 2
    inv = jnp.power(ROPE_THETA, -jnp.arange(half, dtype=jnp.float32) / half)
    ang = positions.astype(jnp.float32)[..., None] * inv
    cos = jnp.cos(ang)[:, :, None, :]
    sin = jnp.sin(ang)[:, :, None, :]
    x1 = x[..., :half].astype(jnp.float32)
    x2 = x[..., half:].astype(jnp.float32)
    return jnp.concatenate([x1 * cos - x2 * sin, x1 * sin + x2 * cos], axis=-1).astype(x.dtype)


def multiscale_pool(u, pool_w, pool_scale):
    S = u.shape[1]
    cs = jnp.cumsum(u.astype(jnp.float32), axis=1)
    t = jnp.arange(1, S + 1, dtype=jnp.float32)[None, :, None]
    outs = []
    for gi, w in enumerate(POOL_WINDOWS):
        sl = slice(gi * POOL_GROUP_CH, (gi + 1) * POOL_GROUP_CH)
        csg = cs[..., sl]
        lag = jnp.pad(csg, ((0, 0), (w, 0), (0, 0)))[:, :S]
        mean = (csg - lag) / jnp.minimum(t, float(w))
        d = (mean - u[..., sl].astype(jnp.float32)).astype(u.dtype)
        outs.append(d @ pool_w[gi])
    return jnp.concatenate(outs, axis=-1) * pool_scale


def conv_pool_mixer(h, w_in, conv_w, pool_w, pool_scale, w_out):
    S = h.shape[1]
    z = h @ w_in
    b_gate, c_gate, xa, u = jnp.split(z, [CONV_CH, 2 * CONV_CH, 3 * CONV_CH], axis=-1)
    v = c_gate * xa
    vp = jnp.pad(v, ((0, 0), (CONV_K - 1, 0), (0, 0)))
    conv = sum(vp[:, CONV_K - 1 - k: CONV_K - 1 - k + S] * conv_w[k] for k in range(CONV_K))
    y_a = b_gate * conv
    y_b = multiscale_pool(u, pool_w, pool_scale)
    return jnp.concatenate([y_a, y_b], axis=-1) @ w_out


def swa_sink_attention(q, k, v, q_g, k_g, sinks):
    B, S = q.shape[:2]
    nb = S // BLOCK
    q = rms_norm(q, q_g)
    k = rms_norm(k, k_g)
    qb = q.reshape(B, nb, BLOCK, SWA_KV_HEADS, SWA_GROUP, HEAD_DIM)

    def band(t):
        tb = t.reshape(B, nb, BLOCK, SWA_KV_HEADS, HEAD_DIM)
        prev = jnp.pad(tb, ((0, 0), (1, 0), (0, 0), (0, 0), (0, 0)))[:, :-1]
        return jnp.concatenate([prev, tb], axis=2)

    kb, vb = band(k), band(v)
    s = jnp.einsum('bnqkgd,bnskd->bnkgqs', qb, kb).astype(jnp.float32) * (HEAD_DIM ** -0.5)
    qi = jnp.arange(BLOCK)[:, None]
    si = jnp.arange(2 * BLOCK)[None, :]
    rel = qi + BLOCK - si
    in_win = (rel >= 0) & (rel < WINDOW)
    valid = (jnp.arange(nb)[:, None, None] * BLOCK - BLOCK + si[None]) >= 0
    mask = (in_win[None] & valid)[None, :, None, None]
    s = jnp.where(mask, s, -jnp.inf)
    sink = sinks.astype(jnp.float32).reshape(1, 1, SWA_KV_HEADS, SWA_GROUP, 1, 1)
    m = jnp.maximum(jnp.max(s, axis=-1, keepdims=True), sink)
    e = jnp.exp(s - m)
    p = e / (jnp.sum(e, axis=-1, keepdims=True) + jnp.exp(sink - m))
    o = jnp.einsum('bnkgqs,bnskd->bnqkgd', p.astype(v.dtype), vb)
    return o.reshape(B, S, SWA_HEADS * HEAD_DIM)


def mla_attention(c_q, c_kv, k_rope, positions, q_norm_g, kv_norm_g, w_uq, w_ukv, q_g, k_g):
    B, S = c_q.shape[:2]
    nb = S // BLOCK
    q = (rms_norm(c_q, q_norm_g) @ w_uq).reshape(B, S, MLA_HEADS, MLA_QK)
    kv = (rms_norm(c_kv, kv_norm_g) @ w_ukv).reshape(B, S, MLA_HEADS, MLA_NOPE + MLA_V)
    k_nope, v = kv[..., :MLA_NOPE], kv[..., MLA_NOPE:]
    k = jnp.concatenate(
        [k_nope, jnp.broadcast_to(k_rope[:, :, None, :], (B, S, MLA_HEADS, MLA_ROPE))], axis=-1)
    q = rms_norm(q, q_g)
    k = rms_norm(k, k_g)
    q = jnp.concatenate([q[..., :MLA_NOPE], apply_rope(q[..., MLA_NOPE:], positions)], axis=-1)
    k = jnp.concatenate([k[..., :MLA_NOPE], apply_rope(k[..., MLA_NOPE:], positions)], axis=-1)
    qb = q.reshape(B, nb, BLOCK, MLA_HEADS, MLA_QK).transpose(1, 0, 2, 3, 4)
    kpos = jnp.arange(S)

    def one_block(args):
        qblk, n = args
        s = jnp.einsum('bqhd,bkhd->bhqk', qblk, k).astype(jnp.float32) * (MLA_QK ** -0.5)
        qpos = n * BLOCK + jnp.arange(BLOCK)
        s = jnp.where(kpos[None, :] <= qpos[:, None], s, -jnp.inf)
        p = jax.nn.softmax(s, axis=-1)
        return jnp.einsum('bhqk,bkhd->bqhd', p.astype(v.dtype), v)

    o = lax.map(one_block, (qb, jnp.arange(nb)))
    return o.transpose(1, 0, 2, 3, 4).reshape(B, S, MLA_HEADS * MLA_V)


def attention_mixer(h, positions, w_in, swa_q_g, swa_k_g, swa_sinks, mla_q_norm_g,
                    mla_kv_norm_g, mla_w_uq, mla_w_ukv, mla_q_g, mla_k_g, w_out):
    B, S = h.shape[:2]
    z = h @ w_in
    q_s, k_s, v_s, c_q, c_kv, k_r = jnp.split(z, list(np.cumsum(ODD_SPLITS)[:-1]), axis=-1)
    y_c = swa_sink_attention(q_s.reshape(B, S, SWA_HEADS, HEAD_DIM),
                             k_s.reshape(B, S, SWA_KV_HEADS, HEAD_DIM),
                             v_s.reshape(B, S, SWA_KV_HEADS, HEAD_DIM),
                             swa_q_g, swa_k_g, swa_sinks)
    y_d = mla_attention(c_q, c_kv, k_r, positions, mla_q_norm_g, mla_kv_norm_g,
                        mla_w_uq, mla_w_ukv, mla_q_g, mla_k_g)
    return jnp.concatenate([y_c, y_d], axis=-1) @ w_out


def hier_moe(h, w_group, b_group, w_expert, b_expert, w_gate, w_up, w_down):
    B, S, _ = h.shape
    g_logits = (h @ w_group).astype(jnp.float32) + b_group
    p_group = jax.nn.softmax(g_logits, axis=-1)
    g_w, g_idx = lax.top_k(p_group, 1)
    e_logits = ((h @ w_expert).astype(jnp.float32) + b_expert).reshape(
        B, S, N_GROUPS, EXPERTS_PER_GROUP)
    sel = jnp.take_along_axis(e_logits, g_idx[..., None], axis=2)[:, :, 0]
    p_exp = jax.nn.softmax(sel, axis=-1)
    top_v, top_i = lax.top_k(p_exp, TOP_K)
    weights = g_w * top_v / jnp.sum(top_v, axis=-1, keepdims=True)
    global_idx = g_idx * EXPERTS_PER_GROUP + top_i
    gates = jnp.sum(jax.nn.one_hot(global_idx, N_EXPERTS, dtype=jnp.float32)
                    * weights[..., None], axis=-2).astype(h.dtype)

    def expert(acc, xs):
        wg, wu, wd, g = xs
        y = (jax.nn.silu(h @ wg) * (h @ wu)) @ wd
        return acc + g[..., None] * y, None

    out, _ = lax.scan(expert, jnp.zeros_like(h), (w_gate, w_up, w_down, jnp.moveaxis(gates, -1, 0)))
    return out


def setup_inputs(seed: int = 0) -> dict:
    key = jax.random.key(seed)
    ks = iter(jax.random.split(key, 40))
    f32 = jnp.float32

    def nrm(shape, scale):
        return jax.random.normal(next(ks), shape, f32) * scale

    def gain(shape):
        return 1.0 + 0.1 * jax.random.normal(next(ks), shape, f32)

    D = D_MODEL
    return {
        "x": nrm((BATCH, SEQ, D), 1.0),
        "c": nrm((BATCH, D), 1.0),
        "positions": jnp.broadcast_to(jnp.arange(SEQ, dtype=jnp.int32), (BATCH, SEQ)),
        "ada_w": nrm((DEPTH, D, 6 * D), 0.5 * D ** -0.5),
        "ada_b": nrm((DEPTH, 6 * D), 0.02),
        "norm1_g": gain((DEPTH, D)),
        "norm2_g": gain((DEPTH, D)),
        "cp_w_in": nrm((N_EVEN, D, EVEN_IN), D ** -0.5),
        "conv_w": nrm((N_EVEN, CONV_K, CONV_CH), CONV_K ** -0.5),
        "pool_w": nrm((N_EVEN, len(POOL_WINDOWS), POOL_GROUP_CH, POOL_GROUP_CH), POOL_GROUP_CH ** -0.5),
        "pool_scale": gain((N_EVEN, POOL_CH)),
        "cp_w_out": nrm((N_EVEN, EVEN_MIX, D), EVEN_MIX ** -0.5),
        "at_w_in": nrm((N_ODD, D, ODD_IN), D ** -0.5),
        "swa_q_g": gain((N_ODD, HEAD_DIM)),
        "swa_k_g": gain((N_ODD, HEAD_DIM)),
        "swa_sinks": nrm((N_ODD, SWA_HEADS), 0.5),
        "mla_q_norm_g": gain((N_ODD, Q_LORA)),
        "mla_kv_norm_g": gain((N_ODD, KV_LORA)),
        "mla_w_uq": nrm((N_ODD, Q_LORA, MLA_HEADS * MLA_QK), Q_LORA ** -0.5),
        "mla_w_ukv": nrm((N_ODD, KV_LORA, MLA_HEADS * (MLA_NOPE + MLA_V)), KV_LORA ** -0.5),
        "mla_q_g": gain((N_ODD, MLA_QK)),
        "mla_k_g": gain((N_ODD, MLA_QK)),
        "at_w_out": nrm((N_ODD, ODD_MIX, D), ODD_MIX ** -0.5),
        "moe_w_group": nrm((DEPTH, D, N_GROUPS), D ** -0.5),
        "moe_b_group": nrm((DEPTH, N_GROUPS), 0.01),
        "moe_w_expert": nrm((DEPTH, D, N_EXPERTS), D ** -0.5),
        "moe_b_expert": nrm((DEPTH, N_EXPERTS), 0.01),
        "moe_w_gate": nrm((DEPTH, N_EXPERTS, D, EXPERT_FF), D ** -0.5),
        "moe_w_up": nrm((DEPTH, N_EXPERTS, D, EXPERT_FF), D ** -0.5),
        "moe_w_down": nrm((DEPTH, N_EXPERTS, EXPERT_FF, D), EXPERT_FF ** -0.5),
    }


def reference(x, c, positions, ada_w, ada_b, norm1_g, norm2_g, cp_w_in, conv_w, pool_w,
              pool_scale, cp_w_out, at_w_in, swa_q_g, swa_k_g, swa_sinks, mla_q_norm_g,
              mla_kv_norm_g, mla_w_uq, mla_w_ukv, mla_q_g, mla_k_g, at_w_out, moe_w_group,
              moe_b_group, moe_w_expert, moe_b_expert, moe_w_gate, moe_w_up, moe_w_down):
    c_act = jax.nn.silu(c)
    for l in range(DEPTH):
        mod = (c_act @ ada_w[l] + ada_b[l])[:, None, :]
        shift1, scale1, gate1, shift2, scale2, gate2 = jnp.split(mod, 6, axis=-1)
        h = rms_norm(x, norm1_g[l]) * (1 + scale1) + shift1
        i = l // 2
        if l % 2 == 0:
            y = conv_pool_mixer(h, cp_w_in[i], conv_w[i], pool_w[i], pool_scale[i], cp_w_out[i])
        else:
            y = attention_mixer(h, positions, at_w_in[i], swa_q_g[i], swa_k_g[i], swa_sinks[i],
                                mla_q_norm_g[i], mla_kv_norm_g[i], mla_w_uq[i], mla_w_ukv[i],
                                mla_q_g[i], mla_k_g[i], at_w_out[i])
        x = x + gate1 * y
        h = rms_norm(x, norm2_g[l]) * (1 + scale2) + shift2
        x = x + gate2 * hier_moe(h, moe_w_group[l], moe_b_group[l], moe_w_expert[l],
                                 moe_b_expert[l], moe_w_gate[l], moe_w_up[l], moe_w_down[l])
    return x
```

```python
import numpy as np
import ml_dtypes
from contextlib import ExitStack
import concourse.bass as bass
import concourse.mybir as mybir
from concourse.bass_utils import run_bass_kernel_spmd

F32 = mybir.dt.float32
BF16 = mybir.dt.bfloat16
I32 = mybir.dt.int32
ALU = mybir.AluOpType
AF = mybir.ActivationFunctionType
AX = mybir.AxisListType

NCORE = 8
S = 16384
D = 1024
NT = S // NCORE
TN = 512
NTILE = NT // TN
KC = D // 128
EPS = 1e-6
NE = 32
FF = 256


class Tok:
    __slots__ = ("name", "writer", "readers", "dsem", "dcount")

    def __init__(self, name=""):
        self.name = name
        self.writer = None
        self.readers = {}
        self.dsem = None
        self.dcount = 0


class Eng:
    def __init__(self, name, sem):
        self.name = name
        self.sem = sem
        self.count = 0
        self.ops = []
        self.waited = {}


class Prog:
    def __init__(self, nc, es):
        self.nc = nc
        self.es = es
        self.engs = {}
        for n in ("pe", "act", "dve", "pool", "sp"):
            sem = es.enter_context(nc.semaphore("s_" + n))
            self.engs[n] = Eng(n, sem)
        self.ntok = 0
        self.nsb = 0
        self.es_outer = es
        self.free_dsems = []
        self.stage_toks = []
        self.all_dtoks = []
        self.in_stage = False
        self.rec = None

    def sbuf(self, shape, dt, name=None):
        self.nsb += 1
        return self.es.enter_context(self.nc.sbuf_tensor(f"S{self.nsb}_{name or ''}", list(shape), dt))

    def psum(self, shape, dt=F32, name=None):
        self.nsb += 1
        return self.es.enter_context(self.nc.psum_tensor(name or f"ps{self.nsb}", list(shape), dt))

    def tok(self, name=""):
        self.ntok += 1
        return Tok(name or f"t{self.ntok}")

    def _dsem(self, t):
        if t.dsem is None:
            if self.free_dsems:
                sem, cnt = self.free_dsems.pop()
            else:
                sem = self.es_outer.enter_context(self.nc.semaphore(f"d{self.ntok}"))
                self.ntok += 1
                cnt = 0
            t.dsem = sem
            t.dcount = cnt
            self.all_dtoks.append(t)
            if self.in_stage:
                self.stage_toks.append(t)
        return t.dsem

    def barrier(self):
        for E in self.engs.values():
            waits = []
            for E2 in self.engs.values():
                if E2 is not E and E2.count > E.waited.get(id(E2.sem), 0):
                    E.waited[id(E2.sem)] = E2.count
                    waits.append((E2.sem, E2.count))
            for t in self.all_dtoks:
                if t.dcount > E.waited.get(id(t.dsem), 0):
                    E.waited[id(t.dsem)] = t.dcount
                    waits.append((t.dsem, t.dcount))
            E.ops.append((waits, None, []))

    def end_stage(self):
        self.barrier()
        self.emit()
        for t in self.stage_toks:
            self.free_dsems.append((t.dsem, t.dcount))
            self.all_dtoks.remove(t)
            t.dsem = None
        self.stage_toks = []

    def _collect(self, E, reads, writes, skip_sid=None):
        need = {}

        def add(p, same_ok):
            if p is None:
                return
            sid, sem, val = p
            if sid == skip_sid:
                return
            if sid == id(E.sem) and (not same_ok or E.name == "pe"):
                return
            if need.get(sid, (None, -1))[1] < val:
                need[sid] = (sem, val)

        for t in reads:
            add(t.writer, True)
        for t in writes:
            add(t.writer, True)
            for sid, (sem, val) in t.readers.items():
                add((sid, sem, val), False)
        waits = []
        for sid, (sem, val) in need.items():
            if E.waited.get(sid, 0) < val:
                E.waited[sid] = val
                waits.append((sem, val))
        return waits

    def op(self, eng, fn, reads=(), writes=(), inc=True):
        if self.rec is not None:
            self.rec.append(("op", (eng, fn, tuple(reads), tuple(writes), inc)))
            return
        E = self.engs[eng]
        waits = self._collect(E, reads, writes)
        if inc:
            E.count += 1
            cnt = E.count
            incs = [(E.sem, 1)]
        else:
            cnt = E.count + 1
            incs = []
        me = (id(E.sem), E.sem, cnt)
        E.ops.append((waits, fn, incs))
        for t in reads:
            t.readers[id(E.sem)] = (E.sem, cnt)
        for t in writes:
            t.writer = me
            t.readers = {}

    def dma(self, eng, fn, reads=(), writes=(), inc=16, serial=False):
        if self.rec is not None:
            self.rec.append(("dma", (eng, fn, tuple(reads), tuple(writes), inc, serial)))
            return
        E = self.engs[eng]
        t0 = writes[0]
        sem = self._dsem(t0)
        waits = self._collect(E, reads, writes, skip_sid=None if serial else id(sem))
        t0.dcount += inc
        me = (id(sem), sem, t0.dcount)
        E.ops.append((waits, fn, [(sem, inc)]))
        for t in reads:
            t.readers[id(sem)] = (sem, t0.dcount)
        for t in writes:
            t.writer = me
            t.readers = {}

    def replay(self, q, n):
        assert self.rec is None
        while n > 0 and q:
            kind, args = q.pop(0)
            if kind == "op":
                self.op(*args)
            else:
                self.dma(*args)
            n -= 1

    def wait_all(self, eng, toks):
        E = self.engs[eng]
        waits = self._collect(E, toks, ())
        E.ops.append((waits, None, []))

    def emit(self):
        nc = self.nc
        with nc.Block() as block:
            def run(E, h):
                for waits, fn, incs in E.ops:
                    for sem, val in waits:
                        h.wait_ge(sem, val)
                    if fn is None:
                        continue
                    r = fn(h)
                    for sem, v in incs:
                        r.then_inc(sem, v)

            @block.tensor
            def _(h):
                run(self.engs["pe"], h)

            @block.scalar
            def _(h):
                run(self.engs["act"], h)

            @block.vector
            def _(h):
                run(self.engs["dve"], h)

            @block.gpsimd
            def _(h):
                run(self.engs["pool"], h)

            @block.sync
            def _(h):
                run(self.engs["sp"], h)
        for E in self.engs.values():
            E.ops = []


class Ctx:
    def __init__(self, nc, es):
        self.nc = nc
        self.P = Prog(nc, es)
        P = self.P
        self.banks = [(P.psum([128, 512], F32, name=f"bank{i}"), P.tok(f"bank{i}")) for i in range(8)]
        self.bi = 0
        self.pre = None
        self.pbi = 0
        self.ones = P.sbuf([128, 128], BF16, "ones_bf")
        self.t_ones = P.tok("ones")
        self.epsc = P.sbuf([128, 1], F32, "epsc")
        self.t_eps = P.tok("eps")
        P.op("pool", lambda h: h.memset(self.ones[:], 1.0), writes=[self.t_ones])
        P.op("pool", lambda h: h.memset(self.epsc[:], EPS), writes=[self.t_eps])
        self.sq = [(P.sbuf([128, 512], BF16, f"sq{i}"), P.tok(f"sq{i}")) for i in range(2)]
        self.sqi = 0
        self.dq = 0

    def bank(self):
        if self.pre is not None:
            b = self.pre[self.pbi % len(self.pre)]
            self.pbi += 1
            return b
        b = self.banks[self.bi % len(self.banks)]
        self.bi += 1
        return b

    def reserve(self, n):
        r = self.banks[-n:]
        self.banks = self.banks[:-n]
        return r

    def tile(self, shape, dt, name=None):
        t = self.P.sbuf(shape, dt, name)
        return t, self.P.tok(name or "tile")

    def ACT(self, out, in_, func, r, w, **kw):
        self.P.op("act", lambda h: h.activation(out=out, in_=in_, func=func, **kw), r, w)

    def MM(self, out, lhsT, rhs, start, stop, r, w, lazy=False):
        self.P.op("pe", lambda h: h.matmul(out, lhsT=lhsT, rhs=rhs, start=start, stop=stop), r, w,
                  inc=(stop or not lazy))

    def TT(self, eng, out, in0, in1, op, r, w):
        self.P.op(eng, lambda h: h.tensor_tensor(out=out, in0=in0, in1=in1, op=op), r, w)

    def TS(self, eng, out, in0, s1, s2, op0, op1, r, w):
        if s2 is None:
            self.P.op(eng, lambda h: h.tensor_scalar(out=out, in0=in0, scalar1=s1, scalar2=None, op0=op0), r, w)
        else:
            self.P.op(eng, lambda h: h.tensor_scalar(out=out, in0=in0, scalar1=s1, scalar2=s2, op0=op0, op1=op1), r, w)

    def STT(self, eng, out, in0, scalar, in1, op0, op1, r, w):
        self.P.op(eng, lambda h: h.scalar_tensor_tensor(out=out, in0=in0, scalar=scalar, in1=in1, op0=op0, op1=op1), r, w)

    def CP(self, eng, out, in_, r, w):
        self.P.op(eng, lambda h: h.tensor_copy(out=out, in_=in_), r, w)

    def RED(self, eng, out, in_, op, r, w):
        self.P.op(eng, lambda h: h.tensor_reduce(out=out, in_=in_, axis=AX.X, op=op), r, w)

    def RECIP(self, out, in_, r, w):
        self.P.op("dve", lambda h: h.reciprocal(out=out, in_=in_), r, w)

    def MEMSET(self, eng, ap, val, w):
        self.P.op(eng, lambda h: h.memset(ap, val), (), w)

    def DMA(self, out, in_, r, w, eng=None):
        if eng is None:
            eng = "sp"
            self.dq += 1
        self.P.dma(eng, lambda h: h.dma_start(out=out, in_=in_), r, w)

    def load(self, dram_ap, shape, dt, name, eng=None):
        t, tk = self.tile(shape, dt, name)
        self.DMA(t[:], dram_ap, [], [tk], eng=eng)
        return t, tk

    def rstd_bc(self, chunks, n, inv_d, out, t_out, ones_ap=None, np_out=128):
        bank, tb = self.bank()
        last = len(chunks) - 1
        for k, (ap, tk) in enumerate(chunks):
            sq, tsq = self.sq[self.sqi % 2]
            self.sqi += 1
            p = ap.shape[0]
            self.ACT(sq[:p, :n], ap, AF.Square, [tk], [tsq])
            lhs = self.ones[:p, :np_out] if ones_ap is None else ones_ap
            self.MM(bank[:np_out, :n], lhs, sq[:p, :n], k == 0, k == last, [tsq, self.t_ones], [tb])
        self.ACT(out, bank[:np_out, :n], AF.Ln, [tb, self.t_eps], [t_out], scale=inv_d, bias=self.epsc[:np_out, :])
        self.ACT(out, out, AF.Exp, [t_out], [t_out], scale=-0.5)

    def mod_cols(self, cbc_d, adaT_d, adab_d, nvec, scratch):
        cb, t_cb = scratch[0]
        self.DMA(cb[:], cbc_d, [], [t_cb])
        ca, t_ca = scratch[1]
        self.ACT(ca[:], cb[:], AF.Silu, [t_cb], [t_ca])
        ab, t_ab = self.load(adab_d, [128, nvec * KC], F32, "adab")
        mod, t_mod = self.tile([128, nvec * KC], F32, "mod")
        abuf = scratch[2:4]
        prod, t_prod = scratch[4]
        for j in range(nvec * KC):
            a, t_a = abuf[j % 2]
            self.DMA(a[:], adaT_d[j], [], [t_a])
            self.TT("dve", prod[:], a[:], ca[:], ALU.mult, [t_a, t_ca], [t_prod])
            self.RED("dve", mod[:, j:j + 1], prod[:], ALU.add, [t_prod], [t_mod])
        self.TT("dve", mod[:], mod[:], ab[:], ALU.add, [t_mod, t_ab], [t_mod])
        return mod, t_mod


def emit_moe(C, x, tx, x0, mod, t_mod, vb, g2_d, wr_d, br_d, wg_d, wu_d, wd_d, ident_d, stg):
    P = C.P
    g2, t_g2 = C.load(g2_d, [128, KC], F32, "g2")
    wr, t_wr = C.load(wr_d, [128, KC, 36], F32, "wr")
    br, t_br = C.load(br_d, [128, 36], F32, "br")
    ident, t_id = C.load(ident_d, [128, 128], F32, "ident")
    sel, t_sel = C.tile([32, NE, 128], BF16, "gsel")
    C.CP("dve", sel[:], ident[0:32, 0:NE].unsqueeze(2).to_broadcast([32, NE, 128]), [t_id], [t_sel])
    a2, t_a2 = C.tile([128, KC], F32, "a2")
    C.TS("dve", a2[:], mod[:, vb + 8:vb + 16], 1.0, None, ALU.add, None, [t_mod], [t_a2])
    C.TT("dve", a2[:], a2[:], g2[:], ALU.mult, [t_a2, t_g2], [t_a2])
    sh2 = mod[:, vb:vb + 8]
    gt2 = mod[:, vb + 16:vb + 24]

    h2 = P.sbuf([128, KC, NT], BF16, "h2")
    th2 = [[P.tok(f"h2_{k}_{i}") for i in range(NTILE)] for k in range(KC)]
    NB = NT // 128
    logits, t_lg = C.tile([128, NB, 36], F32, "logits")
    rs, t_rs = C.tile([128, TN], F32, "rs2")
    tmpb = [C.tile([128, TN], F32, f"n2tmp{i}") for i in range(2)]
    hfb = [C.tile([128, TN], F32, f"n2hf{i}") for i in range(2)]

    for i in range(NTILE):
        c0 = x0 + i * TN
        C.rstd_bc([(x[:, k, c0:c0 + TN], tx[k][i]) for k in range(KC)], TN, 1.0 / D, rs[:], t_rs)
        bls = [C.bank() for _ in range(4)]
        for k in range(KC):
            tmp, t_tmp = tmpb[k % 2]
            hf, t_hf = hfb[k % 2]
            C.STT("dve", tmp[:], x[:, k, c0:c0 + TN], a2[:, k:k + 1], rs[:], ALU.mult, ALU.mult,
                  [tx[k][i], t_a2, t_rs], [t_tmp])
            C.ACT(hf[:], tmp[:], AF.Identity, [t_tmp, t_mod], [t_hf], bias=sh2[:, k:k + 1], scale=1.0)
            for b in range(4):
                C.MM(bls[b][0][:, 0:36], hf[:, b * 128:(b + 1) * 128], wr[:, k, :], k == 0, k == KC - 1,
                     [t_hf, t_wr], [bls[b][1]])
            C.CP("pool", h2[:, k, i * TN:(i + 1) * TN], hf[:], [t_hf], [th2[k][i]])
        for b in range(4):
            C.TT("dve", logits[:, i * 4 + b, :], bls[b][0][:, 0:36], br[:], ALU.add, [bls[b][1], t_br], [t_lg])

    def T(shape, name):
        return C.tile(shape, F32, name)

    gl = logits[:, :, 0:4]
    el = logits[:, :, 4:36].rearrange("p b (g i) -> p b g i", i=8)
    gmax, t_gmax = T([128, NB], "gmax")
    C.RED("dve", gmax[:], gl, ALU.max, [t_lg], [t_gmax])
    ohg, t_ohg = T([128, NB, 4], "ohg")
    C.TT("dve", ohg[:], gl, gmax[:].unsqueeze(2).to_broadcast([128, NB, 4]), ALU.is_equal, [t_lg, t_gmax], [t_ohg])
    gex, t_gex = T([128, NB, 4], "gex")
    C.TT("dve", gex[:], gl, gmax[:].unsqueeze(2).to_broadcast([128, NB, 4]), ALU.subtract, [t_lg, t_gmax], [t_gex])
    C.ACT(gex[:], gex[:], AF.Exp, [t_gex], [t_gex])
    gw, t_gw = T([128, NB], "gw")
    C.RED("dve", gw[:], gex[:], ALU.add, [t_gex], [t_gw])
    C.RECIP(gw[:], gw[:], [t_gw], [t_gw])
    tmp4, t_tmp4 = T([128, NB, 4, 8], "tmp4")
    C.TT("dve", tmp4[:], el, ohg[:].unsqueeze(3).to_broadcast([128, NB, 4, 8]), ALU.mult, [t_lg, t_ohg], [t_tmp4])
    sel8, t_sel8 = T([128, NB, 8], "sel8")
    C.RED("dve", sel8[:], tmp4[:].rearrange("p b g i -> p b i g"), ALU.add, [t_tmp4], [t_sel8])
    m1, t_m1 = T([128, NB], "m1")
    C.RED("dve", m1[:], sel8[:], ALU.max, [t_sel8], [t_m1])
    oh1, t_oh1 = T([128, NB, 8], "oh1")
    C.TT("dve", oh1[:], sel8[:], m1[:].unsqueeze(2).to_broadcast([128, NB, 8]), ALU.is_equal, [t_sel8, t_m1], [t_oh1])
    sel2, t_sel2 = T([128, NB, 8], "sel2")
    C.STT("dve", sel2[:], oh1[:], -1e30, sel8[:], ALU.mult, ALU.add, [t_oh1, t_sel8], [t_sel2])
    m2, t_m2 = T([128, NB], "m2")
    C.RED("dve", m2[:], sel2[:], ALU.max, [t_sel2], [t_m2])
    oh2, t_oh2 = T([128, NB, 8], "oh2")
    C.TT("dve", oh2[:], sel2[:], m2[:].unsqueeze(2).to_broadcast([128, NB, 8]), ALU.is_equal, [t_sel2, t_m2], [t_oh2])
    rr, t_rr = T([128, NB], "rr")
    C.TT("dve", rr[:], m2[:], m1[:], ALU.subtract, [t_m1, t_m2], [t_rr])
    C.ACT(rr[:], rr[:], AF.Exp, [t_rr], [t_rr])
    w1, t_w1 = T([128, NB], "w1")
    C.TS("dve", w1[:], rr[:], 1.0, None, ALU.add, None, [t_rr], [t_w1])
    C.RECIP(w1[:], w1[:], [t_w1], [t_w1])
    w2, t_w2 = T([128, NB], "w2")
    C.TT("dve", w2[:], rr[:], w1[:], ALU.mult, [t_rr, t_w1], [t_w2])
    C.TT("dve", w1[:], w1[:], gw[:], ALU.mult, [t_w1, t_gw], [t_w1])
    C.TT("dve", w2[:], w2[:], gw[:], ALU.mult, [t_w2, t_gw], [t_w2])
    C.TT("dve", oh1[:], oh1[:], w1[:].unsqueeze(2).to_broadcast([128, NB, 8]), ALU.mult, [t_oh1, t_w1], [t_oh1])
    C.TT("dve", oh2[:], oh2[:], w2[:].unsqueeze(2).to_broadcast([128, NB, 8]), ALU.mult, [t_oh2, t_w2], [t_oh2])
    C.TT("dve", oh1[:], oh1[:], oh2[:], ALU.add, [t_oh1, t_oh2], [t_oh1])
    gates, t_gates = T([128, NB, 4, 8], "gates")
    C.TT("dve", gates[:], ohg[:].unsqueeze(3).to_broadcast([128, NB, 4, 8]),
         oh1[:].unsqueeze(2).to_broadcast([128, NB, 4, 8]), ALU.mult, [t_ohg, t_oh1], [t_gates])

    GT, t_GT = C.tile([32, NT], F32, "GT")
    GTh, t_GTh = C.tile([32, NT], BF16, "GTh")
    GTl, t_GTl = C.tile([32, NT], BF16, "GTl")
    for i in range(NTILE):
        bk, tb = C.bank()
        for b in range(4):
            C.MM(bk[0:32, b * 128:(b + 1) * 128], gates[:, i * 4 + b, :, :].rearrange("p g i -> p (g i)"), ident[:],
                 True, True, [t_gates, t_id], [tb])
        C.ACT(GT[:, i * TN:(i + 1) * TN], bk[0:32, :], AF.Copy, [tb], [t_GT])
        C.CP("dve", GTh[:, i * TN:(i + 1) * TN], GT[:, i * TN:(i + 1) * TN], [t_GT], [t_GTh])
        C.TT("dve", GTl[:, i * TN:(i + 1) * TN], GT[:, i * TN:(i + 1) * TN], GTh[:, i * TN:(i + 1) * TN], ALU.subtract,
             [t_GT, t_GTh], [t_GTl])

    wb = [[C.tile([128, 2048], BF16, f"wb{j}_{s}") for s in range(2)] for j in range(3)]
    sgb = tmpb
    t1b = hfb
    acb = [[C.tile([128, TN], BF16, f"actp{c}_{i}") for i in range(2)] for c in range(2)]

    def load_w(e):
        s = e % 2
        srcs = (wg_d[e].rearrange("(k p) f -> p k f", p=128), wu_d[e].rearrange("(k p) f -> p k f", p=128),
                wd_d[e].rearrange("(k p) f -> p k f", p=128))
        shp = ((KC, FF), (KC, FF), (2, D))
        for j in range(3):
            w, t_w = wb[j][s]
            kk = shp[j][0] // 2
            for hh in range(2):
                st, t_st = stg[j][hh]
                C.DMA(st[:].rearrange("p (k f) -> p k f", k=kk), srcs[j][:, hh * kk:(hh + 1) * kk, :], [], [t_st])
                C.CP("pool", w[:, hh * 1024:(hh + 1) * 1024], st[:], [t_st], [t_w])

    units = [(e, i) for e in range(NE) for i in range(NTILE)]
    state = {}

    def GU(u):
        e, i = units[u]
        s = e % 2
        wgb, t_wgb = wb[0][s]
        wub, t_wub = wb[1][s]
        wgv = wgb[:].rearrange("p (k f) -> p k f", k=KC)
        wuv = wub[:].rearrange("p (k f) -> p k f", k=KC)
        bg, tbg = C.bank()
        C.MM(bg[:], sel[:, e, :], GTh[:, i * TN:(i + 1) * TN], True, False, [t_sel, t_GTh], [tbg], lazy=True)
        C.MM(bg[:], sel[:, e, :], GTl[:, i * TN:(i + 1) * TN], False, True, [t_sel, t_GTl], [tbg], lazy=True)
        acts = []
        for c in range(2):
            bga, tbga = C.bank()
            bup, tbup = C.bank()
            for k in range(KC):
                C.MM(bga[:], wgv[:, k, c * 128:(c + 1) * 128], h2[:, k, i * TN:(i + 1) * TN], k == 0, k == KC - 1,
                     [t_wgb, th2[k][i]], [tbga], lazy=True)
            for k in range(KC):
                C.MM(bup[:], wuv[:, k, c * 128:(c + 1) * 128], h2[:, k, i * TN:(i + 1) * TN], k == 0, k == KC - 1,
                     [t_wub, th2[k][i]], [tbup], lazy=True)
            sg, t_sg = sgb[c]
            t1, t_t1 = t1b[c]
            ac, t_ac = acb[c][u % 2]
            C.ACT(sg[:], bga[:], AF.Silu, [tbga], [t_sg])
            C.TT("dve", t1[:], sg[:], bg[:], ALU.mult, [t_sg, tbg], [t_t1])
            C.TT("dve", ac[:], t1[:], bup[:], ALU.mult, [t_t1, tbup], [t_ac])
            acts.append((ac, t_ac))
        state[u] = acts

    def DOWN(u):
        e, i = units[u]
        s = e % 2
        wdb, t_wdb = wb[2][s]
        wdv = wdb[:].rearrange("p (k f) -> p k f", k=2)
        acts = state.pop(u)
        c0 = x0 + i * TN
        for m in range(KC):
            bd, tbd = C.bank()
            for c in range(2):
                C.MM(bd[:], wdv[:, c, m * 128:(m + 1) * 128], acts[c][0][:], c == 0, c == 1,
                     [t_wdb, acts[c][1]], [tbd], lazy=True)
            C.STT("dve", x[:, m, c0:c0 + TN], bd[:], gt2[:, m:m + 1], x[:, m, c0:c0 + TN], ALU.mult, ALU.add,
                  [tbd, t_mod, tx[m][i]], [tx[m][i]])

    load_w(0)
    for u in range(len(units)):
        e, i = units[u]
        if i == 0 and e + 1 < NE:
            load_w(e + 1)
        if u == 0:
            GU(0)
        if u + 1 < len(units):
            GU(u + 1)
        DOWN(u)


POOL_W = (2, 4, 8, 16)
PI = float(np.pi)
PERM96 = list(range(64)) + list(range(80, 96)) + list(range(64, 80))
G4 = [[0, 1, 2, 3], [4, 5, 6, 7]]
G2 = [[0, 4], [1, 5], [2, 6], [3, 7]]
NZ_ATT = 1440
ATT_ROWS = [128] * 11 + [32]


def stage(C, fn):
    P = C.P
    with ExitStack() as es_s:
        P.es = es_s
        P.in_stage = True
        fn()
        P.end_stage()
    P.es = P.es_outer
    P.in_stage = False


def CC(C, kind, op, groups, src_ap, dst_ap, r_toks, w_tok):
    C.P.dma("pool", lambda h: h.collective_compute(kind, op, replica_groups=groups, ins=[src_ap], outs=[dst_ap]),
            r_toks, [w_tok], inc=1, serial=True)


def st_normproj(C, G, l, w_d, nout, dst_d, t_dst, dst_dt, halo):
    P = C.P
    x, tx = G["x"], G["tx"]
    nj = (nout + 127) // 128
    scr = [C.tile([128, D], F32, f"scr{i}") for i in range(5)]
    mod, t_mod = C.mod_cols(G["cbc"], G["ada"][l][0:16], G["adab"][l][:, 0:16], 2, scr)
    g1s, t_g1 = C.load(G["g1"][l], [128, KC], F32, "g1")
    a1, t_a1 = C.tile([128, KC], F32, "a1")
    C.TS("dve", a1[:], mod[:, 8:16], 1.0, None, ALU.add, None, [t_mod], [t_a1])
    C.TT("dve", a1[:], a1[:], g1s[:], ALU.mult, [t_a1, t_g1], [t_a1])
    wbf, t_wbf = C.tile([128, KC, nout], BF16, "wbf")
    wst = [C.tile([128, nout], F32, f"wst{i}") for i in range(2)]
    for k in range(KC):
        st, t_st = wst[k % 2]
        C.DMA(st[:], w_d[k * 128:(k + 1) * 128, :], [], [t_st])
        C.CP("pool", wbf[:, k, :], st[:], [t_st], [t_wbf])
    rs, t_rs = C.tile([128, TN], F32, "rs")
    tmpb = [C.tile([128, TN], F32, f"tmp{i}") for i in range(2)]
    hb, t_hb = C.tile([128, KC, TN], BF16, "hb")
    zo = [C.tile([128, TN], dst_dt, f"zo{i}") for i in range(4)]
    zst = [P.tok(f"zst{i}") for i in range(4)]
    for i in range(NTILE):
        c0 = i * TN
        C.rstd_bc([(x[:, k, c0:c0 + TN], tx[k][i]) for k in range(KC)], TN, 1.0 / D, rs[:], t_rs)
        for k in range(KC):
            tmp, t_tmp = tmpb[k % 2]
            C.STT("dve", tmp[:], x[:, k, c0:c0 + TN], a1[:, k:k + 1], rs[:], ALU.mult, ALU.mult,
                  [tx[k][i], t_a1, t_rs], [t_tmp])
            C.ACT(hb[:, k, :], tmp[:], AF.Identity, [t_tmp, t_mod], [t_hb], bias=mod[:, k:k + 1], scale=1.0)
        for j in range(nj):
            pj = min(128, nout - j * 128)
            bk, tb = C.bank()
            for k in range(KC):
                C.MM(bk[:pj, :], wbf[:, k, j * 128:j * 128 + pj], hb[:, k, :], k == 0, k == KC - 1, [t_wbf, t_hb], [tb],
                     lazy=True)
            z, t_z = zo[j % 4]
            t_zst = zst[j % 4]
            if j % 2 == 0:
                C.ACT(z[:pj, :], bk[:pj, :], AF.Copy, [tb], [t_z])
            else:
                C.CP("dve", z[:pj, :], bk[:pj, :], [tb], [t_z])
            if isinstance(dst_d, list):
                C.DMA(dst_d[j][0:pj, c0:c0 + TN], z[:pj, :], [t_z], [t_zst])
            else:
                C.DMA(dst_d[j * 128:j * 128 + pj, c0:c0 + TN], z[:pj, :], [t_z], [t_zst])
            if halo and i == NTILE - 1 and j >= 4:
                C.DMA(G["hsrc"][:, (j - 4) * 16:(j - 3) * 16], z[:, TN - 16:TN], [t_z], [G["t_hsrc"]])


def st_evenmix(C, G, i_even):
    P = C.P
    zT, t_zs = G["zs"], G["t_zs"]
    yT, t_ys = G["ys"], G["t_ys"]
    HB = 16
    W = HB + TN
    CC(C, "AllGather", ALU.bypass, G4, G["hsrc"], G["h4"], [G["t_hsrc"]], G["t_h4"])
    H, t_H = C.tile([128, 8, 192], F32, "H")
    hsel, t_hsel = C.load(G["hsel"], [128, 8], F32, "hsel")
    halo, t_halo = C.tile([128, 192], F32, "halo")

    def halo_step(n):
        if n == 0:
            CC(C, "AllGather", ALU.bypass, G2, G["h4"], G["h8"], [G["t_h4"]], G["t_h8"])
        elif n == 1:
            C.DMA(H[:], G["h8"].rearrange("(r p) f -> p r f", p=128), [G["t_h8"]], [t_H])
            C.TS("dve", halo[:], H[:, 0, :], hsel[:, 0:1], None, ALU.mult, None, [t_H, t_hsel], [t_halo])
            for r in range(1, 8):
                C.STT("dve", halo[:], H[:, r, :], hsel[:, r:r + 1], halo[:], ALU.mult, ALU.add,
                      [t_H, t_hsel, t_halo], [t_halo])
    cr, t_cr = C.load(G["corr"], [128, 4, 16], F32, "corr")
    cw, t_cw = C.load(G["convw"][i_even], [128, 4, 3], F32, "convw")
    pwf, t_pwf = C.load(G["poolw"][i_even], [128, 4, 128], F32, "poolwf")
    pw, t_pw = C.tile([128, 4, 128], BF16, "poolw")
    C.CP("pool", pw[:], pwf[:], [t_pwf], [t_pw])
    ps, t_ps = C.load(G["pscale"][i_even], [128, 4], F32, "pscale")
    R = lambda n: [C.tile([128, W], F32, f"{n}{i}") for i in range(2)]
    cb_, xb_, ub_, sA, sB, vb_ = R("c"), R("xa"), R("u"), R("sA"), R("sB"), R("v")
    bb_ = [C.tile([128, TN], F32, f"b{i}") for i in range(2)]
    t0b = [C.tile([128, TN], F32, f"t0{i}") for i in range(2)]
    yab = [C.tile([128, TN], F32, f"ya{i}") for i in range(2)]
    ybb = [C.tile([128, TN], F32, f"yb{i}") for i in range(2)]
    db = [C.tile([128, TN], BF16, f"d{i}") for i in range(2)]
    t16 = [C.tile([128, 16], F32, f"t16{i}") for i in range(2)]
    it = 0
    yast = [P.tok(f"yast{i}") for i in range(2)]
    ybst = [P.tok(f"ybst{i}") for i in range(2)]

    def load_halo(buf, tk, row0, i):
        c0 = i * TN
        if i == 0:
            j = (row0 - 512) // 128
            C.DMA(buf[:, HB:W], zT[row0:row0 + 128, 0:TN], [t_zs], [tk])
            C.ACT(buf[:, 0:HB], halo[:, j * 16:(j + 1) * 16], AF.Copy, [t_halo], [tk])
        else:
            C.DMA(buf[:, :], zT[row0:row0 + 128, c0 - HB:c0 + TN], [t_zs], [tk])

    for n_i, i in enumerate(list(range(1, NTILE)) + [0]):
        if n_i in (1, 2):
            halo_step(n_i - 1)
        c0 = i * TN
        for g in range(4):
            s = it % 2
            it += 1
            cc, t_c = cb_[s]
            xa, t_xa = xb_[s]
            bb, t_b = bb_[s]
            v, t_v = vb_[s]
            load_halo(cc, t_c, (4 + g) * 128, i)
            load_halo(xa, t_xa, (8 + g) * 128, i)
            C.DMA(bb[:], zT[g * 128:(g + 1) * 128, c0:c0 + TN], [t_zs], [t_b])
            C.TT("pool", v[:], cc[:], xa[:], ALU.mult, [t_c, t_xa], [t_v])
            t0, t_t0 = t0b[s]
            C.ACT(t0[:], v[:, HB:W], AF.Copy, [t_v, t_cw], [t_t0], scale=cw[:, g, 0:1])
            C.STT("dve", t0[:], v[:, HB - 1:W - 1], cw[:, g, 1:2], t0[:], ALU.mult, ALU.add, [t_v, t_cw, t_t0], [t_t0])
            C.STT("dve", t0[:], v[:, HB - 2:W - 2], cw[:, g, 2:3], t0[:], ALU.mult, ALU.add, [t_v, t_cw, t_t0], [t_t0])
            ya, t_ya = yab[s]
            C.TT("pool", ya[:], t0[:], bb[:], ALU.mult, [t_t0, t_b], [t_ya])
            C.DMA(yT[g * 128:(g + 1) * 128, c0:c0 + TN], ya[:], [t_ya], [yast[s]], eng="act")
            u, t_u = ub_[s]
            load_halo(u, t_u, (12 + g) * 128, i)
            a, t_a = sA[s]
            b2, t_b2 = sB[s]
            src, t_src = u, t_u
            sh = 1
            for st in range(g + 1):
                dst, t_dst = (a, t_a) if st % 2 == 0 else (b2, t_b2)
                lo = 2 * sh - 1
                C.TT("dve", dst[:, lo:W], src[:, lo:W], src[:, lo - sh:W - sh], ALU.add, [t_src], [t_dst])
                src, t_src = dst, t_dst
                sh *= 2
            wdw = float(POOL_W[g])
            d, t_d = db[s]
            C.STT("dve", d[:], src[:, HB:W], 1.0 / wdw, u[:, HB:W], ALU.mult, ALU.subtract, [t_src, t_u], [t_d])
            if i == 0:
                tt, t_tt = t16[s]
                C.TT("dve", tt[:], src[:, HB:HB + 16], cr[:, g, :], ALU.mult, [t_src, t_cr], [t_tt])
                C.TT("dve", d[:, 0:16], tt[:], u[:, HB:HB + 16], ALU.subtract, [t_tt, t_u], [t_d])
            bk, tb = C.bank()
            C.MM(bk[:], pw[:, g, :], d[:], True, True, [t_pw, t_d], [tb])
            yb, t_yb = ybb[s]
            C.ACT(yb[:], bk[:], AF.Copy, [tb, t_ps], [t_yb], scale=ps[:, g:g + 1])
            C.DMA(yT[(4 + g) * 128:(5 + g) * 128, c0:c0 + TN], yb[:], [t_yb], [ybst[s]], eng="act")


def st_proj(C, G, l, w_d):
    P = C.P
    x, tx = G["x"], G["tx"]
    scr = [C.tile([128, D], F32, f"scr{i}") for i in range(5)]
    mod, t_mod = C.mod_cols(G["cbc"], G["ada"][l][16:24], G["adab"][l][:, 16:24], 1, scr)
    if w_d is None:
        yb = [C.tile([128, TN], BF16, f"yrd{i}") for i in range(4)]
        q = 0
        for i in range(NTILE):
            c0 = i * TN
            for m in range(KC):
                y, t_y = yb[q % 4]
                q += 1
                C.DMA(y[:], G["yred"][i][m * 128:(m + 1) * 128, :], [G["t_yred"]], [t_y])
                C.STT("dve", x[:, m, c0:c0 + TN], y[:], mod[:, m:m + 1], x[:, m, c0:c0 + TN], ALU.mult, ALU.add,
                      [t_y, t_mod, tx[m][i]], [tx[m][i]])
        return
    wbf, t_wbf = C.tile([128, KC, D], BF16, "wbf")
    wst = [C.tile([128, D], F32, f"wst{i}") for i in range(2)]
    for k in range(KC):
        st, t_st = wst[k % 2]
        C.DMA(st[:], w_d[k * 128:(k + 1) * 128, :], [], [t_st])
        C.CP("pool", wbf[:, k, :], st[:], [t_st], [t_wbf])
    yst = [C.tile([128, TN], F32, f"yst{i}") for i in range(3)]
    ybf = [[C.tile([128, TN], BF16, f"ybf{k}_{s}") for s in range(2)] for k in range(KC)]
    q = 0
    for i in range(NTILE):
        c0 = i * TN
        for k in range(KC):
            st, t_st = yst[q % 3]
            q += 1
            C.DMA(st[:], G["ys"][k * 128:(k + 1) * 128, c0:c0 + TN], [G["t_ys"]], [t_st])
            C.CP("pool", ybf[k][i % 2][0][:], st[:], [t_st], [ybf[k][i % 2][1]])
        for m in range(KC):
            bk, tb = C.bank()
            for k in range(KC):
                C.MM(bk[:], wbf[:, k, m * 128:(m + 1) * 128], ybf[k][i % 2][0][:], k == 0, k == KC - 1,
                     [t_wbf, ybf[k][i % 2][1]], [tb], lazy=True)
            C.STT("dve", x[:, m, c0:c0 + TN], bk[:], mod[:, m:m + 1], x[:, m, c0:c0 + TN], ALU.mult, ALU.add,
                  [tb, t_mod, tx[m][i]], [tx[m][i]])


def st_moe(C, G, l):
    stg = [[C.tile([128, 1024], F32, f"stg{j}_{hh}") for hh in range(2)] for j in range(3)]
    mod, t_mod = C.mod_cols(G["cbc"], G["ada"][l][24:48], G["adab"][l][:, 24:48], 3,
                            [stg[0][0], stg[0][1], stg[1][0], stg[1][1], stg[2][0]])
    emit_moe(C, G["x"], G["tx"], 0, mod, t_mod, 0, G["g2"][l], G["wr"][l], G["br"][l], G["wg"][l], G["wu"][l],
             G["wd"][l], G["ident"], stg)


def st_attn(C, G, ia):
    P = C.P
    T = S
    NQ = T // TN
    NBK = T // 128
    g8 = G["g8"]
    GA = (9, 10, 11)
    t_g4a, t_g8a, t_g4b, t_g8b = P.tok("g4a"), P.tok("g8a"), P.tok("g4b"), P.tok("g8b")

    def g8tok(f0):
        return t_g8a if (f0 // 128) in GA else t_g8b

    def gathers():
        for j in GA:
            CC(C, "AllGather", ALU.bypass, G4, G["gsrc"][j], G["g4"][j], [G["t_gsrc"], t_g8a], t_g4a)
            CC(C, "AllGather", ALU.bypass, G2, G["g4"][j], g8[j], [t_g4a], t_g8a)
        for j in range(len(g8)):
            if j in GA:
                continue
            CC(C, "AllGather", ALU.bypass, G4, G["gsrc"][j], G["g4"][j], [G["t_gsrc"], t_g8a, t_g8b], t_g4b)
            CC(C, "AllGather", ALU.bypass, G2, G["g4"][j], g8[j], [t_g4b], t_g8b)

    def gsl(f0, n, t):
        r = t // 4
        lc = (t % 4) * TN
        j = f0 // 128
        rows = ATT_ROWS[j]
        o = f0 % 128
        assert o + n <= rows
        return g8[j][r * rows + o:r * rows + o + n, lc:lc + TN]

    accs = C.reserve(2)
    cols, t_cols = C.load(G["cols"][ia], [128, 16], F32, "cols")
    ng, t_ng = C.tile([128, 4], F32, "ng")
    C.TS("dve", ng[:96, 0:1], cols[:96, 7:8], 1.0, None, ALU.mult, None, [t_cols], [t_ng])
    C.STT("dve", ng[:96, 1:2], cols[:96, 8:9], 1.0, cols[:96, 12:13], ALU.mult, ALU.mult, [t_cols, t_ng], [t_ng])
    C.TS("dve", ng[:96, 2:3], cols[:96, 9:10], 1.0, None, ALU.mult, None, [t_cols, t_ng], [t_ng])
    C.STT("dve", ng[:96, 3:4], cols[:96, 10:11], 1.0, cols[:96, 12:13], ALU.mult, ALU.mult, [t_cols, t_ng], [t_ng])
    esk, t_esk = C.tile([128, 1], F32, "esk")
    C.ACT(esk[:], cols[:, 13:14], AF.Exp, [t_cols], [t_esk])

    wstage, t_wstage = C.tile([128, 1024], F32, "wstage")

    def wload(shape, name, view):
        n = int(np.prod(shape[1:]))
        p = shape[0]
        if len(shape) == 3:
            sv = wstage[:p, :n].rearrange("p (k f) -> p k f", k=shape[1])
        else:
            sv = wstage[:p, :n]
        C.DMA(sv, view, [], [t_wstage])
        b, t_b = C.tile(shape, BF16, name)
        C.CP("pool", b[:], sv, [t_wstage], [t_b])
        return b, t_b

    wuq_d, wuqs_d, wk_d, wks_d, wv_d, wo_d = (G[n][ia] for n in ("wuq", "wuqs", "wk", "wks", "wv", "wo"))
    wuq, t_wuq = wload([128, 3, 96], "wuq", wuq_d.rearrange("(k p) f -> p k f", p=128))
    wuqs, t_wuqs = wload([128, 3, 96], "wuqs", wuqs_d.rearrange("(k p) f -> p k f", p=128))
    wk, t_wk = wload([128, 2, 96], "wk", wk_d[0:256, :].rearrange("(k p) f -> p k f", p=128))
    wks, t_wks = wload([128, 2, 96], "wks", wks_d[0:256, :].rearrange("(k p) f -> p k f", p=128))
    wkr, t_wkr = wload([32, 96], "wkr", wk_d[256:288, :])
    wkrs, t_wkrs = wload([32, 96], "wkrs", wks_d[256:288, :])
    wv, t_wv = wload([128, 2, 64], "wv", wv_d.rearrange("(k p) f -> p k f", p=128))
    wos, t_wos = wload([64, D], "wos", wo_d[0:64, :])
    wom, t_wom = wload([64, D], "wom", wo_d[64:128, :])
    masks, t_masks = C.tile([128, 9, TN], BF16, "masks")
    C.DMA(masks[:], G["masks"].rearrange("j p q -> p j q"), [], [t_masks])
    selq, t_selq = C.load(G["selq"], [128, 4, 64], BF16, "selq")
    selkv, t_selkv = C.load(G["selkv"], [128, 64], BF16, "selkv")

    Kml = P.sbuf([96, T], BF16, "Kml")
    Vml = P.sbuf([128, NBK, 128], BF16, "Vml")
    tKml = [P.tok(f"Kml{i}") for i in range(NQ)]
    tVml = [P.tok(f"Vml{i}") for i in range(NQ)]
    t_on1 = P.tok("von1")
    C.MEMSET("pool", Vml[:, :, 64:128], 1.0, [t_on1])
    Kroll, t_Kroll = C.tile([64, 128 + TN], BF16, "Kroll")
    Vroll, t_Vroll = C.tile([128, 5, 128], BF16, "Vroll")
    C.MEMSET("pool", Vroll[:, :, 64:128], 1.0, [t_Vroll])

    F = lambda shape, name, d=F32: C.tile(shape, d, name)
    pos_t, t_pos = F([96, TN], "pos_t", I32)
    posf, t_posf = F([96, TN], "posf")
    a1, t_a1 = F([96, TN], "a1")
    a2, t_a2 = F([96, TN], "a2")
    Ct, t_Ct = F([96, TN], "Ct")
    St, t_St = F([96, TN], "St")
    rsA, t_rsA = F([128, TN], "rsA")
    rs96, t_rs96 = F([96, TN], "rs96")
    rs64, t_rs64 = F([64, TN], "rs64")
    u1, t_u1 = F([96, TN], "u1")
    u2, t_u2 = F([96, TN], "u2")
    xin, t_xin = F([128, 4, TN], "xin", BF16)
    xn, t_xn = F([128, 3, TN], "xn", BF16)
    krb, t_krb = F([32, TN], "krb", BF16)
    xk, t_xk = F([128, TN], "xk", BF16)
    xv, t_xv = F([128, TN], "xv", BF16)
    Q, t_Q = F([96, TN], "Q", BF16)
    Qs, t_Qs = F([64, TN], "Qs", BF16)
    pb = [F([128, TN], f"p{i}", BF16) for i in range(4)]
    rd, t_rd = F([128, TN], "rd")
    rd2, t_rd2 = F([128, TN], "rd2")
    o1, t_o1 = F([64, TN], "o1b", BF16)
    o2, t_o2 = F([64, TN], "o2b", BF16)
    yo = [F([128, TN], f"yo{i}", BF16) for i in range(4)]
    yq, t_yq = u2, t_u2
    qi, t_qi = pos_t, t_pos
    t_yp = G["t_yp"]

    def reduce_angle(a, t_a):
        C.TS("dve", yq[:], a[:], 1.0 / (2 * PI), None, ALU.mult, None, [t_a], [t_yq])
        C.CP("dve", qi[:], yq[:], [t_yq], [t_qi])
        C.CP("dve", yq[:], qi[:], [t_qi], [t_yq])
        C.STT("dve", a[:], yq[:], -2 * PI, a[:], ALU.mult, ALU.add, [t_yq, t_a], [t_a])
        C.TS("dve", yq[:], a[:], PI, None, ALU.is_ge, None, [t_a], [t_yq])
        C.STT("dve", a[:], yq[:], -2 * PI, a[:], ALU.mult, ALU.add, [t_yq, t_a], [t_a])
        C.TS("dve", a[:], a[:], PI, -PI, ALU.min, ALU.max, [t_a], [t_a])

    def rope_tabs(c0, ca, cb):
        C.DMA(pos_t[:], G["pos"][:, c0:c0 + TN], [], [t_pos])
        C.CP("dve", posf[:], pos_t[:], [t_pos], [t_posf])
        C.TS("dve", a1[:], posf[:], cols[:96, 11:12], None, ALU.mult, None, [t_posf, t_cols], [t_a1])
        C.TS("dve", a2[:], posf[:], cols[:96, 11:12], PI / 2, ALU.mult, ALU.add, [t_posf, t_cols], [t_a2])
        reduce_angle(a1, t_a1)
        reduce_angle(a2, t_a2)
        C.ACT(a1[:], a1[:], AF.Sin, [t_a1], [t_a1])
        C.ACT(a2[:], a2[:], AF.Sin, [t_a2], [t_a2])
        C.TS("dve", Ct[:], a2[:], ng[:96, ca:ca + 1], None, ALU.mult, None, [t_a2, t_ng], [t_Ct])
        C.TS("dve", St[:], a1[:], ng[:96, cb:cb + 1], None, ALU.mult, None, [t_a1, t_ng], [t_St])

    def norm_in(f0, nk, gcol0, t, inv_d):
        for k in range(nk):
            C.DMA(xin[:, k, :], gsl(f0 + k * 128, 128, t), [g8tok(f0 + k * 128)], [t_xin])
        C.rstd_bc([(xin[:, k, :], t_xin) for k in range(nk)], TN, inv_d, rsA[:], t_rsA)
        for k in range(nk):
            C.STT("dve", xn[:, k, :], xin[:, k, :], cols[:, gcol0 + k:gcol0 + k + 1], rsA[:], ALU.mult, ALU.mult,
                  [t_xin, t_cols, t_rsA], [t_xn])

    def rope_norm(bA, tA, bB, tB, out_ap, t_o):
        C.rstd_bc([(bA[:96, :], tA)], TN, 1.0 / 96, rs96[:], t_rs96, np_out=96)
        C.TT("dve", u1[:], bA[:96, :], Ct[:], ALU.mult, [tA, t_Ct], [t_u1])
        C.TT("dve", u2[:], bB[:96, :], St[:], ALU.mult, [tB, t_St], [t_u2])
        C.TT("dve", u1[:], u1[:], u2[:], ALU.add, [t_u1, t_u2], [t_u1])
        C.TT("dve", out_ap, u1[:], rs96[:], ALU.mult, [t_u1, t_rs96], [t_o])

    gathers()
    for i in range(NQ):
        c0 = i * TN
        norm_in(1152, 2, 5, i, 1.0 / 256)
        C.DMA(krb[:], gsl(1408, 32, i), [g8tok(1408)], [t_krb])
        bA, tA = C.bank()
        bB, tB = C.bank()
        for (bk, tb, w0, tw0, w1, tw1) in ((bA, tA, wk, t_wk, wkr, t_wkr), (bB, tB, wks, t_wks, wkrs, t_wkrs)):
            C.MM(bk[:96, :], w0[:, 0, :], xn[:, 0, :], True, False, [tw0, t_xn], [tb])
            C.MM(bk[:96, :], w0[:, 1, :], xn[:, 1, :], False, False, [tw0, t_xn], [tb])
            C.MM(bk[:96, :], w1[:, :], krb[:, :], False, True, [tw1, t_krb], [tb])
        rope_tabs(c0, 2, 3)
        rope_norm(bA, tA, bB, tB, Kml[:, c0:c0 + TN], tKml[i])
        bv, tbv = C.bank()
        for b in range(4):
            for k in range(2):
                C.MM(bv[:, b * 64:(b + 1) * 64], xn[:, k, b * 128:(b + 1) * 128], wv[:, k, :], k == 0, k == 1,
                     [t_xn, t_wv], [tbv])
        C.ACT(Vml[:, 4 * i:4 * i + 4, 0:64], bv[:, 0:256].rearrange("p (b d) -> p b d", d=64), AF.Copy,
              [tbv, t_on1], [tVml[i]])

    pre_ring = C.reserve(3)
    Qb = [(Q, t_Q), F([96, TN], "Q1", BF16)]
    Qsb = [(Qs, t_Qs), F([64, TN], "Qs1", BF16)]
    Krb = [(Kroll, t_Kroll), F([64, 128 + TN], "Kroll1", BF16)]
    Vr1, t_Vr1 = F([128, 5, 128], "Vroll1", BF16)
    C.MEMSET("pool", Vr1[:, :, 64:128], 1.0, [t_Vr1])
    Vrb = [(Vroll, t_Vroll), (Vr1, t_Vr1)]
    pi_ = 0
    yi_ = [0]

    def PRE(i):
        par = i % 2
        Qp, t_Qp = Qb[par]
        Qsp, t_Qsp = Qsb[par]
        Kr, t_Kr = Krb[par]
        Vr, t_Vr = Vrb[par]
        c0 = i * TN
        if i > 0:
            Kp, t_Kp = Krb[1 - par]
            Vp, t_Vp = Vrb[1 - par]
            C.CP("dve", Kr[:, 0:128], Kp[:, TN:TN + 128], [t_Kp], [t_Kr])
            C.CP("dve", Vr[:, 0, 0:64], Vp[:, 4, 0:64], [t_Vp], [t_Vr])
        norm_in(768, 3, 2, i, 1.0 / 384)
        bA, tA = C.bank()
        bB, tB = C.bank()
        for (bk, tb, w0, tw0) in ((bA, tA, wuq, t_wuq), (bB, tB, wuqs, t_wuqs)):
            for k in range(3):
                C.MM(bk[:96, :], w0[:, k, :], xn[:, k, :], k == 0, k == 2, [tw0, t_xn], [tb])
        rope_tabs(c0, 0, 1)
        rope_norm(bA, tA, bB, tB, Qp[:], t_Qp)
        for k in range(4):
            C.DMA(xin[:, k, :], gsl(k * 128, 128, i), [g8tok(k * 128)], [t_xin])
        bq, tbq = C.bank()
        for k in range(4):
            C.MM(bq[:64, :], selq[:, k, :], xin[:, k, :], k == 0, k == 3, [t_selq, t_xin], [tbq])
        C.rstd_bc([(bq[:64, :], tbq)], TN, 1.0 / 64, rs64[:], t_rs64, np_out=64)
        C.STT("dve", Qsp[:], bq[:64, :], cols[:64, 0:1], rs64[:], ALU.mult, ALU.mult, [tbq, t_cols, t_rs64], [t_Qsp])
        C.DMA(xk[:], gsl(512, 128, i), [g8tok(512)], [t_xk])
        bk2, tbk2 = C.bank()
        C.MM(bk2[:64, :], selkv[:], xk[:], True, True, [t_selkv, t_xk], [tbk2])
        C.rstd_bc([(bk2[:64, :], tbk2)], TN, 1.0 / 64, rs64[:], t_rs64, np_out=64)
        C.STT("dve", Kr[:, 128:128 + TN], bk2[:64, :], cols[:64, 1:2], rs64[:], ALU.mult, ALU.mult,
              [tbk2, t_cols, t_rs64], [t_Kr])
        C.DMA(xv[:], gsl(640, 128, i), [g8tok(640)], [t_xv])
        bv2, tbv2 = C.bank()
        for b in range(4):
            C.MM(bv2[:, b * 64:(b + 1) * 64], xv[:, b * 128:(b + 1) * 128], selkv[:], True, True, [t_xv, t_selkv], [tbv2])
        C.ACT(Vr[:, 1:5, 0:64], bv2[:, 0:256].rearrange("p (b d) -> p b d", d=64), AF.Copy, [tbv2], [t_Vr])

    def POSTb(i):
        r0 = (i // 4) * D
        lc = (i % 4) * TN
        for m in range(KC):
            by, tby = C.bank()
            C.MM(by[:], wos[:, m * 128:(m + 1) * 128], o2[:], True, False, [t_wos, t_o2], [tby])
            C.MM(by[:], wom[:, m * 128:(m + 1) * 128], o1[:], False, True, [t_wom, t_o1], [tby])
            yk = yi_[0] % len(yo)
            y, t_y = yo[yk]
            yi_[0] += 1
            if m % 2 == 0:
                C.ACT(y[:], by[:], AF.Copy, [tby], [t_y])
            else:
                C.CP("dve", y[:], by[:], [tby], [t_y])
            C.DMA(G["yp"][i % 4][r0 + m * 128:r0 + (m + 1) * 128, :], y[:], [t_y], [t_yst[yk]], eng="sp")

    bgq = []

    t_yst = [P.tok(f"yst{k}") for k in range(len(yo))]

    def reduce_quarter(q):
        CC(C, "ReduceScatter", ALU.add, G2, G["yp"][q], G["y2"][q], t_yst + [G["t_yred"]], G["t_y2"])
        CC(C, "ReduceScatter", ALU.add, G4, G["y2"][q], G["yred"][q], [G["t_y2"]], G["t_yred"])

    def record(fn, *a):
        P.rec = bgq
        C.pre = pre_ring
        fn(*a)
        P.rec = None
        C.pre = None

    record(PRE, 0)
    P.replay(bgq, len(bgq))
    acc, t_acc = accs[0]
    acc2, t_acc2 = accs[1]
    for i in range(NQ):
        par = i % 2
        Qp, t_Qp = Qb[par]
        Qsp, t_Qsp = Qsb[par]
        Kr, t_Kr = Krb[par]
        Vr, t_Vr = Vrb[par]
        if i + 1 < NQ:
            record(PRE, i + 1)
        nkb = 4 * i + 4
        items = []
        for kb in range(nkb):
            items.append((Kml[:, kb * 128:(kb + 1) * 128], [tKml[kb // 4]], Qp, t_Qp, float(96 ** -0.5),
                          masks[:, kb - 4 * i, :] if kb >= 4 * i else None,
                          Vml[:, kb, :], [tVml[kb // 4], t_on1], acc, t_acc, kb == 0, kb == nkb - 1))
        rs_ = [r for r in range(5) if not (i == 0 and r == 0)]
        for n_, r in enumerate(rs_):
            items.append((Kr[:, r * 128:(r + 1) * 128], [t_Kr], Qsp, t_Qsp, 0.125, masks[:, 4 + r, :],
                          Vr[:, r, :], [t_Vr], acc2, t_acc2, n_ == 0, n_ == len(rs_) - 1))
        LA = 2
        pend = []
        nbg = len(bgq)
        done_bg = 0

        def emitA(it, pp):
            C.MM(it[8][:], it[6], pp[0][:], it[10], it[11], it[7] + [pp[1]], [it[9]])

        for n_it, it in enumerate(items):
            bs, tbs = C.bank()
            C.MM(bs[:], it[0], it[2][:], True, True, it[1] + [it[3]], [tbs])
            p, t_p = pb[pi_ % len(pb)]
            pi_ += 1
            C.ACT(p[:], bs[:], AF.Exp, [tbs], [t_p], scale=it[4])
            if it[5] is not None:
                C.TT("dve", p[:], p[:], it[5], ALU.mult, [t_p, t_masks], [t_p])
            pend.append((it, (p, t_p)))
            if len(pend) > LA:
                emitA(*pend.pop(0))
            want = ((n_it + 1) * nbg) // len(items)
            P.replay(bgq, want - done_bg)
            done_bg = want
        while pend:
            emitA(*pend.pop(0))
        P.replay(bgq, len(bgq))
        if i - 1 >= NQ - 4:
            reduce_quarter((i - 1) % 4)
        C.ACT(rd[64:128, :], acc[64:128, :], AF.Ln, [t_acc], [t_rd])
        C.ACT(rd[64:128, :], rd[64:128, :], AF.Exp, [t_rd], [t_rd], scale=-1.0)
        C.TT("dve", o1[:], acc[0:64, :], rd[64:128, :], ALU.mult, [t_acc, t_rd], [t_o1])
        C.ACT(rd2[64:128, :], acc2[64:128, :], AF.Ln, [t_acc2, t_esk], [t_rd2], bias=esk[64:128, :], scale=1.0)
        C.ACT(rd2[64:128, :], rd2[64:128, :], AF.Exp, [t_rd2], [t_rd2], scale=-1.0)
        C.TT("dve", o2[:], acc2[0:64, :], rd2[64:128, :], ALU.mult, [t_acc2, t_rd2], [t_o2])
        record(POSTb, i)
    P.replay(bgq, len(bgq))
    reduce_quarter((NQ - 1) % 4)
    C.banks = C.banks + pre_ring
    C.banks = C.banks + accs


def build_fused(nlayers=4):
    nc = bass.Bass("TRN2", target_bir_lowering=False)
    din = lambda name, shape, d=F32: nc.dram_tensor(name, list(shape), d, kind="ExternalInput").ap()
    dsc = lambda name, shape, d=F32: nc.dram_tensor(name, list(shape), d).ap()
    G = {}
    xT = din("xT", [D, NT])
    G["cbc"] = din("cbc", [128, D])
    G["ada"] = din("ada", [4, 48, 128, D])
    G["adab"] = din("adab", [4, 128, 48])
    G["g1"] = din("g1", [4, 128, KC])
    G["g2"] = din("g2", [4, 128, KC])
    cp_w_in = din("cp_w_in", [2, D, 2048])
    G["convw"] = din("convw", [2, 128, 4, 3])
    G["poolw"] = din("poolw", [2, 128, 4, 128])
    G["pscale"] = din("pscale", [2, 128, 4])
    cp_w_out = din("cp_w_out", [2, D, D])
    at_w_in = din("at_w_in", [2, D, NZ_ATT])
    G["corr"] = din("corr", [128, 4, 16])
    G["hsel"] = din("hsel", [128, 8])
    G["selq"] = din("selq", [128, 4, 64], BF16)
    G["selkv"] = din("selkv", [128, 64], BF16)
    G["wuq"] = din("wuq", [2, 384, 96])
    G["wuqs"] = din("wuqs", [2, 384, 96])
    G["wk"] = din("wk", [2, 288, 96])
    G["wks"] = din("wks", [2, 288, 96])
    G["wv"] = din("wv", [2, 256, 64])
    G["wo"] = din("wo", [2, 128, D])
    G["cols"] = din("cols", [2, 128, 16])
    G["masks"] = din("masks", [9, 128, TN], BF16)
    G["pos"] = din("pos", [96, S], I32)
    G["wr"] = din("wr", [4, 128, KC, 36])
    G["br"] = din("br", [4, 128, 36])
    G["wg"] = din("wg", [4, NE, D, FF])
    G["wu"] = din("wu", [4, NE, D, FF])
    G["wd"] = din("wd", [4, NE, FF, D])
    G["ident"] = din("ident", [128, 128])
    xo = nc.dram_tensor("xo", [D, NT], F32, kind="ExternalOutput").ap()
    G["zs"] = dsc("zs", [2048, NT])
    G["ys"] = dsc("ys", [D, NT])
    G["hsrc"] = dsc("hsrc", [128, 192])
    G["h4"] = dsc("h4", [512, 192])
    G["h8"] = dsc("h8", [1024, 192])
    G["gsrc"] = [dsc(f"gsrc{j}", [r, NT], BF16) for j, r in enumerate(ATT_ROWS)]
    G["g4"] = [dsc(f"g4_{j}", [4 * r, NT], BF16) for j, r in enumerate(ATT_ROWS)]
    G["g8"] = [dsc(f"g8_{j}", [8 * r, NT], BF16) for j, r in enumerate(ATT_ROWS)]
    G["yp"] = [dsc(f"yp{q}", [8 * D, TN], BF16) for q in range(NTILE)]
    G["y2"] = [dsc(f"y2_{q}", [4 * D, TN], BF16) for q in range(NTILE)]
    G["yred"] = [dsc(f"yred{q}", [D, TN], BF16) for q in range(NTILE)]
    with ExitStack() as es:
        C = Ctx(nc, es)
        P = C.P
        for n in ("zs", "ys", "hsrc", "h4", "h8", "gsrc", "g4", "g8", "yp", "y2", "yred"):
            G["t_" + n] = P.tok(n)
        G["t_ypq"] = [P.tok(f"ypq{q}") for q in range(NTILE)]
        x = P.sbuf([128, KC, NT], F32, "x")
        tx = [[P.tok(f"x{m}_{i}") for i in range(NTILE)] for m in range(KC)]
        G["x"], G["tx"] = x, tx
        for m in range(KC):
            C.DMA(x[:, m, :], xT[m * 128:(m + 1) * 128, :], [], tx[m])
        for l in range(nlayers):
            i = l // 2
            if l % 2 == 0:
                stage(C, lambda: st_normproj(C, G, l, cp_w_in[i], 2048, G["zs"], G["t_zs"], F32, True))
                stage(C, lambda: st_evenmix(C, G, i))
                stage(C, lambda: st_proj(C, G, l, cp_w_out[i]))
            else:
                stage(C, lambda: st_normproj(C, G, l, at_w_in[i], NZ_ATT, G["gsrc"], G["t_gsrc"], BF16, False))
                stage(C, lambda: st_attn(C, G, i))
                stage(C, lambda: st_proj(C, G, l, None))
            stage(C, lambda: st_moe(C, G, l))
        t_out = P.tok("xo")
        for m in range(KC):
            C.DMA(xo[m * 128:(m + 1) * 128, :], x[:, m, :], tx[m], [t_out], eng="sp")
        P.wait_all("sp", [t_out])
        P.emit()
    return nc


def _cols(v, n=KC):
    return np.ascontiguousarray(np.asarray(v, np.float32).reshape(n, 128).T)


def _attn_masks():
    k = np.arange(128)[:, None]
    q = np.arange(TN)[None, :]
    m = np.zeros((9, 128, TN), np.float32)
    for d in range(4):
        m[d] = (q >= d * 128 + k)
    for r in range(5):
        kp = (r - 1) * 128 + k
        m[4 + r] = (q >= kp) & (q < kp + 128)
    return m.astype(ml_dtypes.bfloat16)


_PROG = {}
NL = 4


def kernel(**inputs):
    inp = {k: np.asarray(v) for k, v in inputs.items()}
    nl = NL
    if ("fused", nl) not in _PROG:
        _PROG[("fused", nl)] = build_fused(nl)
    nc = _PROG[("fused", nl)]
    f32 = np.float32
    x = inp["x"][0]
    ada = np.ascontiguousarray(inp["ada_w"].transpose(0, 2, 1).reshape(4, 48, 128, D))
    adab = np.ascontiguousarray(inp["ada_b"].reshape(4, 48, 128).transpose(0, 2, 1))
    com = dict(
        cbc=np.ascontiguousarray(np.broadcast_to(inp["c"].reshape(1, D), (128, D))),
        ada=ada, adab=adab,
        g1=np.ascontiguousarray(inp["norm1_g"].reshape(4, KC, 128).transpose(0, 2, 1)),
        g2=np.ascontiguousarray(inp["norm2_g"].reshape(4, KC, 128).transpose(0, 2, 1)),
        cp_w_in=np.ascontiguousarray(inp["cp_w_in"]),
        convw=np.ascontiguousarray(inp["conv_w"].reshape(2, 3, 4, 128).transpose(0, 3, 2, 1)),
        poolw=np.ascontiguousarray(inp["pool_w"].transpose(0, 2, 1, 3)),
        pscale=np.ascontiguousarray(inp["pool_scale"].reshape(2, 4, 128).transpose(0, 2, 1)),
        cp_w_out=np.ascontiguousarray(inp["cp_w_out"]),
        at_w_in=np.ascontiguousarray(inp["at_w_in"]),
        masks=_attn_masks(),
        pos=np.ascontiguousarray(np.broadcast_to(inp["positions"].reshape(1, S).astype(np.int32), (96, S))),
        wr=np.ascontiguousarray(np.concatenate([inp["moe_w_group"], inp["moe_w_expert"]], axis=2)
                                .reshape(4, KC, 128, 36).transpose(0, 2, 1, 3)),
        br=np.ascontiguousarray(np.broadcast_to(
            np.concatenate([inp["moe_b_group"], inp["moe_b_expert"]], axis=1)[:, None, :], (4, 128, 36))),
        wg=np.ascontiguousarray(inp["moe_w_gate"]), wu=np.ascontiguousarray(inp["moe_w_up"]),
        wd=np.ascontiguousarray(inp["moe_w_down"]),
        ident=np.eye(128, dtype=f32),
    )
    inv = np.power(f32(10000.0), -np.arange(16, dtype=f32) / 16).astype(f32)
    maps = []
    for c in range(NCORE):
        h = c
        kvh = h // 4
        corr = np.zeros((128, 4, 16), f32)
        for g, wd_ in enumerate(POOL_W):
            t = np.arange(16, dtype=f32)
            corr[:, g, :] = (1.0 / np.minimum(t + 1.0, float(wd_))) if c == 0 else (1.0 / wd_)
        hsel = np.zeros((128, 8), f32)
        if c > 0:
            hsel[:, c - 1] = 1.0
        selq = np.zeros((128, 4, 64), f32)
        for j in range(64):
            f = h * 64 + j
            selq[f % 128, f // 128, j] = 1.0
        selkv = np.zeros((128, 64), f32)
        for j in range(64):
            selkv[kvh * 64 + j, j] = 1.0
        wuq = np.ascontiguousarray(inp["mla_w_uq"][:, :, h * 96:(h + 1) * 96])
        wkv = inp["mla_w_ukv"][:, :, h * 128:(h + 1) * 128]
        wk = np.zeros((2, 288, 96), f32)
        wk[:, 0:256, 0:64] = wkv[:, :, 0:64]
        wk[:, 256:288, 64:96] = np.eye(32, dtype=f32)
        cols = np.zeros((2, 128, 16), f32)
        for i in range(2):
            cols[i, 0:64, 0] = inp["swa_q_g"][i]
            cols[i, 0:64, 1] = inp["swa_k_g"][i]
            cols[i, :, 2:5] = _cols(inp["mla_q_norm_g"][i], 3)
            cols[i, :, 5:7] = _cols(inp["mla_kv_norm_g"][i], 2)
            cols[i, 0:96, 7] = inp["mla_q_g"][i]
            cols[i, 0:96, 8] = inp["mla_q_g"][i][PERM96]
            cols[i, 0:96, 9] = inp["mla_k_g"][i]
            cols[i, 0:96, 10] = inp["mla_k_g"][i][PERM96]
            cols[i, 64:80, 11] = inv
            cols[i, 80:96, 11] = inv
            cols[i, 64:80, 12] = -1.0
            cols[i, 80:96, 12] = 1.0
            cols[i, :, 13] = inp["swa_sinks"][i][h]
        wo = np.ascontiguousarray(np.concatenate(
            [inp["at_w_out"][:, h * 64:(h + 1) * 64, :], inp["at_w_out"][:, 512 + h * 64:512 + (h + 1) * 64, :]], axis=1))
        maps.append(dict(
            com, xT=np.ascontiguousarray(x[c * NT:(c + 1) * NT].T), corr=corr, hsel=hsel,
            selq=selq.astype(ml_dtypes.bfloat16), selkv=selkv.astype(ml_dtypes.bfloat16),
            wuq=wuq, wuqs=np.ascontiguousarray(wuq[:, :, PERM96]), wk=wk, wks=np.ascontiguousarray(wk[:, :, PERM96]),
            wv=np.ascontiguousarray(wkv[:, :, 64:128]), wo=wo, cols=cols))
    res = run_bass_kernel_spmd(nc, maps, core_ids=list(range(NCORE)))
    out = np.concatenate([res.results[c]["xo"].T for c in range(NCORE)], axis=0)[None]
    return np.ascontiguousarray(out.astype(np.float32))
```

```python
import numpy as np
import ml_dtypes
from contextlib import ExitStack
import concourse.bass as bass
import concourse.mybir as mybir
from concourse.bass_utils import run_bass_kernel_spmd

F32 = mybir.dt.float32
BF16 = mybir.dt.bfloat16
I32 = mybir.dt.int32
ALU = mybir.AluOpType
AF = mybir.ActivationFunctionType
AX = mybir.AxisListType

NCORE = 8
S = 16384
D = 1024
NT = S // NCORE
TN = 512
NTILE = NT // TN
KC = D // 128
EPS = 1e-6
NE = 32
FF = 256


class Tok:
    __slots__ = ("name", "writer", "readers", "dsem", "dcount")

    def __init__(self, name=""):
        self.name = name
        self.writer = None
        self.readers = {}
        self.dsem = None
        self.dcount = 0


class Eng:
    def __init__(self, name, sem):
        self.name = name
        self.sem = sem
        self.count = 0
        self.ops = []
        self.waited = {}


class Prog:
    def __init__(self, nc, es):
        self.nc = nc
        self.es = es
        self.engs = {}
        for n in ("pe", "act", "dve", "pool", "sp"):
            sem = es.enter_context(nc.semaphore("s_" + n))
            self.engs[n] = Eng(n, sem)
        self.ntok = 0
        self.nsb = 0
        self.es_outer = es
        self.free_dsems = []
        self.stage_toks = []
        self.all_dtoks = []
        self.in_stage = False
        self.rec = None

    def sbuf(self, shape, dt, name=None):
        self.nsb += 1
        return self.es.enter_context(self.nc.sbuf_tensor(f"S{self.nsb}_{name or ''}", list(shape), dt))

    def psum(self, shape, dt=F32, name=None):
        self.nsb += 1
        return self.es.enter_context(self.nc.psum_tensor(name or f"ps{self.nsb}", list(shape), dt))

    def tok(self, name=""):
        self.ntok += 1
        return Tok(name or f"t{self.ntok}")

    def _dsem(self, t):
        if t.dsem is None:
            if self.free_dsems:
                sem, cnt = self.free_dsems.pop()
            else:
                sem = self.es_outer.enter_context(self.nc.semaphore(f"d{self.ntok}"))
                self.ntok += 1
                cnt = 0
            t.dsem = sem
            t.dcount = cnt
            self.all_dtoks.append(t)
            if self.in_stage:
                self.stage_toks.append(t)
        return t.dsem

    def barrier(self):
        for E in self.engs.values():
            waits = []
            for E2 in self.engs.values():
                if E2 is not E and E2.count > E.waited.get(id(E2.sem), 0):
                    E.waited[id(E2.sem)] = E2.count
                    waits.append((E2.sem, E2.count))
            for t in self.all_dtoks:
                if t.dcount > E.waited.get(id(t.dsem), 0):
                    E.waited[id(t.dsem)] = t.dcount
                    waits.append((t.dsem, t.dcount))
            E.ops.append((waits, None, []))

    def end_stage(self):
        self.barrier()
        self.emit()
        for t in self.stage_toks:
            self.free_dsems.append((t.dsem, t.dcount))
            self.all_dtoks.remove(t)
            t.dsem = None
        self.stage_toks = []

    def _collect(self, E, reads, writes, skip_sid=None):
        need = {}

        def add(p, same_ok):
            if p is None:
                return
            sid, sem, val = p
            if sid == skip_sid:
                return
            if sid == id(E.sem) and (not same_ok or E.name == "pe"):
                return
            if need.get(sid, (None, -1))[1] < val:
                need[sid] = (sem, val)

        for t in reads:
            add(t.writer, True)
        for t in writes:
            add(t.writer, True)
            for sid, (sem, val) in t.readers.items():
                add((sid, sem, val), False)
        waits = []
        for sid, (sem, val) in need.items():
            if E.waited.get(sid, 0) < val:
                E.waited[sid] = val
                waits.append((sem, val))
        return waits

    def op(self, eng, fn, reads=(), writes=(), inc=True):
        if self.rec is not None:
            self.rec.append(("op", (eng, fn, tuple(reads), tuple(writes), inc)))
            return
        E = self.engs[eng]
        waits = self._collect(E, reads, writes)
        if inc:
            E.count += 1
            cnt = E.count
            incs = [(E.sem, 1)]
        else:
            cnt = E.count + 1
            incs = []
        me = (id(E.sem), E.sem, cnt)
        E.ops.append((waits, fn, incs))
        for t in reads:
            t.readers[id(E.sem)] = (E.sem, cnt)
        for t in writes:
            t.writer = me
            t.readers = {}

    def dma(self, eng, fn, reads=(), writes=(), inc=16, serial=False):
        if self.rec is not None:
            self.rec.append(("dma", (eng, fn, tuple(reads), tuple(writes), inc, serial)))
            return
        E = self.engs[eng]
        t0 = writes[0]
        sem = self._dsem(t0)
        waits = self._collect(E, reads, writes, skip_sid=None if serial else id(sem))
        t0.dcount += inc
        me = (id(sem), sem, t0.dcount)
        E.ops.append((waits, fn, [(sem, inc)]))
        for t in reads:
            t.readers[id(sem)] = (sem, t0.dcount)
        for t in writes:
            t.writer = me
            t.readers = {}

    def replay(self, q, n):
        assert self.rec is None
        while n > 0 and q:
            kind, args = q.pop(0)
            if kind == "op":
                self.op(*args)
            else:
                self.dma(*args)
            n -= 1

    def wait_all(self, eng, toks):
        E = self.engs[eng]
        waits = self._collect(E, toks, ())
        E.ops.append((waits, None, []))

    def emit(self):
        nc = self.nc
        with nc.Block() as block:
            def run(E, h):
                for waits, fn, incs in E.ops:
                    for sem, val in waits:
                        h.wait_ge(sem, val)
                    if fn is None:
                        continue
                    r = fn(h)
                    for sem, v in incs:
                        r.then_inc(sem, v)

            @block.tensor
            def _(h):
                run(self.engs["pe"], h)

            @block.scalar
            def _(h):
                run(self.engs["act"], h)

            @block.vector
            def _(h):
                run(self.engs["dve"], h)

            @block.gpsimd
            def _(h):
                run(self.engs["pool"], h)

            @block.sync
            def _(h):
                run(self.engs["sp"], h)
        for E in self.engs.values():
            E.ops = []


class Ctx:
    def __init__(self, nc, es):
        self.nc = nc
        self.P = Prog(nc, es)
        P = self.P
        self.banks = [(P.psum([128, 512], F32, name=f"bank{i}"), P.tok(f"bank{i}")) for i in range(8)]
        self.bi = 0
        self.pre = None
        self.pbi = 0
        self.sq_eng = "act"
        self.ones = P.sbuf([128, 128], BF16, "ones_bf")
        self.t_ones = P.tok("ones")
        self.epsc = P.sbuf([128, 1], F32, "epsc")
        self.t_eps = P.tok("eps")
        P.op("pool", lambda h: h.memset(self.ones[:], 1.0), writes=[self.t_ones])
        P.op("pool", lambda h: h.memset(self.epsc[:], EPS), writes=[self.t_eps])
        self.sq = [(P.sbuf([128, 512], BF16, f"sq{i}"), P.tok(f"sq{i}")) for i in range(2)]
        self.sqi = 0
        self.dq = 0

    def bank(self):
        if self.pre is not None:
            b = self.pre[self.pbi % len(self.pre)]
            self.pbi += 1
            return b
        b = self.banks[self.bi % len(self.banks)]
        self.bi += 1
        return b

    def reserve(self, n):
        r = self.banks[-n:]
        self.banks = self.banks[:-n]
        return r

    def tile(self, shape, dt, name=None):
        t = self.P.sbuf(shape, dt, name)
        return t, self.P.tok(name or "tile")

    def ACT(self, out, in_, func, r, w, **kw):
        self.P.op("act", lambda h: h.activation(out=out, in_=in_, func=func, **kw), r, w)

    def MM(self, out, lhsT, rhs, start, stop, r, w, lazy=False):
        self.P.op("pe", lambda h: h.matmul(out, lhsT=lhsT, rhs=rhs, start=start, stop=stop), r, w,
                  inc=(stop or not lazy))

    def TT(self, eng, out, in0, in1, op, r, w):
        self.P.op(eng, lambda h: h.tensor_tensor(out=out, in0=in0, in1=in1, op=op), r, w)

    def TS(self, eng, out, in0, s1, s2, op0, op1, r, w):
        if s2 is None:
            self.P.op(eng, lambda h: h.tensor_scalar(out=out, in0=in0, scalar1=s1, scalar2=None, op0=op0), r, w)
        else:
            self.P.op(eng, lambda h: h.tensor_scalar(out=out, in0=in0, scalar1=s1, scalar2=s2, op0=op0, op1=op1), r, w)

    def STT(self, eng, out, in0, scalar, in1, op0, op1, r, w):
        self.P.op(eng, lambda h: h.scalar_tensor_tensor(out=out, in0=in0, scalar=scalar, in1=in1, op0=op0, op1=op1), r, w)

    def CP(self, eng, out, in_, r, w):
        self.P.op(eng, lambda h: h.tensor_copy(out=out, in_=in_), r, w)

    def RED(self, eng, out, in_, op, r, w):
        self.P.op(eng, lambda h: h.tensor_reduce(out=out, in_=in_, axis=AX.X, op=op), r, w)

    def RECIP(self, out, in_, r, w):
        self.P.op("dve", lambda h: h.reciprocal(out=out, in_=in_), r, w)

    def MEMSET(self, eng, ap, val, w):
        self.P.op(eng, lambda h: h.memset(ap, val), (), w)

    def DMA(self, out, in_, r, w, eng=None):
        if eng is None:
            eng = "sp"
            self.dq += 1
        self.P.dma(eng, lambda h: h.dma_start(out=out, in_=in_), r, w)

    def load(self, dram_ap, shape, dt, name, eng=None):
        t, tk = self.tile(shape, dt, name)
        self.DMA(t[:], dram_ap, [], [tk], eng=eng)
        return t, tk

    def rstd_bc(self, chunks, n, inv_d, out, t_out, ones_ap=None, np_out=128, sq_eng=None):
        bank, tb = self.bank()
        last = len(chunks) - 1
        for k, (ap, tk) in enumerate(chunks):
            sq, tsq = self.sq[self.sqi % 2]
            self.sqi += 1
            p = ap.shape[0]
            if sq_eng == "dve":
                self.TT("dve", sq[:p, :n], ap, ap, ALU.mult, [tk], [tsq])
            else:
                self.ACT(sq[:p, :n], ap, AF.Square, [tk], [tsq])
            lhs = self.ones[:p, :np_out] if ones_ap is None else ones_ap
            self.MM(bank[:np_out, :n], lhs, sq[:p, :n], k == 0, k == last, [tsq, self.t_ones], [tb])
        self.ACT(out, bank[:np_out, :n], AF.Ln, [tb, self.t_eps], [t_out], scale=inv_d, bias=self.epsc[:np_out, :])
        self.ACT(out, out, AF.Exp, [t_out], [t_out], scale=-0.5)

    def mod_cols(self, cbc_d, adaT_d, adab_d, nvec, scratch):
        cb, t_cb = scratch[0]
        self.DMA(cb[:], cbc_d, [], [t_cb])
        ca, t_ca = scratch[1]
        self.ACT(ca[:], cb[:], AF.Silu, [t_cb], [t_ca])
        ab, t_ab = self.load(adab_d, [128, nvec * KC], F32, "adab")
        mod, t_mod = self.tile([128, nvec * KC], F32, "mod")
        abuf = scratch[2:4]
        prod, t_prod = scratch[4]
        for j in range(nvec * KC):
            a, t_a = abuf[j % 2]
            self.DMA(a[:], adaT_d[j], [], [t_a])
            self.TT("dve", prod[:], a[:], ca[:], ALU.mult, [t_a, t_ca], [t_prod])
            self.RED("dve", mod[:, j:j + 1], prod[:], ALU.add, [t_prod], [t_mod])
        self.TT("dve", mod[:], mod[:], ab[:], ALU.add, [t_mod, t_ab], [t_mod])
        return mod, t_mod


def emit_moe(C, x, tx, x0, mod, t_mod, vb, g2_d, wr_d, br_d, wg_d, wu_d, wd_d, ident_d, stg):
    P = C.P
    g2, t_g2 = C.load(g2_d, [128, KC], F32, "g2")
    wr, t_wr = C.load(wr_d, [128, KC, 36], F32, "wr")
    br, t_br = C.load(br_d, [128, 36], F32, "br")
    ident, t_id = C.load(ident_d, [128, 128], F32, "ident")
    sel, t_sel = C.tile([32, NE, 128], BF16, "gsel")
    C.CP("dve", sel[:], ident[0:32, 0:NE].unsqueeze(2).to_broadcast([32, NE, 128]), [t_id], [t_sel])
    a2, t_a2 = C.tile([128, KC], F32, "a2")
    C.TS("dve", a2[:], mod[:, vb + 8:vb + 16], 1.0, None, ALU.add, None, [t_mod], [t_a2])
    C.TT("dve", a2[:], a2[:], g2[:], ALU.mult, [t_a2, t_g2], [t_a2])
    sh2 = mod[:, vb:vb + 8]
    gt2 = mod[:, vb + 16:vb + 24]

    h2 = P.sbuf([128, KC, NT], BF16, "h2")
    th2 = [[P.tok(f"h2_{k}_{i}") for i in range(NTILE)] for k in range(KC)]
    NB = NT // 128
    logits, t_lg = C.tile([128, NB, 36], F32, "logits")
    rs, t_rs = C.tile([128, TN], F32, "rs2")
    tmpb = [C.tile([128, TN], F32, f"n2tmp{i}") for i in range(2)]
    hfb = [C.tile([128, TN], F32, f"n2hf{i}") for i in range(2)]

    for i in range(NTILE):
        c0 = x0 + i * TN
        C.rstd_bc([(x[:, k, c0:c0 + TN], tx[k][i]) for k in range(KC)], TN, 1.0 / D, rs[:], t_rs)
        bls = [C.bank() for _ in range(4)]
        for k in range(KC):
            tmp, t_tmp = tmpb[k % 2]
            hf, t_hf = hfb[k % 2]
            C.STT("dve", tmp[:], x[:, k, c0:c0 + TN], a2[:, k:k + 1], rs[:], ALU.mult, ALU.mult,
                  [tx[k][i], t_a2, t_rs], [t_tmp])
            C.ACT(hf[:], tmp[:], AF.Identity, [t_tmp, t_mod], [t_hf], bias=sh2[:, k:k + 1], scale=1.0)
            for b in range(4):
                C.MM(bls[b][0][:, 0:36], hf[:, b * 128:(b + 1) * 128], wr[:, k, :], k == 0, k == KC - 1,
                     [t_hf, t_wr], [bls[b][1]])
            C.CP("pool", h2[:, k, i * TN:(i + 1) * TN], hf[:], [t_hf], [th2[k][i]])
        for b in range(4):
            C.TT("dve", logits[:, i * 4 + b, :], bls[b][0][:, 0:36], br[:], ALU.add, [bls[b][1], t_br], [t_lg])

    def T(shape, name):
        return C.tile(shape, F32, name)

    gl = logits[:, :, 0:4]
    el = logits[:, :, 4:36].rearrange("p b (g i) -> p b g i", i=8)
    gmax, t_gmax = T([128, NB], "gmax")
    C.RED("dve", gmax[:], gl, ALU.max, [t_lg], [t_gmax])
    ohg, t_ohg = T([128, NB, 4], "ohg")
    C.TT("dve", ohg[:], gl, gmax[:].unsqueeze(2).to_broadcast([128, NB, 4]), ALU.is_equal, [t_lg, t_gmax], [t_ohg])
    gex, t_gex = T([128, NB, 4], "gex")
    C.TT("dve", gex[:], gl, gmax[:].unsqueeze(2).to_broadcast([128, NB, 4]), ALU.subtract, [t_lg, t_gmax], [t_gex])
    C.ACT(gex[:], gex[:], AF.Exp, [t_gex], [t_gex])
    gw, t_gw = T([128, NB], "gw")
    C.RED("dve", gw[:], gex[:], ALU.add, [t_gex], [t_gw])
    C.RECIP(gw[:], gw[:], [t_gw], [t_gw])
    tmp4, t_tmp4 = T([128, NB, 4, 8], "tmp4")
    C.TT("dve", tmp4[:], el, ohg[:].unsqueeze(3).to_broadcast([128, NB, 4, 8]), ALU.mult, [t_lg, t_ohg], [t_tmp4])
    sel8, t_sel8 = T([128, NB, 8], "sel8")
    C.RED("dve", sel8[:], tmp4[:].rearrange("p b g i -> p b i g"), ALU.add, [t_tmp4], [t_sel8])
    m1, t_m1 = T([128, NB], "m1")
    C.RED("dve", m1[:], sel8[:], ALU.max, [t_sel8], [t_m1])
    oh1, t_oh1 = T([128, NB, 8], "oh1")
    C.TT("dve", oh1[:], sel8[:], m1[:].unsqueeze(2).to_broadcast([128, NB, 8]), ALU.is_equal, [t_sel8, t_m1], [t_oh1])
    sel2, t_sel2 = T([128, NB, 8], "sel2")
    C.STT("dve", sel2[:], oh1[:], -1e30, sel8[:], ALU.mult, ALU.add, [t_oh1, t_sel8], [t_sel2])
    m2, t_m2 = T([128, NB], "m2")
    C.RED("dve", m2[:], sel2[:], ALU.max, [t_sel2], [t_m2])
    oh2, t_oh2 = T([128, NB, 8], "oh2")
    C.TT("dve", oh2[:], sel2[:], m2[:].unsqueeze(2).to_broadcast([128, NB, 8]), ALU.is_equal, [t_sel2, t_m2], [t_oh2])
    rr, t_rr = T([128, NB], "rr")
    C.TT("dve", rr[:], m2[:], m1[:], ALU.subtract, [t_m1, t_m2], [t_rr])
    C.ACT(rr[:], rr[:], AF.Exp, [t_rr], [t_rr])
    w1, t_w1 = T([128, NB], "w1")
    C.TS("dve", w1[:], rr[:], 1.0, None, ALU.add, None, [t_rr], [t_w1])
    C.RECIP(w1[:], w1[:], [t_w1], [t_w1])
    w2, t_w2 = T([128, NB], "w2")
    C.TT("dve", w2[:], rr[:], w1[:], ALU.mult, [t_rr, t_w1], [t_w2])
    C.TT("dve", w1[:], w1[:], gw[:], ALU.mult, [t_w1, t_gw], [t_w1])
    C.TT("dve", w2[:], w2[:], gw[:], ALU.mult, [t_w2, t_gw], [t_w2])
    C.TT("dve", oh1[:], oh1[:], w1[:].unsqueeze(2).to_broadcast([128, NB, 8]), ALU.mult, [t_oh1, t_w1], [t_oh1])
    C.TT("dve", oh2[:], oh2[:], w2[:].unsqueeze(2).to_broadcast([128, NB, 8]), ALU.mult, [t_oh2, t_w2], [t_oh2])
    C.TT("dve", oh1[:], oh1[:], oh2[:], ALU.add, [t_oh1, t_oh2], [t_oh1])
    gates, t_gates = T([128, NB, 4, 8], "gates")
    C.TT("dve", gates[:], ohg[:].unsqueeze(3).to_broadcast([128, NB, 4, 8]),
         oh1[:].unsqueeze(2).to_broadcast([128, NB, 4, 8]), ALU.mult, [t_ohg, t_oh1], [t_gates])

    GT, t_GT = C.tile([32, NT], F32, "GT")
    GTh, t_GTh = C.tile([32, NT], BF16, "GTh")
    GTl, t_GTl = C.tile([32, NT], BF16, "GTl")
    for i in range(NTILE):
        bk, tb = C.bank()
        for b in range(4):
            C.MM(bk[0:32, b * 128:(b + 1) * 128], gates[:, i * 4 + b, :, :].rearrange("p g i -> p (g i)"), ident[:],
                 True, True, [t_gates, t_id], [tb])
        C.ACT(GT[:, i * TN:(i + 1) * TN], bk[0:32, :], AF.Copy, [tb], [t_GT])
        C.CP("dve", GTh[:, i * TN:(i + 1) * TN], GT[:, i * TN:(i + 1) * TN], [t_GT], [t_GTh])
        C.TT("dve", GTl[:, i * TN:(i + 1) * TN], GT[:, i * TN:(i + 1) * TN], GTh[:, i * TN:(i + 1) * TN], ALU.subtract,
             [t_GT, t_GTh], [t_GTl])

    wb = [[C.tile([128, 2048], BF16, f"wb{j}_{s}") for s in range(2)] for j in range(3)]
    sgb = tmpb
    t1b = hfb
    acb = [[C.tile([128, TN], BF16, f"actp{c}_{i}") for i in range(2)] for c in range(2)]

    def load_w(e):
        s = e % 2
        srcs = (wg_d[e].rearrange("(k p) f -> p k f", p=128), wu_d[e].rearrange("(k p) f -> p k f", p=128),
                wd_d[e].rearrange("(k p) f -> p k f", p=128))
        shp = ((KC, FF), (KC, FF), (2, D))
        for j in range(3):
            w, t_w = wb[j][s]
            kk = shp[j][0] // 2
            for hh in range(2):
                st, t_st = stg[j][hh]
                C.DMA(st[:].rearrange("p (k f) -> p k f", k=kk), srcs[j][:, hh * kk:(hh + 1) * kk, :], [], [t_st])
                C.CP("pool", w[:, hh * 1024:(hh + 1) * 1024], st[:], [t_st], [t_w])

    units = [(e, i) for e in range(NE) for i in range(NTILE)]
    state = {}

    def GU(u):
        e, i = units[u]
        s = e % 2
        wgb, t_wgb = wb[0][s]
        wub, t_wub = wb[1][s]
        wgv = wgb[:].rearrange("p (k f) -> p k f", k=KC)
        wuv = wub[:].rearrange("p (k f) -> p k f", k=KC)
        bg, tbg = C.bank()
        C.MM(bg[:], sel[:, e, :], GTh[:, i * TN:(i + 1) * TN], True, False, [t_sel, t_GTh], [tbg], lazy=True)
        C.MM(bg[:], sel[:, e, :], GTl[:, i * TN:(i + 1) * TN], False, True, [t_sel, t_GTl], [tbg], lazy=True)
        acts = []
        for c in range(2):
            bga, tbga = C.bank()
            bup, tbup = C.bank()
            for k in range(KC):
                C.MM(bga[:], wgv[:, k, c * 128:(c + 1) * 128], h2[:, k, i * TN:(i + 1) * TN], k == 0, k == KC - 1,
                     [t_wgb, th2[k][i]], [tbga], lazy=True)
            for k in range(KC):
                C.MM(bup[:], wuv[:, k, c * 128:(c + 1) * 128], h2[:, k, i * TN:(i + 1) * TN], k == 0, k == KC - 1,
                     [t_wub, th2[k][i]], [tbup], lazy=True)
            sg, t_sg = sgb[c]
            t1, t_t1 = t1b[c]
            ac, t_ac = acb[c][u % 2]
            C.ACT(sg[:], bga[:], AF.Silu, [tbga], [t_sg])
            C.TT("dve", t1[:], sg[:], bg[:], ALU.mult, [t_sg, tbg], [t_t1])
            C.TT("dve", ac[:], t1[:], bup[:], ALU.mult, [t_t1, tbup], [t_ac])
            acts.append((ac, t_ac))
        state[u] = acts

    def DOWN(u):
        e, i = units[u]
        s = e % 2
        wdb, t_wdb = wb[2][s]
        wdv = wdb[:].rearrange("p (k f) -> p k f", k=2)
        acts = state.pop(u)
        c0 = x0 + i * TN
        for m in range(KC):
            bd, tbd = C.bank()
            for c in range(2):
                C.MM(bd[:], wdv[:, c, m * 128:(m + 1) * 128], acts[c][0][:], c == 0, c == 1,
                     [t_wdb, acts[c][1]], [tbd], lazy=True)
            C.STT("dve", x[:, m, c0:c0 + TN], bd[:], gt2[:, m:m + 1], x[:, m, c0:c0 + TN], ALU.mult, ALU.add,
                  [tbd, t_mod, tx[m][i]], [tx[m][i]])

    load_w(0)
    for u in range(len(units)):
        e, i = units[u]
        if i == 0 and e + 1 < NE:
            load_w(e + 1)
        if u == 0:
            GU(0)
        if u + 1 < len(units):
            GU(u + 1)
        DOWN(u)


POOL_W = (2, 4, 8, 16)
PI = float(np.pi)
PERM96 = list(range(64)) + list(range(80, 96)) + list(range(64, 80))
G4 = [[0, 1, 2, 3], [4, 5, 6, 7]]
G2 = [[0, 4], [1, 5], [2, 6], [3, 7]]
NZ_ATT = 1440
ATT_ROWS = [128] * 11 + [32]


def stage(C, fn):
    P = C.P
    with ExitStack() as es_s:
        P.es = es_s
        P.in_stage = True
        fn()
        P.end_stage()
    P.es = P.es_outer
    P.in_stage = False


def CC(C, kind, op, groups, src_ap, dst_ap, r_toks, w_tok):
    C.P.dma("pool", lambda h: h.collective_compute(kind, op, replica_groups=groups, ins=[src_ap], outs=[dst_ap]),
            r_toks, [w_tok], inc=1, serial=True)


def st_normproj(C, G, l, w_d, nout, dst_d, t_dst, dst_dt, halo):
    P = C.P
    x, tx = G["x"], G["tx"]
    nj = (nout + 127) // 128
    scr = [C.tile([128, D], F32, f"scr{i}") for i in range(5)]
    mod, t_mod = C.mod_cols(G["cbc"], G["ada"][l][0:16], G["adab"][l][:, 0:16], 2, scr)
    g1s, t_g1 = C.load(G["g1"][l], [128, KC], F32, "g1")
    a1, t_a1 = C.tile([128, KC], F32, "a1")
    C.TS("dve", a1[:], mod[:, 8:16], 1.0, None, ALU.add, None, [t_mod], [t_a1])
    C.TT("dve", a1[:], a1[:], g1s[:], ALU.mult, [t_a1, t_g1], [t_a1])
    wbf, t_wbf = C.tile([128, KC, nout], BF16, "wbf")
    wst = [C.tile([128, nout], F32, f"wst{i}") for i in range(2)]
    for k in range(KC):
        st, t_st = wst[k % 2]
        C.DMA(st[:], w_d[k * 128:(k + 1) * 128, :], [], [t_st])
        C.CP("pool", wbf[:, k, :], st[:], [t_st], [t_wbf])
    rs, t_rs = C.tile([128, TN], F32, "rs")
    tmpb = [C.tile([128, TN], F32, f"tmp{i}") for i in range(2)]
    hb, t_hb = C.tile([128, KC, TN], BF16, "hb")
    zo = [C.tile([128, TN], dst_dt, f"zo{i}") for i in range(4)]
    zst = [P.tok(f"zst{i}") for i in range(4)]
    for i in range(NTILE):
        c0 = i * TN
        C.rstd_bc([(x[:, k, c0:c0 + TN], tx[k][i]) for k in range(KC)], TN, 1.0 / D, rs[:], t_rs)
        for k in range(KC):
            tmp, t_tmp = tmpb[k % 2]
            C.STT("dve", tmp[:], x[:, k, c0:c0 + TN], a1[:, k:k + 1], rs[:], ALU.mult, ALU.mult,
                  [tx[k][i], t_a1, t_rs], [t_tmp])
            C.ACT(hb[:, k, :], tmp[:], AF.Identity, [t_tmp, t_mod], [t_hb], bias=mod[:, k:k + 1], scale=1.0)
        for j in range(nj):
            pj = min(128, nout - j * 128)
            bk, tb = C.bank()
            for k in range(KC):
                C.MM(bk[:pj, :], wbf[:, k, j * 128:j * 128 + pj], hb[:, k, :], k == 0, k == KC - 1, [t_wbf, t_hb], [tb],
                     lazy=True)
            z, t_z = zo[j % 4]
            t_zst = zst[j % 4]
            if j % 2 == 0:
                C.ACT(z[:pj, :], bk[:pj, :], AF.Copy, [tb], [t_z])
            else:
                C.CP("dve", z[:pj, :], bk[:pj, :], [tb], [t_z])
            if isinstance(dst_d, list):
                C.DMA(dst_d[j][0:pj, c0:c0 + TN], z[:pj, :], [t_z], [t_zst])
            else:
                C.DMA(dst_d[j * 128:j * 128 + pj, c0:c0 + TN], z[:pj, :], [t_z], [t_zst])
            if halo and i == NTILE - 1 and j >= 4:
                C.DMA(G["hsrc"][:, (j - 4) * 16:(j - 3) * 16], z[:, TN - 16:TN], [t_z], [G["t_hsrc"]])


def st_evenmix(C, G, i_even):
    P = C.P
    zT, t_zs = G["zs"], G["t_zs"]
    yT, t_ys = G["ys"], G["t_ys"]
    HB = 16
    W = HB + TN
    CC(C, "AllGather", ALU.bypass, G4, G["hsrc"], G["h4"], [G["t_hsrc"]], G["t_h4"])
    H, t_H = C.tile([128, 8, 192], F32, "H")
    hsel, t_hsel = C.load(G["hsel"], [128, 8], F32, "hsel")
    halo, t_halo = C.tile([128, 192], F32, "halo")

    def halo_step(n):
        if n == 0:
            CC(C, "AllGather", ALU.bypass, G2, G["h4"], G["h8"], [G["t_h4"]], G["t_h8"])
        elif n == 1:
            C.DMA(H[:], G["h8"].rearrange("(r p) f -> p r f", p=128), [G["t_h8"]], [t_H])
            C.TS("dve", halo[:], H[:, 0, :], hsel[:, 0:1], None, ALU.mult, None, [t_H, t_hsel], [t_halo])
            for r in range(1, 8):
                C.STT("dve", halo[:], H[:, r, :], hsel[:, r:r + 1], halo[:], ALU.mult, ALU.add,
                      [t_H, t_hsel, t_halo], [t_halo])
    cr, t_cr = C.load(G["corr"], [128, 4, 16], F32, "corr")
    cw, t_cw = C.load(G["convw"][i_even], [128, 4, 3], F32, "convw")
    pwf, t_pwf = C.load(G["poolw"][i_even], [128, 4, 128], F32, "poolwf")
    pw, t_pw = C.tile([128, 4, 128], BF16, "poolw")
    C.CP("pool", pw[:], pwf[:], [t_pwf], [t_pw])
    ps, t_ps = C.load(G["pscale"][i_even], [128, 4], F32, "pscale")
    R = lambda n: [C.tile([128, W], F32, f"{n}{i}") for i in range(2)]
    cb_, xb_, ub_, sA, sB, vb_ = R("c"), R("xa"), R("u"), R("sA"), R("sB"), R("v")
    bb_ = [C.tile([128, TN], F32, f"b{i}") for i in range(2)]
    t0b = [C.tile([128, TN], F32, f"t0{i}") for i in range(2)]
    yab = [C.tile([128, TN], F32, f"ya{i}") for i in range(2)]
    ybb = [C.tile([128, TN], F32, f"yb{i}") for i in range(2)]
    db = [C.tile([128, TN], BF16, f"d{i}") for i in range(2)]
    t16 = [C.tile([128, 16], F32, f"t16{i}") for i in range(2)]
    it = 0
    yast = [P.tok(f"yast{i}") for i in range(2)]
    ybst = [P.tok(f"ybst{i}") for i in range(2)]

    def load_halo(buf, tk, row0, i):
        c0 = i * TN
        if i == 0:
            j = (row0 - 512) // 128
            C.DMA(buf[:, HB:W], zT[row0:row0 + 128, 0:TN], [t_zs], [tk])
            C.ACT(buf[:, 0:HB], halo[:, j * 16:(j + 1) * 16], AF.Copy, [t_halo], [tk])
        else:
            C.DMA(buf[:, :], zT[row0:row0 + 128, c0 - HB:c0 + TN], [t_zs], [tk])

    for n_i, i in enumerate(list(range(1, NTILE)) + [0]):
        if n_i in (1, 2):
            halo_step(n_i - 1)
        c0 = i * TN
        for g in range(4):
            s = it % 2
            it += 1
            cc, t_c = cb_[s]
            xa, t_xa = xb_[s]
            bb, t_b = bb_[s]
            v, t_v = vb_[s]
            load_halo(cc, t_c, (4 + g) * 128, i)
            load_halo(xa, t_xa, (8 + g) * 128, i)
            C.DMA(bb[:], zT[g * 128:(g + 1) * 128, c0:c0 + TN], [t_zs], [t_b])
            C.TT("pool", v[:], cc[:], xa[:], ALU.mult, [t_c, t_xa], [t_v])
            t0, t_t0 = t0b[s]
            C.ACT(t0[:], v[:, HB:W], AF.Copy, [t_v, t_cw], [t_t0], scale=cw[:, g, 0:1])
            C.STT("dve", t0[:], v[:, HB - 1:W - 1], cw[:, g, 1:2], t0[:], ALU.mult, ALU.add, [t_v, t_cw, t_t0], [t_t0])
            C.STT("dve", t0[:], v[:, HB - 2:W - 2], cw[:, g, 2:3], t0[:], ALU.mult, ALU.add, [t_v, t_cw, t_t0], [t_t0])
            ya, t_ya = yab[s]
            C.TT("pool", ya[:], t0[:], bb[:], ALU.mult, [t_t0, t_b], [t_ya])
            C.DMA(yT[g * 128:(g + 1) * 128, c0:c0 + TN], ya[:], [t_ya], [yast[s]], eng="act")
            u, t_u = ub_[s]
            load_halo(u, t_u, (12 + g) * 128, i)
            a, t_a = sA[s]
            b2, t_b2 = sB[s]
            src, t_src = u, t_u
            sh = 1
            for st in range(g + 1):
                dst, t_dst = (a, t_a) if st % 2 == 0 else (b2, t_b2)
                lo = 2 * sh - 1
                C.TT("dve", dst[:, lo:W], src[:, lo:W], src[:, lo - sh:W - sh], ALU.add, [t_src], [t_dst])
                src, t_src = dst, t_dst
                sh *= 2
            wdw = float(POOL_W[g])
            d, t_d = db[s]
            C.STT("dve", d[:], src[:, HB:W], 1.0 / wdw, u[:, HB:W], ALU.mult, ALU.subtract, [t_src, t_u], [t_d])
            if i == 0:
                tt, t_tt = t16[s]
                C.TT("dve", tt[:], src[:, HB:HB + 16], cr[:, g, :], ALU.mult, [t_src, t_cr], [t_tt])
                C.TT("dve", d[:, 0:16], tt[:], u[:, HB:HB + 16], ALU.subtract, [t_tt, t_u], [t_d])
            bk, tb = C.bank()
            C.MM(bk[:], pw[:, g, :], d[:], True, True, [t_pw, t_d], [tb])
            yb, t_yb = ybb[s]
            C.ACT(yb[:], bk[:], AF.Copy, [tb, t_ps], [t_yb], scale=ps[:, g:g + 1])
            C.DMA(yT[(4 + g) * 128:(5 + g) * 128, c0:c0 + TN], yb[:], [t_yb], [ybst[s]], eng="act")


def st_proj(C, G, l, w_d):
    P = C.P
    x, tx = G["x"], G["tx"]
    scr = [C.tile([128, D], F32, f"scr{i}") for i in range(5)]
    mod, t_mod = C.mod_cols(G["cbc"], G["ada"][l][16:24], G["adab"][l][:, 16:24], 1, scr)
    if w_d is None:
        yb = [C.tile([128, TN], BF16, f"yrd{i}") for i in range(4)]
        q = 0
        for i in range(NTILE):
            c0 = i * TN
            for m in range(KC):
                y, t_y = yb[q % 4]
                q += 1
                C.DMA(y[:], G["yred"][i][m * 128:(m + 1) * 128, :], [G["t_yred"]], [t_y])
                C.STT("dve", x[:, m, c0:c0 + TN], y[:], mod[:, m:m + 1], x[:, m, c0:c0 + TN], ALU.mult, ALU.add,
                      [t_y, t_mod, tx[m][i]], [tx[m][i]])
        return
    wbf, t_wbf = C.tile([128, KC, D], BF16, "wbf")
    wst = [C.tile([128, D], F32, f"wst{i}") for i in range(2)]
    for k in range(KC):
        st, t_st = wst[k % 2]
        C.DMA(st[:], w_d[k * 128:(k + 1) * 128, :], [], [t_st])
        C.CP("pool", wbf[:, k, :], st[:], [t_st], [t_wbf])
    yst = [C.tile([128, TN], F32, f"yst{i}") for i in range(3)]
    ybf = [[C.tile([128, TN], BF16, f"ybf{k}_{s}") for s in range(2)] for k in range(KC)]
    q = 0
    for i in range(NTILE):
        c0 = i * TN
        for k in range(KC):
            st, t_st = yst[q % 3]
            q += 1
            C.DMA(st[:], G["ys"][k * 128:(k + 1) * 128, c0:c0 + TN], [G["t_ys"]], [t_st])
            C.CP("pool", ybf[k][i % 2][0][:], st[:], [t_st], [ybf[k][i % 2][1]])
        for m in range(KC):
            bk, tb = C.bank()
            for k in range(KC):
                C.MM(bk[:], wbf[:, k, m * 128:(m + 1) * 128], ybf[k][i % 2][0][:], k == 0, k == KC - 1,
                     [t_wbf, ybf[k][i % 2][1]], [tb], lazy=True)
            C.STT("dve", x[:, m, c0:c0 + TN], bk[:], mod[:, m:m + 1], x[:, m, c0:c0 + TN], ALU.mult, ALU.add,
                  [tb, t_mod, tx[m][i]], [tx[m][i]])


def st_moe(C, G, l):
    stg = [[C.tile([128, 1024], F32, f"stg{j}_{hh}") for hh in range(2)] for j in range(3)]
    mod, t_mod = C.mod_cols(G["cbc"], G["ada"][l][24:48], G["adab"][l][:, 24:48], 3,
                            [stg[0][0], stg[0][1], stg[1][0], stg[1][1], stg[2][0]])
    emit_moe(C, G["x"], G["tx"], 0, mod, t_mod, 0, G["g2"][l], G["wr"][l], G["br"][l], G["wg"][l], G["wu"][l],
             G["wd"][l], G["ident"], stg)


def st_attn(C, G, ia):
    P = C.P
    T = S
    NQ = T // TN
    NBK = T // 128
    g8 = G["g8"]
    GA = (9, 10, 11)
    t_g4a, t_g8a, t_g4b, t_g8b = P.tok("g4a"), P.tok("g8a"), P.tok("g4b"), P.tok("g8b")

    def g8tok(f0):
        return t_g8a if (f0 // 128) in GA else t_g8b

    def gathers():
        for j in GA:
            CC(C, "AllGather", ALU.bypass, G4, G["gsrc"][j], G["g4"][j], [G["t_gsrc"], t_g8a], t_g4a)
            CC(C, "AllGather", ALU.bypass, G2, G["g4"][j], g8[j], [t_g4a], t_g8a)
        for j in range(len(g8)):
            if j in GA:
                continue
            CC(C, "AllGather", ALU.bypass, G4, G["gsrc"][j], G["g4"][j], [G["t_gsrc"], t_g8a, t_g8b], t_g4b)
            CC(C, "AllGather", ALU.bypass, G2, G["g4"][j], g8[j], [t_g4b], t_g8b)

    def gsl(f0, n, t):
        r = t // 4
        lc = (t % 4) * TN
        j = f0 // 128
        rows = ATT_ROWS[j]
        o = f0 % 128
        assert o + n <= rows
        return g8[j][r * rows + o:r * rows + o + n, lc:lc + TN]

    accs = C.reserve(2)
    cols, t_cols = C.load(G["cols"][ia], [128, 16], F32, "cols")
    ng, t_ng = C.tile([128, 4], F32, "ng")
    C.TS("dve", ng[:96, 0:1], cols[:96, 7:8], 1.0, None, ALU.mult, None, [t_cols], [t_ng])
    C.STT("dve", ng[:96, 1:2], cols[:96, 8:9], 1.0, cols[:96, 12:13], ALU.mult, ALU.mult, [t_cols, t_ng], [t_ng])
    C.TS("dve", ng[:96, 2:3], cols[:96, 9:10], 1.0, None, ALU.mult, None, [t_cols, t_ng], [t_ng])
    C.STT("dve", ng[:96, 3:4], cols[:96, 10:11], 1.0, cols[:96, 12:13], ALU.mult, ALU.mult, [t_cols, t_ng], [t_ng])
    esk, t_esk = C.tile([128, 1], F32, "esk")
    C.ACT(esk[:], cols[:, 13:14], AF.Exp, [t_cols], [t_esk])

    wstage, t_wstage = C.tile([128, 1024], F32, "wstage")

    def wload(shape, name, view):
        n = int(np.prod(shape[1:]))
        p = shape[0]
        if len(shape) == 3:
            sv = wstage[:p, :n].rearrange("p (k f) -> p k f", k=shape[1])
        else:
            sv = wstage[:p, :n]
        C.DMA(sv, view, [], [t_wstage])
        b, t_b = C.tile(shape, BF16, name)
        C.CP("pool", b[:], sv, [t_wstage], [t_b])
        return b, t_b

    wuq_d, wuqs_d, wk_d, wks_d, wv_d, wo_d = (G[n][ia] for n in ("wuq", "wuqs", "wk", "wks", "wv", "wo"))
    wuq, t_wuq = wload([128, 3, 96], "wuq", wuq_d.rearrange("(k p) f -> p k f", p=128))
    wuqs, t_wuqs = wload([128, 3, 96], "wuqs", wuqs_d.rearrange("(k p) f -> p k f", p=128))
    wk, t_wk = wload([128, 2, 96], "wk", wk_d[0:256, :].rearrange("(k p) f -> p k f", p=128))
    wks, t_wks = wload([128, 2, 96], "wks", wks_d[0:256, :].rearrange("(k p) f -> p k f", p=128))
    wkr, t_wkr = wload([32, 96], "wkr", wk_d[256:288, :])
    wkrs, t_wkrs = wload([32, 96], "wkrs", wks_d[256:288, :])
    wv, t_wv = wload([128, 2, 64], "wv", wv_d.rearrange("(k p) f -> p k f", p=128))
    wos, t_wos = wload([64, D], "wos", wo_d[0:64, :])
    wom, t_wom = wload([64, D], "wom", wo_d[64:128, :])
    masks, t_masks = C.tile([128, 9, TN], BF16, "masks")
    C.DMA(masks[:], G["masks"].rearrange("j p q -> p j q"), [], [t_masks])
    selq, t_selq = C.load(G["selq"], [128, 4, 64], BF16, "selq")
    selkv, t_selkv = C.load(G["selkv"], [128, 64], BF16, "selkv")

    Kml = P.sbuf([96, T], BF16, "Kml")
    Vml = P.sbuf([128, NBK, 128], BF16, "Vml")
    tKml = [P.tok(f"Kml{i}") for i in range(NQ)]
    tVml = [P.tok(f"Vml{i}") for i in range(NQ)]
    t_on1 = P.tok("von1")
    C.MEMSET("pool", Vml[:, :, 64:128], 1.0, [t_on1])
    Kroll, t_Kroll = C.tile([64, 128 + TN], BF16, "Kroll")
    Vroll, t_Vroll = C.tile([128, 5, 128], BF16, "Vroll")
    C.MEMSET("pool", Vroll[:, :, 64:128], 1.0, [t_Vroll])

    F = lambda shape, name, d=F32: C.tile(shape, d, name)
    pos_t, t_pos = F([96, TN], "pos_t", I32)
    posf, t_posf = F([96, TN], "posf")
    a1, t_a1 = F([96, TN], "a1")
    a2, t_a2 = F([96, TN], "a2")
    Ct, t_Ct = F([96, TN], "Ct")
    St, t_St = F([96, TN], "St")
    rsA, t_rsA = F([128, TN], "rsA")
    rs96, t_rs96 = F([96, TN], "rs96")
    rs64, t_rs64 = F([64, TN], "rs64")
    u1, t_u1 = F([96, TN], "u1")
    u2, t_u2 = F([96, TN], "u2")
    xin, t_xin = F([128, 4, TN], "xin", BF16)
    xn, t_xn = F([128, 3, TN], "xn", BF16)
    krb, t_krb = F([32, TN], "krb", BF16)
    xk, t_xk = F([128, TN], "xk", BF16)
    xv, t_xv = F([128, TN], "xv", BF16)
    Q, t_Q = F([96, TN], "Q", BF16)
    Qs, t_Qs = F([64, TN], "Qs", BF16)
    pb = [F([128, TN], f"p{i}", BF16) for i in range(4)]
    rd, t_rd = F([128, TN], "rd")
    rd2, t_rd2 = F([128, TN], "rd2")
    o1, t_o1 = F([64, TN], "o1b", BF16)
    o2, t_o2 = F([64, TN], "o2b", BF16)
    yo = [F([128, TN], f"yo{i}", BF16) for i in range(4)]
    yq, t_yq = u2, t_u2
    qi, t_qi = pos_t, t_pos
    t_yp = G["t_yp"]

    def reduce_angle(a, t_a):
        C.TS("dve", yq[:], a[:], 1.0 / (2 * PI), None, ALU.mult, None, [t_a], [t_yq])
        C.CP("dve", qi[:], yq[:], [t_yq], [t_qi])
        C.CP("dve", yq[:], qi[:], [t_qi], [t_yq])
        C.STT("dve", a[:], yq[:], -2 * PI, a[:], ALU.mult, ALU.add, [t_yq, t_a], [t_a])
        C.TS("dve", yq[:], a[:], PI, None, ALU.is_ge, None, [t_a], [t_yq])
        C.STT("dve", a[:], yq[:], -2 * PI, a[:], ALU.mult, ALU.add, [t_yq, t_a], [t_a])
        C.TS("dve", a[:], a[:], PI, -PI, ALU.min, ALU.max, [t_a], [t_a])

    def rope_tabs(c0, ca, cb):
        C.DMA(pos_t[:], G["pos"][:, c0:c0 + TN], [], [t_pos])
        C.CP("dve", posf[:], pos_t[:], [t_pos], [t_posf])
        C.TS("dve", a1[:], posf[:], cols[:96, 11:12], None, ALU.mult, None, [t_posf, t_cols], [t_a1])
        C.TS("dve", a2[:], posf[:], cols[:96, 11:12], PI / 2, ALU.mult, ALU.add, [t_posf, t_cols], [t_a2])
        reduce_angle(a1, t_a1)
        reduce_angle(a2, t_a2)
        C.ACT(a1[:], a1[:], AF.Sin, [t_a1], [t_a1])
        C.ACT(a2[:], a2[:], AF.Sin, [t_a2], [t_a2])
        C.TS("dve", Ct[:], a2[:], ng[:96, ca:ca + 1], None, ALU.mult, None, [t_a2, t_ng], [t_Ct])
        C.TS("dve", St[:], a1[:], ng[:96, cb:cb + 1], None, ALU.mult, None, [t_a1, t_ng], [t_St])

    def norm_in(f0, nk, gcol0, t, inv_d):
        for k in range(nk):
            C.DMA(xin[:, k, :], gsl(f0 + k * 128, 128, t), [g8tok(f0 + k * 128)], [t_xin])
        C.rstd_bc([(xin[:, k, :], t_xin) for k in range(nk)], TN, inv_d, rsA[:], t_rsA, sq_eng=C.sq_eng)
        for k in range(nk):
            C.STT("dve", xn[:, k, :], xin[:, k, :], cols[:, gcol0 + k:gcol0 + k + 1], rsA[:], ALU.mult, ALU.mult,
                  [t_xin, t_cols, t_rsA], [t_xn])

    def rope_norm(bA, tA, bB, tB, out_ap, t_o):
        C.rstd_bc([(bA[:96, :], tA)], TN, 1.0 / 96, rs96[:], t_rs96, np_out=96)
        C.TT("dve", u1[:], bA[:96, :], Ct[:], ALU.mult, [tA, t_Ct], [t_u1])
        C.TT("dve", u2[:], bB[:96, :], St[:], ALU.mult, [tB, t_St], [t_u2])
        C.TT("dve", u1[:], u1[:], u2[:], ALU.add, [t_u1, t_u2], [t_u1])
        C.TT("dve", out_ap, u1[:], rs96[:], ALU.mult, [t_u1, t_rs96], [t_o])

    gathers()
    for i in range(NQ):
        c0 = i * TN
        norm_in(1152, 2, 5, i, 1.0 / 256)
        C.DMA(krb[:], gsl(1408, 32, i), [g8tok(1408)], [t_krb])
        bA, tA = C.bank()
        bB, tB = C.bank()
        for (bk, tb, w0, tw0, w1, tw1) in ((bA, tA, wk, t_wk, wkr, t_wkr), (bB, tB, wks, t_wks, wkrs, t_wkrs)):
            C.MM(bk[:96, :], w0[:, 0, :], xn[:, 0, :], True, False, [tw0, t_xn], [tb])
            C.MM(bk[:96, :], w0[:, 1, :], xn[:, 1, :], False, False, [tw0, t_xn], [tb])
            C.MM(bk[:96, :], w1[:, :], krb[:, :], False, True, [tw1, t_krb], [tb])
        rope_tabs(c0, 2, 3)
        rope_norm(bA, tA, bB, tB, Kml[:, c0:c0 + TN], tKml[i])
        bv, tbv = C.bank()
        for b in range(4):
            for k in range(2):
                C.MM(bv[:, b * 64:(b + 1) * 64], xn[:, k, b * 128:(b + 1) * 128], wv[:, k, :], k == 0, k == 1,
                     [t_xn, t_wv], [tbv])
        C.ACT(Vml[:, 4 * i:4 * i + 4, 0:64], bv[:, 0:256].rearrange("p (b d) -> p b d", d=64), AF.Copy,
              [tbv, t_on1], [tVml[i]])

    pre_ring = C.reserve(3)
    Qb = [(Q, t_Q), F([96, TN], "Q1", BF16)]
    Qsb = [(Qs, t_Qs), F([64, TN], "Qs1", BF16)]
    Krb = [(Kroll, t_Kroll), F([64, 128 + TN], "Kroll1", BF16)]
    Vr1, t_Vr1 = F([128, 5, 128], "Vroll1", BF16)
    C.MEMSET("pool", Vr1[:, :, 64:128], 1.0, [t_Vr1])
    Vrb = [(Vroll, t_Vroll), (Vr1, t_Vr1)]
    pi_ = 0
    yi_ = [0]

    def PRE(i):
        par = i % 2
        Qp, t_Qp = Qb[par]
        Qsp, t_Qsp = Qsb[par]
        Kr, t_Kr = Krb[par]
        Vr, t_Vr = Vrb[par]
        c0 = i * TN
        if i > 0:
            Kp, t_Kp = Krb[1 - par]
            Vp, t_Vp = Vrb[1 - par]
            C.CP("dve", Kr[:, 0:128], Kp[:, TN:TN + 128], [t_Kp], [t_Kr])
            C.CP("dve", Vr[:, 0, 0:64], Vp[:, 4, 0:64], [t_Vp], [t_Vr])
        norm_in(768, 3, 2, i, 1.0 / 384)
        bA, tA = C.bank()
        bB, tB = C.bank()
        for (bk, tb, w0, tw0) in ((bA, tA, wuq, t_wuq), (bB, tB, wuqs, t_wuqs)):
            for k in range(3):
                C.MM(bk[:96, :], w0[:, k, :], xn[:, k, :], k == 0, k == 2, [tw0, t_xn], [tb])
        rope_tabs(c0, 0, 1)
        rope_norm(bA, tA, bB, tB, Qp[:], t_Qp)
        for k in range(4):
            C.DMA(xin[:, k, :], gsl(k * 128, 128, i), [g8tok(k * 128)], [t_xin])
        bq, tbq = C.bank()
        for k in range(4):
            C.MM(bq[:64, :], selq[:, k, :], xin[:, k, :], k == 0, k == 3, [t_selq, t_xin], [tbq])
        C.rstd_bc([(bq[:64, :], tbq)], TN, 1.0 / 64, rs64[:], t_rs64, np_out=64)
        C.STT("dve", Qsp[:], bq[:64, :], cols[:64, 0:1], rs64[:], ALU.mult, ALU.mult, [tbq, t_cols, t_rs64], [t_Qsp])
        C.DMA(xk[:], gsl(512, 128, i), [g8tok(512)], [t_xk])
        bk2, tbk2 = C.bank()
        C.MM(bk2[:64, :], selkv[:], xk[:], True, True, [t_selkv, t_xk], [tbk2])
        C.rstd_bc([(bk2[:64, :], tbk2)], TN, 1.0 / 64, rs64[:], t_rs64, np_out=64)
        C.STT("dve", Kr[:, 128:128 + TN], bk2[:64, :], cols[:64, 1:2], rs64[:], ALU.mult, ALU.mult,
              [tbk2, t_cols, t_rs64], [t_Kr])
        C.DMA(xv[:], gsl(640, 128, i), [g8tok(640)], [t_xv])
        bv2, tbv2 = C.bank()
        for b in range(4):
            C.MM(bv2[:, b * 64:(b + 1) * 64], xv[:, b * 128:(b + 1) * 128], selkv[:], True, True, [t_xv, t_selkv], [tbv2])
        C.ACT(Vr[:, 1:5, 0:64], bv2[:, 0:256].rearrange("p (b d) -> p b d", d=64), AF.Copy, [tbv2], [t_Vr])

    def POSTb(i):
        r0 = (i // 4) * D
        lc = (i % 4) * TN
        for m in range(KC):
            by, tby = C.bank()
            C.MM(by[:], wos[:, m * 128:(m + 1) * 128], o2[:], True, False, [t_wos, t_o2], [tby])
            C.MM(by[:], wom[:, m * 128:(m + 1) * 128], o1[:], False, True, [t_wom, t_o1], [tby])
            yk = yi_[0] % len(yo)
            y, t_y = yo[yk]
            yi_[0] += 1
            if m % 4 == 0:
                C.ACT(y[:], by[:], AF.Copy, [tby], [t_y])
            else:
                C.CP("dve", y[:], by[:], [tby], [t_y])
            C.DMA(G["yp"][i % 4][r0 + m * 128:r0 + (m + 1) * 128, :], y[:], [t_y], [t_yst[yk]], eng="sp")

    bgq = []

    t_yst = [P.tok(f"yst{k}") for k in range(len(yo))]

    def reduce_quarter(q):
        CC(C, "ReduceScatter", ALU.add, G2, G["yp"][q], G["y2"][q], t_yst + [G["t_yred"]], G["t_y2"])
        CC(C, "ReduceScatter", ALU.add, G4, G["y2"][q], G["yred"][q], [G["t_y2"]], G["t_yred"])

    def record(fn, *a):
        P.rec = bgq
        C.pre = pre_ring
        C.sq_eng = "dve"
        fn(*a)
        P.rec = None
        C.pre = None
        C.sq_eng = "act"

    record(PRE, 0)
    P.replay(bgq, len(bgq))
    acc, t_acc = accs[0]
    acc2, t_acc2 = accs[1]
    for i in range(NQ):
        par = i % 2
        Qp, t_Qp = Qb[par]
        Qsp, t_Qsp = Qsb[par]
        Kr, t_Kr = Krb[par]
        Vr, t_Vr = Vrb[par]
        if i + 1 < NQ:
            record(PRE, i + 1)
        nkb = 4 * i + 4
        items = []
        for kb in range(nkb):
            items.append((Kml[:, kb * 128:(kb + 1) * 128], [tKml[kb // 4]], Qp, t_Qp, float(96 ** -0.5),
                          masks[:, kb - 4 * i, :] if kb >= 4 * i else None,
                          Vml[:, kb, :], [tVml[kb // 4], t_on1], acc, t_acc, kb == 0, kb == nkb - 1))
        rs_ = [r for r in range(5) if not (i == 0 and r == 0)]
        for n_, r in enumerate(rs_):
            items.append((Kr[:, r * 128:(r + 1) * 128], [t_Kr], Qsp, t_Qsp, 0.125, masks[:, 4 + r, :],
                          Vr[:, r, :], [t_Vr], acc2, t_acc2, n_ == 0, n_ == len(rs_) - 1))
        LA = 2
        pend = []
        nbg = len(bgq)
        done_bg = 0

        def emitA(it, pp):
            C.MM(it[8][:], it[6], pp[0][:], it[10], it[11], it[7] + [pp[1]], [it[9]])

        for n_it, it in enumerate(items):
            bs, tbs = C.bank()
            C.MM(bs[:], it[0], it[2][:], True, True, it[1] + [it[3]], [tbs])
            p, t_p = pb[pi_ % len(pb)]
            pi_ += 1
            C.ACT(p[:], bs[:], AF.Exp, [tbs], [t_p], scale=it[4])
            if it[5] is not None:
                C.TT("dve", p[:], p[:], it[5], ALU.mult, [t_p, t_masks], [t_p])
            pend.append((it, (p, t_p)))
            if len(pend) > LA:
                emitA(*pend.pop(0))
            want = ((n_it + 1) * nbg) // len(items)
            P.replay(bgq, want - done_bg)
            done_bg = want
        while pend:
            emitA(*pend.pop(0))
        P.replay(bgq, len(bgq))
        if i - 1 >= NQ - 4:
            reduce_quarter((i - 1) % 4)
        C.ACT(rd[64:128, :], acc[64:128, :], AF.Ln, [t_acc], [t_rd])
        C.ACT(rd[64:128, :], rd[64:128, :], AF.Exp, [t_rd], [t_rd], scale=-1.0)
        C.TT("dve", o1[:], acc[0:64, :], rd[64:128, :], ALU.mult, [t_acc, t_rd], [t_o1])
        C.ACT(rd2[64:128, :], acc2[64:128, :], AF.Ln, [t_acc2, t_esk], [t_rd2], bias=esk[64:128, :], scale=1.0)
        C.ACT(rd2[64:128, :], rd2[64:128, :], AF.Exp, [t_rd2], [t_rd2], scale=-1.0)
        C.TT("dve", o2[:], acc2[0:64, :], rd2[64:128, :], ALU.mult, [t_acc2, t_rd2], [t_o2])
        record(POSTb, i)
    P.replay(bgq, len(bgq))
    reduce_quarter((NQ - 1) % 4)
    C.banks = C.banks + pre_ring
    C.banks = C.banks + accs


def build_fused(nlayers=4):
    nc = bass.Bass("TRN2", target_bir_lowering=False)
    din = lambda name, shape, d=F32: nc.dram_tensor(name, list(shape), d, kind="ExternalInput").ap()
    dsc = lambda name, shape, d=F32: nc.dram_tensor(name, list(shape), d).ap()
    G = {}
    xT = din("xT", [D, NT])
    G["cbc"] = din("cbc", [128, D])
    G["ada"] = din("ada", [4, 48, 128, D])
    G["adab"] = din("adab", [4, 128, 48])
    G["g1"] = din("g1", [4, 128, KC])
    G["g2"] = din("g2", [4, 128, KC])
    cp_w_in = din("cp_w_in", [2, D, 2048])
    G["convw"] = din("convw", [2, 128, 4, 3])
    G["poolw"] = din("poolw", [2, 128, 4, 128])
    G["pscale"] = din("pscale", [2, 128, 4])
    cp_w_out = din("cp_w_out", [2, D, D])
    at_w_in = din("at_w_in", [2, D, NZ_ATT])
    G["corr"] = din("corr", [128, 4, 16])
    G["hsel"] = din("hsel", [128, 8])
    G["selq"] = din("selq", [128, 4, 64], BF16)
    G["selkv"] = din("selkv", [128, 64], BF16)
    G["wuq"] = din("wuq", [2, 384, 96])
    G["wuqs"] = din("wuqs", [2, 384, 96])
    G["wk"] = din("wk", [2, 288, 96])
    G["wks"] = din("wks", [2, 288, 96])
    G["wv"] = din("wv", [2, 256, 64])
    G["wo"] = din("wo", [2, 128, D])
    G["cols"] = din("cols", [2, 128, 16])
    G["masks"] = din("masks", [9, 128, TN], BF16)
    G["pos"] = din("pos", [96, S], I32)
    G["wr"] = din("wr", [4, 128, KC, 36])
    G["br"] = din("br", [4, 128, 36])
    G["wg"] = din("wg", [4, NE, D, FF])
    G["wu"] = din("wu", [4, NE, D, FF])
    G["wd"] = din("wd", [4, NE, FF, D])
    G["ident"] = din("ident", [128, 128])
    xo = nc.dram_tensor("xo", [D, NT], F32, kind="ExternalOutput").ap()
    G["zs"] = dsc("zs", [2048, NT])
    G["ys"] = dsc("ys", [D, NT])
    G["hsrc"] = dsc("hsrc", [128, 192])
    G["h4"] = dsc("h4", [512, 192])
    G["h8"] = dsc("h8", [1024, 192])
    G["gsrc"] = [dsc(f"gsrc{j}", [r, NT], BF16) for j, r in enumerate(ATT_ROWS)]
    G["g4"] = [dsc(f"g4_{j}", [4 * r, NT], BF16) for j, r in enumerate(ATT_ROWS)]
    G["g8"] = [dsc(f"g8_{j}", [8 * r, NT], BF16) for j, r in enumerate(ATT_ROWS)]
    G["yp"] = [dsc(f"yp{q}", [8 * D, TN], BF16) for q in range(NTILE)]
    G["y2"] = [dsc(f"y2_{q}", [4 * D, TN], BF16) for q in range(NTILE)]
    G["yred"] = [dsc(f"yred{q}", [D, TN], BF16) for q in range(NTILE)]
    with ExitStack() as es:
        C = Ctx(nc, es)
        P = C.P
        for n in ("zs", "ys", "hsrc", "h4", "h8", "gsrc", "g4", "g8", "yp", "y2", "yred"):
            G["t_" + n] = P.tok(n)
        G["t_ypq"] = [P.tok(f"ypq{q}") for q in range(NTILE)]
        x = P.sbuf([128, KC, NT], F32, "x")
        tx = [[P.tok(f"x{m}_{i}") for i in range(NTILE)] for m in range(KC)]
        G["x"], G["tx"] = x, tx
        for m in range(KC):
            C.DMA(x[:, m, :], xT[m * 128:(m + 1) * 128, :], [], tx[m])
        for l in range(nlayers):
            i = l // 2
            if l % 2 == 0:
                stage(C, lambda: st_normproj(C, G, l, cp_w_in[i], 2048, G["zs"], G["t_zs"], F32, True))
                stage(C, lambda: st_evenmix(C, G, i))
                stage(C, lambda: st_proj(C, G, l, cp_w_out[i]))
            else:
                stage(C, lambda: st_normproj(C, G, l, at_w_in[i], NZ_ATT, G["gsrc"], G["t_gsrc"], BF16, False))
                stage(C, lambda: st_attn(C, G, i))
                stage(C, lambda: st_proj(C, G, l, None))
            stage(C, lambda: st_moe(C, G, l))
        t_out = P.tok("xo")
        for m in range(KC):
            C.DMA(xo[m * 128:(m + 1) * 128, :], x[:, m, :], tx[m], [t_out], eng="sp")
        P.wait_all("sp", [t_out])
        P.emit()
    return nc


def _cols(v, n=KC):
    return np.ascontiguousarray(np.asarray(v, np.float32).reshape(n, 128).T)


def _attn_masks():
    k = np.arange(128)[:, None]
    q = np.arange(TN)[None, :]
    m = np.zeros((9, 128, TN), np.float32)
    for d in range(4):
        m[d] = (q >= d * 128 + k)
    for r in range(5):
        kp = (r - 1) * 128 + k
        m[4 + r] = (q >= kp) & (q < kp + 128)
    return m.astype(ml_dtypes.bfloat16)


_PROG = {}
NL = 4


def kernel(**inputs):
    inp = {k: np.asarray(v) for k, v in inputs.items()}
    nl = NL
    if ("fused", nl) not in _PROG:
        _PROG[("fused", nl)] = build_fused(nl)
    nc = _PROG[("fused", nl)]
    f32 = np.float32
    x = inp["x"][0]
    ada = np.ascontiguousarray(inp["ada_w"].transpose(0, 2, 1).reshape(4, 48, 128, D))
    adab = np.ascontiguousarray(inp["ada_b"].reshape(4, 48, 128).transpose(0, 2, 1))
    com = dict(
        cbc=np.ascontiguousarray(np.broadcast_to(inp["c"].reshape(1, D), (128, D))),
        ada=ada, adab=adab,
        g1=np.ascontiguousarray(inp["norm1_g"].reshape(4, KC, 128).transpose(0, 2, 1)),
        g2=np.ascontiguousarray(inp["norm2_g"].reshape(4, KC, 128).transpose(0, 2, 1)),
        cp_w_in=np.ascontiguousarray(inp["cp_w_in"]),
        convw=np.ascontiguousarray(inp["conv_w"].reshape(2, 3, 4, 128).transpose(0, 3, 2, 1)),
        poolw=np.ascontiguousarray(inp["pool_w"].transpose(0, 2, 1, 3)),
        pscale=np.ascontiguousarray(inp["pool_scale"].reshape(2, 4, 128).transpose(0, 2, 1)),
        cp_w_out=np.ascontiguousarray(inp["cp_w_out"]),
        at_w_in=np.ascontiguousarray(inp["at_w_in"]),
        masks=_attn_masks(),
        pos=np.ascontiguousarray(np.broadcast_to(inp["positions"].reshape(1, S).astype(np.int32), (96, S))),
        wr=np.ascontiguousarray(np.concatenate([inp["moe_w_group"], inp["moe_w_expert"]], axis=2)
                                .reshape(4, KC, 128, 36).transpose(0, 2, 1, 3)),
        br=np.ascontiguousarray(np.broadcast_to(
            np.concatenate([inp["moe_b_group"], inp["moe_b_expert"]], axis=1)[:, None, :], (4, 128, 36))),
        wg=np.ascontiguousarray(inp["moe_w_gate"]), wu=np.ascontiguousarray(inp["moe_w_up"]),
        wd=np.ascontiguousarray(inp["moe_w_down"]),
        ident=np.eye(128, dtype=f32),
    )
    inv = np.power(f32(10000.0), -np.arange(16, dtype=f32) / 16).astype(f32)
    maps = []
    for c in range(NCORE):
        h = c
        kvh = h // 4
        corr = np.zeros((128, 4, 16), f32)
        for g, wd_ in enumerate(POOL_W):
            t = np.arange(16, dtype=f32)
            corr[:, g, :] = (1.0 / np.minimum(t + 1.0, float(wd_))) if c == 0 else (1.0 / wd_)
        hsel = np.zeros((128, 8), f32)
        if c > 0:
            hsel[:, c - 1] = 1.0
        selq = np.zeros((128, 4, 64), f32)
        for j in range(64):
            f = h * 64 + j
            selq[f % 128, f // 128, j] = 1.0
        selkv = np.zeros((128, 64), f32)
        for j in range(64):
            selkv[kvh * 64 + j, j] = 1.0
        wuq = np.ascontiguousarray(inp["mla_w_uq"][:, :, h * 96:(h + 1) * 96])
        wkv = inp["mla_w_ukv"][:, :, h * 128:(h + 1) * 128]
        wk = np.zeros((2, 288, 96), f32)
        wk[:, 0:256, 0:64] = wkv[:, :, 0:64]
        wk[:, 256:288, 64:96] = np.eye(32, dtype=f32)
        cols = np.zeros((2, 128, 16), f32)
        for i in range(2):
            cols[i, 0:64, 0] = inp["swa_q_g"][i]
            cols[i, 0:64, 1] = inp["swa_k_g"][i]
            cols[i, :, 2:5] = _cols(inp["mla_q_norm_g"][i], 3)
            cols[i, :, 5:7] = _cols(inp["mla_kv_norm_g"][i], 2)
            cols[i, 0:96, 7] = inp["mla_q_g"][i]
            cols[i, 0:96, 8] = inp["mla_q_g"][i][PERM96]
            cols[i, 0:96, 9] = inp["mla_k_g"][i]
            cols[i, 0:96, 10] = inp["mla_k_g"][i][PERM96]
            cols[i, 64:80, 11] = inv
            cols[i, 80:96, 11] = inv
            cols[i, 64:80, 12] = -1.0
            cols[i, 80:96, 12] = 1.0
            cols[i, :, 13] = inp["swa_sinks"][i][h]
        wo = np.ascontiguousarray(np.concatenate(
            [inp["at_w_out"][:, h * 64:(h + 1) * 64, :], inp["at_w_out"][:, 512 + h * 64:512 + (h + 1) * 64, :]], axis=1))
        maps.append(dict(
            com, xT=np.ascontiguousarray(x[c * NT:(c + 1) * NT].T), corr=corr, hsel=hsel,
            selq=selq.astype(ml_dtypes.bfloat16), selkv=selkv.astype(ml_dtypes.bfloat16),
            wuq=wuq, wuqs=np.ascontiguousarray(wuq[:, :, PERM96]), wk=wk, wks=np.ascontiguousarray(wk[:, :, PERM96]),
            wv=np.ascontiguousarray(wkv[:, :, 64:128]), wo=wo, cols=cols))
    res = run_bass_kernel_spmd(nc, maps, core_ids=list(range(NCORE)))
    out = np.concatenate([res.results[c]["xo"].T for c in range(NCORE)], axis=0)[None]
    return np.ascontiguousarray(out.astype(np.float32))
```

```python
import numpy as np
import ml_dtypes
from contextlib import ExitStack
import concourse.bass as bass
import concourse.mybir as mybir
from concourse.bass_utils import run_bass_kernel_spmd

F32 = mybir.dt.float32
BF16 = mybir.dt.bfloat16
I32 = mybir.dt.int32
ALU = mybir.AluOpType
AF = mybir.ActivationFunctionType
AX = mybir.AxisListType

NCORE = 8
S = 16384
D = 1024
NT = S // NCORE
TN = 512
NTILE = NT // TN
KC = D // 128
EPS = 1e-6
NE = 32
FF = 256


class Tok:
    __slots__ = ("name", "writer", "readers", "dsem", "dcount")

    def __init__(self, name=""):
        self.name = name
        self.writer = None
        self.readers = {}
        self.dsem = None
        self.dcount = 0


class Eng:
    def __init__(self, name, sem):
        self.name = name
        self.sem = sem
        self.count = 0
        self.ops = []
        self.waited = {}


class Prog:
    def __init__(self, nc, es):
        self.nc = nc
        self.es = es
        self.engs = {}
        for n in ("pe", "act", "dve", "pool", "sp"):
            sem = es.enter_context(nc.semaphore("s_" + n))
            self.engs[n] = Eng(n, sem)
        self.ntok = 0
        self.nsb = 0
        self.es_outer = es
        self.free_dsems = []
        self.stage_toks = []
        self.all_dtoks = []
        self.in_stage = False
        self.rec = None

    def sbuf(self, shape, dt, name=None):
        self.nsb += 1
        return self.es.enter_context(self.nc.sbuf_tensor(f"S{self.nsb}_{name or ''}", list(shape), dt))

    def psum(self, shape, dt=F32, name=None):
        self.nsb += 1
        return self.es.enter_context(self.nc.psum_tensor(name or f"ps{self.nsb}", list(shape), dt))

    def tok(self, name=""):
        self.ntok += 1
        return Tok(name or f"t{self.ntok}")

    def _dsem(self, t):
        if t.dsem is None:
            if self.free_dsems:
                sem, cnt = self.free_dsems.pop()
            else:
                sem = self.es_outer.enter_context(self.nc.semaphore(f"d{self.ntok}"))
                self.ntok += 1
                cnt = 0
            t.dsem = sem
            t.dcount = cnt
            self.all_dtoks.append(t)
            if self.in_stage:
                self.stage_toks.append(t)
        return t.dsem

    def barrier(self):
        for E in self.engs.values():
            waits = []
            for E2 in self.engs.values():
                if E2 is not E and E2.count > E.waited.get(id(E2.sem), 0):
                    E.waited[id(E2.sem)] = E2.count
                    waits.append((E2.sem, E2.count))
            for t in self.all_dtoks:
                if t.dcount > E.waited.get(id(t.dsem), 0):
                    E.waited[id(t.dsem)] = t.dcount
                    waits.append((t.dsem, t.dcount))
            E.ops.append((waits, None, []))

    def end_stage(self):
        self.barrier()
        self.emit()
        for t in self.stage_toks:
            self.free_dsems.append((t.dsem, t.dcount))
            self.all_dtoks.remove(t)
            t.dsem = None
        self.stage_toks = []

    def _collect(self, E, reads, writes, skip_sid=None):
        need = {}

        def add(p, same_ok):
            if p is None:
                return
            sid, sem, val = p
            if sid == skip_sid:
                return
            if sid == id(E.sem) and (not same_ok or E.name == "pe"):
                return
            if need.get(sid, (None, -1))[1] < val:
                need[sid] = (sem, val)

        for t in reads:
            add(t.writer, True)
        for t in writes:
            add(t.writer, True)
            for sid, (sem, val) in t.readers.items():
                add((sid, sem, val), False)
        waits = []
        for sid, (sem, val) in need.items():
            if E.waited.get(sid, 0) < val:
                E.waited[sid] = val
                waits.append((sem, val))
        return waits

    def op(self, eng, fn, reads=(), writes=(), inc=True):
        if self.rec is not None:
            self.rec.append(("op", (eng, fn, tuple(reads), tuple(writes), inc)))
            return
        E = self.engs[eng]
        waits = self._collect(E, reads, writes)
        if inc:
            E.count += 1
            cnt = E.count
            incs = [(E.sem, 1)]
        else:
            cnt = E.count + 1
            incs = []
        me = (id(E.sem), E.sem, cnt)
        E.ops.append((waits, fn, incs))
        for t in reads:
            t.readers[id(E.sem)] = (E.sem, cnt)
        for t in writes:
            t.writer = me
            t.readers = {}

    def dma(self, eng, fn, reads=(), writes=(), inc=16, serial=False):
        if self.rec is not None:
            self.rec.append(("dma", (eng, fn, tuple(reads), tuple(writes), inc, serial)))
            return
        E = self.engs[eng]
        t0 = writes[0]
        sem = self._dsem(t0)
        waits = self._collect(E, reads, writes, skip_sid=None if serial else id(sem))
        t0.dcount += inc
        me = (id(sem), sem, t0.dcount)
        E.ops.append((waits, fn, [(sem, inc)]))
        for t in reads:
            t.readers[id(sem)] = (sem, t0.dcount)
        for t in writes:
            t.writer = me
            t.readers = {}

    def replay(self, q, n):
        assert self.rec is None
        while n > 0 and q:
            kind, args = q.pop(0)
            if kind == "op":
                self.op(*args)
            else:
                self.dma(*args)
            n -= 1

    def wait_all(self, eng, toks):
        E = self.engs[eng]
        waits = self._collect(E, toks, ())
        E.ops.append((waits, None, []))

    def emit(self):
        nc = self.nc
        with nc.Block() as block:
            def run(E, h):
                for waits, fn, incs in E.ops:
                    for sem, val in waits:
                        h.wait_ge(sem, val)
                    if fn is None:
                        continue
                    r = fn(h)
                    for sem, v in incs:
                        r.then_inc(sem, v)

            @block.tensor
            def _(h):
                run(self.engs["pe"], h)

            @block.scalar
            def _(h):
                run(self.engs["act"], h)

            @block.vector
            def _(h):
                run(self.engs["dve"], h)

            @block.gpsimd
            def _(h):
                run(self.engs["pool"], h)

            @block.sync
            def _(h):
                run(self.engs["sp"], h)
        for E in self.engs.values():
            E.ops = []


class Ctx:
    def __init__(self, nc, es):
        self.nc = nc
        self.P = Prog(nc, es)
        P = self.P
        self.banks = [(P.psum([128, 512], F32, name=f"bank{i}"), P.tok(f"bank{i}")) for i in range(8)]
        self.bi = 0
        self.pre = None
        self.pbi = 0
        self.sq_eng = "act"
        self.ones = P.sbuf([128, 128], BF16, "ones_bf")
        self.t_ones = P.tok("ones")
        self.epsc = P.sbuf([128, 1], F32, "epsc")
        self.t_eps = P.tok("eps")
        P.op("pool", lambda h: h.memset(self.ones[:], 1.0), writes=[self.t_ones])
        P.op("pool", lambda h: h.memset(self.epsc[:], EPS), writes=[self.t_eps])
        self.sq = [(P.sbuf([128, 512], BF16, f"sq{i}"), P.tok(f"sq{i}")) for i in range(2)]
        self.sqi = 0
        self.dq = 0

    def bank(self):
        if self.pre is not None:
            b = self.pre[self.pbi % len(self.pre)]
            self.pbi += 1
            return b
        b = self.banks[self.bi % len(self.banks)]
        self.bi += 1
        return b

    def reserve(self, n):
        r = self.banks[-n:]
        self.banks = self.banks[:-n]
        return r

    def tile(self, shape, dt, name=None):
        t = self.P.sbuf(shape, dt, name)
        return t, self.P.tok(name or "tile")

    def ACT(self, out, in_, func, r, w, **kw):
        self.P.op("act", lambda h: h.activation(out=out, in_=in_, func=func, **kw), r, w)

    def MM(self, out, lhsT, rhs, start, stop, r, w, lazy=False):
        self.P.op("pe", lambda h: h.matmul(out, lhsT=lhsT, rhs=rhs, start=start, stop=stop), r, w,
                  inc=(stop or not lazy))

    def TT(self, eng, out, in0, in1, op, r, w):
        self.P.op(eng, lambda h: h.tensor_tensor(out=out, in0=in0, in1=in1, op=op), r, w)

    def TS(self, eng, out, in0, s1, s2, op0, op1, r, w):
        if s2 is None:
            self.P.op(eng, lambda h: h.tensor_scalar(out=out, in0=in0, scalar1=s1, scalar2=None, op0=op0), r, w)
        else:
            self.P.op(eng, lambda h: h.tensor_scalar(out=out, in0=in0, scalar1=s1, scalar2=s2, op0=op0, op1=op1), r, w)

    def STT(self, eng, out, in0, scalar, in1, op0, op1, r, w):
        self.P.op(eng, lambda h: h.scalar_tensor_tensor(out=out, in0=in0, scalar=scalar, in1=in1, op0=op0, op1=op1), r, w)

    def CP(self, eng, out, in_, r, w):
        self.P.op(eng, lambda h: h.tensor_copy(out=out, in_=in_), r, w)

    def RED(self, eng, out, in_, op, r, w):
        self.P.op(eng, lambda h: h.tensor_reduce(out=out, in_=in_, axis=AX.X, op=op), r, w)

    def RECIP(self, out, in_, r, w):
        self.P.op("dve", lambda h: h.reciprocal(out=out, in_=in_), r, w)

    def MEMSET(self, eng, ap, val, w):
        self.P.op(eng, lambda h: h.memset(ap, val), (), w)

    def DMA(self, out, in_, r, w, eng=None):
        if eng is None:
            eng = "sp"
            self.dq += 1
        self.P.dma(eng, lambda h: h.dma_start(out=out, in_=in_), r, w)

    def load(self, dram_ap, shape, dt, name, eng=None):
        t, tk = self.tile(shape, dt, name)
        self.DMA(t[:], dram_ap, [], [tk], eng=eng)
        return t, tk

    def rstd_bc(self, chunks, n, inv_d, out, t_out, ones_ap=None, np_out=128, sq_eng=None):
        bank, tb = self.bank()
        last = len(chunks) - 1
        for k, (ap, tk) in enumerate(chunks):
            sq, tsq = self.sq[self.sqi % 2]
            self.sqi += 1
            p = ap.shape[0]
            if sq_eng == "dve":
                self.TT("dve", sq[:p, :n], ap, ap, ALU.mult, [tk], [tsq])
            else:
                self.ACT(sq[:p, :n], ap, AF.Square, [tk], [tsq])
            lhs = self.ones[:p, :np_out] if ones_ap is None else ones_ap
            self.MM(bank[:np_out, :n], lhs, sq[:p, :n], k == 0, k == last, [tsq, self.t_ones], [tb])
        self.ACT(out, bank[:np_out, :n], AF.Ln, [tb, self.t_eps], [t_out], scale=inv_d, bias=self.epsc[:np_out, :])
        self.ACT(out, out, AF.Exp, [t_out], [t_out], scale=-0.5)

    def mod_cols(self, cbc_d, adaT_d, adab_d, nvec, scratch):
        cb, t_cb = scratch[0]
        self.DMA(cb[:], cbc_d, [], [t_cb])
        ca, t_ca = scratch[1]
        self.ACT(ca[:], cb[:], AF.Silu, [t_cb], [t_ca])
        ab, t_ab = self.load(adab_d, [128, nvec * KC], F32, "adab")
        mod, t_mod = self.tile([128, nvec * KC], F32, "mod")
        abuf = scratch[2:4]
        prod, t_prod = scratch[4]
        for j in range(nvec * KC):
            a, t_a = abuf[j % 2]
            self.DMA(a[:], adaT_d[j], [], [t_a])
            self.TT("dve", prod[:], a[:], ca[:], ALU.mult, [t_a, t_ca], [t_prod])
            self.RED("dve", mod[:, j:j + 1], prod[:], ALU.add, [t_prod], [t_mod])
        self.TT("dve", mod[:], mod[:], ab[:], ALU.add, [t_mod, t_ab], [t_mod])
        return mod, t_mod


def emit_moe(C, x, tx, x0, mod, t_mod, vb, g2_d, wr_d, br_d, wg_d, wu_d, wd_d, ident_d, stg):
    P = C.P
    g2, t_g2 = C.load(g2_d, [128, KC], F32, "g2")
    wr, t_wr = C.load(wr_d, [128, KC, 36], F32, "wr")
    br, t_br = C.load(br_d, [128, 36], F32, "br")
    ident, t_id = C.load(ident_d, [128, 128], F32, "ident")
    sel, t_sel = C.tile([32, NE, 128], BF16, "gsel")
    C.CP("dve", sel[:], ident[0:32, 0:NE].unsqueeze(2).to_broadcast([32, NE, 128]), [t_id], [t_sel])
    a2, t_a2 = C.tile([128, KC], F32, "a2")
    C.TS("dve", a2[:], mod[:, vb + 8:vb + 16], 1.0, None, ALU.add, None, [t_mod], [t_a2])
    C.TT("dve", a2[:], a2[:], g2[:], ALU.mult, [t_a2, t_g2], [t_a2])
    sh2 = mod[:, vb:vb + 8]
    gt2 = mod[:, vb + 16:vb + 24]

    h2 = P.sbuf([128, KC, NT], BF16, "h2")
    th2 = [[P.tok(f"h2_{k}_{i}") for i in range(NTILE)] for k in range(KC)]
    NB = NT // 128
    logits, t_lg = C.tile([128, NB, 36], F32, "logits")
    rs, t_rs = C.tile([128, TN], F32, "rs2")
    tmpb = [C.tile([128, TN], F32, f"n2tmp{i}") for i in range(2)]
    hfb = [C.tile([128, TN], F32, f"n2hf{i}") for i in range(2)]

    for i in range(NTILE):
        c0 = x0 + i * TN
        C.rstd_bc([(x[:, k, c0:c0 + TN], tx[k][i]) for k in range(KC)], TN, 1.0 / D, rs[:], t_rs)
        bls = [C.bank() for _ in range(4)]
        for k in range(KC):
            tmp, t_tmp = tmpb[k % 2]
            hf, t_hf = hfb[k % 2]
            C.STT("dve", tmp[:], x[:, k, c0:c0 + TN], a2[:, k:k + 1], rs[:], ALU.mult, ALU.mult,
                  [tx[k][i], t_a2, t_rs], [t_tmp])
            C.ACT(hf[:], tmp[:], AF.Identity, [t_tmp, t_mod], [t_hf], bias=sh2[:, k:k + 1], scale=1.0)
            for b in range(4):
                C.MM(bls[b][0][:, 0:36], hf[:, b * 128:(b + 1) * 128], wr[:, k, :], k == 0, k == KC - 1,
                     [t_hf, t_wr], [bls[b][1]])
            C.CP("pool", h2[:, k, i * TN:(i + 1) * TN], hf[:], [t_hf], [th2[k][i]])
        for b in range(4):
            C.TT("dve", logits[:, i * 4 + b, :], bls[b][0][:, 0:36], br[:], ALU.add, [bls[b][1], t_br], [t_lg])

    def T(shape, name):
        return C.tile(shape, F32, name)

    gl = logits[:, :, 0:4]
    el = logits[:, :, 4:36].rearrange("p b (g i) -> p b g i", i=8)
    gmax, t_gmax = T([128, NB], "gmax")
    C.RED("dve", gmax[:], gl, ALU.max, [t_lg], [t_gmax])
    ohg, t_ohg = T([128, NB, 4], "ohg")
    C.TT("dve", ohg[:], gl, gmax[:].unsqueeze(2).to_broadcast([128, NB, 4]), ALU.is_equal, [t_lg, t_gmax], [t_ohg])
    gex, t_gex = T([128, NB, 4], "gex")
    C.TT("dve", gex[:], gl, gmax[:].unsqueeze(2).to_broadcast([128, NB, 4]), ALU.subtract, [t_lg, t_gmax], [t_gex])
    C.ACT(gex[:], gex[:], AF.Exp, [t_gex], [t_gex])
    gw, t_gw = T([128, NB], "gw")
    C.RED("dve", gw[:], gex[:], ALU.add, [t_gex], [t_gw])
    C.RECIP(gw[:], gw[:], [t_gw], [t_gw])
    tmp4, t_tmp4 = T([128, NB, 4, 8], "tmp4")
    C.TT("dve", tmp4[:], el, ohg[:].unsqueeze(3).to_broadcast([128, NB, 4, 8]), ALU.mult, [t_lg, t_ohg], [t_tmp4])
    sel8, t_sel8 = T([128, NB, 8], "sel8")
    C.RED("dve", sel8[:], tmp4[:].rearrange("p b g i -> p b i g"), ALU.add, [t_tmp4], [t_sel8])
    m1, t_m1 = T([128, NB], "m1")
    C.RED("dve", m1[:], sel8[:], ALU.max, [t_sel8], [t_m1])
    oh1, t_oh1 = T([128, NB, 8], "oh1")
    C.TT("dve", oh1[:], sel8[:], m1[:].unsqueeze(2).to_broadcast([128, NB, 8]), ALU.is_equal, [t_sel8, t_m1], [t_oh1])
    sel2, t_sel2 = T([128, NB, 8], "sel2")
    C.STT("dve", sel2[:], oh1[:], -1e30, sel8[:], ALU.mult, ALU.add, [t_oh1, t_sel8], [t_sel2])
    m2, t_m2 = T([128, NB], "m2")
    C.RED("dve", m2[:], sel2[:], ALU.max, [t_sel2], [t_m2])
    oh2, t_oh2 = T([128, NB, 8], "oh2")
    C.TT("dve", oh2[:], sel2[:], m2[:].unsqueeze(2).to_broadcast([128, NB, 8]), ALU.is_equal, [t_sel2, t_m2], [t_oh2])
    rr, t_rr = T([128, NB], "rr")
    C.TT("dve", rr[:], m2[:], m1[:], ALU.subtract, [t_m1, t_m2], [t_rr])
    C.ACT(rr[:], rr[:], AF.Exp, [t_rr], [t_rr])
    w1, t_w1 = T([128, NB], "w1")
    C.TS("dve", w1[:], rr[:], 1.0, None, ALU.add, None, [t_rr], [t_w1])
    C.RECIP(w1[:], w1[:], [t_w1], [t_w1])
    w2, t_w2 = T([128, NB], "w2")
    C.TT("dve", w2[:], rr[:], w1[:], ALU.mult, [t_rr, t_w1], [t_w2])
    C.TT("dve", w1[:], w1[:], gw[:], ALU.mult, [t_w1, t_gw], [t_w1])
    C.TT("dve", w2[:], w2[:], gw[:], ALU.mult, [t_w2, t_gw], [t_w2])
    C.TT("dve", oh1[:], oh1[:], w1[:].unsqueeze(2).to_broadcast([128, NB, 8]), ALU.mult, [t_oh1, t_w1], [t_oh1])
    C.TT("dve", oh2[:], oh2[:], w2[:].unsqueeze(2).to_broadcast([128, NB, 8]), ALU.mult, [t_oh2, t_w2], [t_oh2])
    C.TT("dve", oh1[:], oh1[:], oh2[:], ALU.add, [t_oh1, t_oh2], [t_oh1])
    gates, t_gates = T([128, NB, 4, 8], "gates")
    C.TT("dve", gates[:], ohg[:].unsqueeze(3).to_broadcast([128, NB, 4, 8]),
         oh1[:].unsqueeze(2).to_broadcast([128, NB, 4, 8]), ALU.mult, [t_ohg, t_oh1], [t_gates])

    GT, t_GT = C.tile([32, NT], F32, "GT")
    GTh, t_GTh = C.tile([32, NT], BF16, "GTh")
    GTl, t_GTl = C.tile([32, NT], BF16, "GTl")
    for i in range(NTILE):
        bk, tb = C.bank()
        for b in range(4):
            C.MM(bk[0:32, b * 128:(b + 1) * 128], gates[:, i * 4 + b, :, :].rearrange("p g i -> p (g i)"), ident[:],
                 True, True, [t_gates, t_id], [tb])
        C.ACT(GT[:, i * TN:(i + 1) * TN], bk[0:32, :], AF.Copy, [tb], [t_GT])
        C.CP("dve", GTh[:, i * TN:(i + 1) * TN], GT[:, i * TN:(i + 1) * TN], [t_GT], [t_GTh])
        C.TT("dve", GTl[:, i * TN:(i + 1) * TN], GT[:, i * TN:(i + 1) * TN], GTh[:, i * TN:(i + 1) * TN], ALU.subtract,
             [t_GT, t_GTh], [t_GTl])

    wb = [[C.tile([128, 2048], BF16, f"wb{j}_{s}") for s in range(2)] for j in range(3)]
    sgb = tmpb
    t1b = hfb
    acb = [[C.tile([128, TN], BF16, f"actp{c}_{i}") for i in range(2)] for c in range(2)]

    def load_w(e):
        s = e % 2
        srcs = (wg_d[e].rearrange("(k p) f -> p k f", p=128), wu_d[e].rearrange("(k p) f -> p k f", p=128),
                wd_d[e].rearrange("(k p) f -> p k f", p=128))
        shp = ((KC, FF), (KC, FF), (2, D))
        for j in range(3):
            w, t_w = wb[j][s]
            kk = shp[j][0] // 2
            for hh in range(2):
                st, t_st = stg[j][hh]
                C.DMA(st[:].rearrange("p (k f) -> p k f", k=kk), srcs[j][:, hh * kk:(hh + 1) * kk, :], [], [t_st])
                C.CP("pool", w[:, hh * 1024:(hh + 1) * 1024], st[:], [t_st], [t_w])

    units = [(e, i) for e in range(NE) for i in range(NTILE)]
    state = {}

    def GU(u):
        e, i = units[u]
        s = e % 2
        wgb, t_wgb = wb[0][s]
        wub, t_wub = wb[1][s]
        wgv = wgb[:].rearrange("p (k f) -> p k f", k=KC)
        wuv = wub[:].rearrange("p (k f) -> p k f", k=KC)
        bg, tbg = C.bank()
        C.MM(bg[:], sel[:, e, :], GTh[:, i * TN:(i + 1) * TN], True, False, [t_sel, t_GTh], [tbg], lazy=True)
        C.MM(bg[:], sel[:, e, :], GTl[:, i * TN:(i + 1) * TN], False, True, [t_sel, t_GTl], [tbg], lazy=True)
        acts = []
        for c in range(2):
            bga, tbga = C.bank()
            bup, tbup = C.bank()
            for k in range(KC):
                C.MM(bga[:], wgv[:, k, c * 128:(c + 1) * 128], h2[:, k, i * TN:(i + 1) * TN], k == 0, k == KC - 1,
                     [t_wgb, th2[k][i]], [tbga], lazy=True)
            for k in range(KC):
                C.MM(bup[:], wuv[:, k, c * 128:(c + 1) * 128], h2[:, k, i * TN:(i + 1) * TN], k == 0, k == KC - 1,
                     [t_wub, th2[k][i]], [tbup], lazy=True)
            sg, t_sg = sgb[c]
            t1, t_t1 = t1b[c]
            ac, t_ac = acb[c][u % 2]
            C.ACT(sg[:], bga[:], AF.Silu, [tbga], [t_sg])
            C.TT("dve", t1[:], sg[:], bg[:], ALU.mult, [t_sg, tbg], [t_t1])
            C.TT("dve", ac[:], t1[:], bup[:], ALU.mult, [t_t1, tbup], [t_ac])
            acts.append((ac, t_ac))
        state[u] = acts

    def DOWN(u):
        e, i = units[u]
        s = e % 2
        wdb, t_wdb = wb[2][s]
        wdv = wdb[:].rearrange("p (k f) -> p k f", k=2)
        acts = state.pop(u)
        c0 = x0 + i * TN
        for m in range(KC):
            bd, tbd = C.bank()
            for c in range(2):
                C.MM(bd[:], wdv[:, c, m * 128:(m + 1) * 128], acts[c][0][:], c == 0, c == 1,
                     [t_wdb, acts[c][1]], [tbd], lazy=True)
            C.STT("dve", x[:, m, c0:c0 + TN], bd[:], gt2[:, m:m + 1], x[:, m, c0:c0 + TN], ALU.mult, ALU.add,
                  [tbd, t_mod, tx[m][i]], [tx[m][i]])

    load_w(0)
    for u in range(len(units)):
        e, i = units[u]
        if i == 0 and e + 1 < NE:
            load_w(e + 1)
        if u == 0:
            GU(0)
        if u + 1 < len(units):
            GU(u + 1)
        DOWN(u)


POOL_W = (2, 4, 8, 16)
PI = float(np.pi)
PERM96 = list(range(64)) + list(range(80, 96)) + list(range(64, 80))
G4 = [[0, 1, 2, 3], [4, 5, 6, 7]]
G2 = [[0, 4], [1, 5], [2, 6], [3, 7]]
NZ_ATT = 1440
ATT_ROWS = [128] * 11 + [32]


def stage(C, fn):
    P = C.P
    with ExitStack() as es_s:
        P.es = es_s
        P.in_stage = True
        fn()
        P.end_stage()
    P.es = P.es_outer
    P.in_stage = False


def CC(C, kind, op, groups, src_ap, dst_ap, r_toks, w_tok):
    C.P.dma("pool", lambda h: h.collective_compute(kind, op, replica_groups=groups, ins=[src_ap], outs=[dst_ap]),
            r_toks, [w_tok], inc=1, serial=True)


def st_normproj(C, G, l, w_d, nout, dst_d, t_dst, dst_dt, halo):
    P = C.P
    x, tx = G["x"], G["tx"]
    nj = (nout + 127) // 128
    scr = [C.tile([128, D], F32, f"scr{i}") for i in range(5)]
    mod, t_mod = C.mod_cols(G["cbc"], G["ada"][l][0:16], G["adab"][l][:, 0:16], 2, scr)
    g1s, t_g1 = C.load(G["g1"][l], [128, KC], F32, "g1")
    a1, t_a1 = C.tile([128, KC], F32, "a1")
    C.TS("dve", a1[:], mod[:, 8:16], 1.0, None, ALU.add, None, [t_mod], [t_a1])
    C.TT("dve", a1[:], a1[:], g1s[:], ALU.mult, [t_a1, t_g1], [t_a1])
    wbf, t_wbf = C.tile([128, KC, nout], BF16, "wbf")
    wst = [C.tile([128, nout], F32, f"wst{i}") for i in range(2)]
    for k in range(KC):
        st, t_st = wst[k % 2]
        C.DMA(st[:], w_d[k * 128:(k + 1) * 128, :], [], [t_st])
        C.CP("pool", wbf[:, k, :], st[:], [t_st], [t_wbf])
    rs, t_rs = C.tile([128, TN], F32, "rs")
    tmpb = [C.tile([128, TN], F32, f"tmp{i}") for i in range(2)]
    hb, t_hb = C.tile([128, KC, TN], BF16, "hb")
    zo = [C.tile([128, TN], dst_dt, f"zo{i}") for i in range(4)]
    zst = [P.tok(f"zst{i}") for i in range(4)]
    for i in range(NTILE):
        c0 = i * TN
        C.rstd_bc([(x[:, k, c0:c0 + TN], tx[k][i]) for k in range(KC)], TN, 1.0 / D, rs[:], t_rs)
        for k in range(KC):
            tmp, t_tmp = tmpb[k % 2]
            C.STT("dve", tmp[:], x[:, k, c0:c0 + TN], a1[:, k:k + 1], rs[:], ALU.mult, ALU.mult,
                  [tx[k][i], t_a1, t_rs], [t_tmp])
            C.ACT(hb[:, k, :], tmp[:], AF.Identity, [t_tmp, t_mod], [t_hb], bias=mod[:, k:k + 1], scale=1.0)
        for j in range(nj):
            pj = min(128, nout - j * 128)
            bk, tb = C.bank()
            for k in range(KC):
                C.MM(bk[:pj, :], wbf[:, k, j * 128:j * 128 + pj], hb[:, k, :], k == 0, k == KC - 1, [t_wbf, t_hb], [tb],
                     lazy=True)
            z, t_z = zo[j % 4]
            t_zst = zst[j % 4]
            if j % 2 == 0:
                C.ACT(z[:pj, :], bk[:pj, :], AF.Copy, [tb], [t_z])
            else:
                C.CP("dve", z[:pj, :], bk[:pj, :], [tb], [t_z])
            if isinstance(dst_d, list):
                C.DMA(dst_d[j][0:pj, c0:c0 + TN], z[:pj, :], [t_z], [t_zst])
            else:
                C.DMA(dst_d[j * 128:j * 128 + pj, c0:c0 + TN], z[:pj, :], [t_z], [t_zst])
            if halo and i == NTILE - 1 and j >= 4:
                C.DMA(G["hsrc"][:, (j - 4) * 16:(j - 3) * 16], z[:, TN - 16:TN], [t_z], [G["t_hsrc"]])


def st_evenmix(C, G, i_even):
    P = C.P
    zT, t_zs = G["zs"], G["t_zs"]
    yT, t_ys = G["ys"], G["t_ys"]
    HB = 16
    W = HB + TN
    CC(C, "AllGather", ALU.bypass, G4, G["hsrc"], G["h4"], [G["t_hsrc"]], G["t_h4"])
    H, t_H = C.tile([128, 8, 192], F32, "H")
    hsel, t_hsel = C.load(G["hsel"], [128, 8], F32, "hsel")
    halo, t_halo = C.tile([128, 192], F32, "halo")

    def halo_step(n):
        if n == 0:
            CC(C, "AllGather", ALU.bypass, G2, G["h4"], G["h8"], [G["t_h4"]], G["t_h8"])
        elif n == 1:
            C.DMA(H[:], G["h8"].rearrange("(r p) f -> p r f", p=128), [G["t_h8"]], [t_H])
            C.TS("dve", halo[:], H[:, 0, :], hsel[:, 0:1], None, ALU.mult, None, [t_H, t_hsel], [t_halo])
            for r in range(1, 8):
                C.STT("dve", halo[:], H[:, r, :], hsel[:, r:r + 1], halo[:], ALU.mult, ALU.add,
                      [t_H, t_hsel, t_halo], [t_halo])
    cr, t_cr = C.load(G["corr"], [128, 4, 16], F32, "corr")
    cw, t_cw = C.load(G["convw"][i_even], [128, 4, 3], F32, "convw")
    pwf, t_pwf = C.load(G["poolw"][i_even], [128, 4, 128], F32, "poolwf")
    pw, t_pw = C.tile([128, 4, 128], BF16, "poolw")
    C.CP("pool", pw[:], pwf[:], [t_pwf], [t_pw])
    ps, t_ps = C.load(G["pscale"][i_even], [128, 4], F32, "pscale")
    R = lambda n: [C.tile([128, W], F32, f"{n}{i}") for i in range(2)]
    cb_, xb_, ub_, sA, sB, vb_ = R("c"), R("xa"), R("u"), R("sA"), R("sB"), R("v")
    bb_ = [C.tile([128, TN], F32, f"b{i}") for i in range(2)]
    t0b = [C.tile([128, TN], F32, f"t0{i}") for i in range(2)]
    yab = [C.tile([128, TN], F32, f"ya{i}") for i in range(2)]
    ybb = [C.tile([128, TN], F32, f"yb{i}") for i in range(2)]
    db = [C.tile([128, TN], BF16, f"d{i}") for i in range(2)]
    t16 = [C.tile([128, 16], F32, f"t16{i}") for i in range(2)]
    it = 0
    yast = [P.tok(f"yast{i}") for i in range(2)]
    ybst = [P.tok(f"ybst{i}") for i in range(2)]

    def load_halo(buf, tk, row0, i):
        c0 = i * TN
        if i == 0:
            j = (row0 - 512) // 128
            C.DMA(buf[:, HB:W], zT[row0:row0 + 128, 0:TN], [t_zs], [tk])
            C.ACT(buf[:, 0:HB], halo[:, j * 16:(j + 1) * 16], AF.Copy, [t_halo], [tk])
        else:
            C.DMA(buf[:, :], zT[row0:row0 + 128, c0 - HB:c0 + TN], [t_zs], [tk])

    for n_i, i in enumerate(list(range(1, NTILE)) + [0]):
        if n_i in (1, 2):
            halo_step(n_i - 1)
        c0 = i * TN
        for g in range(4):
            s = it % 2
            it += 1
            cc, t_c = cb_[s]
            xa, t_xa = xb_[s]
            bb, t_b = bb_[s]
            v, t_v = vb_[s]
            load_halo(cc, t_c, (4 + g) * 128, i)
            load_halo(xa, t_xa, (8 + g) * 128, i)
            C.DMA(bb[:], zT[g * 128:(g + 1) * 128, c0:c0 + TN], [t_zs], [t_b])
            C.TT("pool", v[:], cc[:], xa[:], ALU.mult, [t_c, t_xa], [t_v])
            t0, t_t0 = t0b[s]
            C.ACT(t0[:], v[:, HB:W], AF.Copy, [t_v, t_cw], [t_t0], scale=cw[:, g, 0:1])
            C.STT("dve", t0[:], v[:, HB - 1:W - 1], cw[:, g, 1:2], t0[:], ALU.mult, ALU.add, [t_v, t_cw, t_t0], [t_t0])
            C.STT("dve", t0[:], v[:, HB - 2:W - 2], cw[:, g, 2:3], t0[:], ALU.mult, ALU.add, [t_v, t_cw, t_t0], [t_t0])
            ya, t_ya = yab[s]
            C.TT("pool", ya[:], t0[:], bb[:], ALU.mult, [t_t0, t_b], [t_ya])
            C.DMA(yT[g * 128:(g + 1) * 128, c0:c0 + TN], ya[:], [t_ya], [yast[s]], eng="act")
            u, t_u = ub_[s]
            load_halo(u, t_u, (12 + g) * 128, i)
            a, t_a = sA[s]
            b2, t_b2 = sB[s]
            src, t_src = u, t_u
            sh = 1
            for st in range(g + 1):
                dst, t_dst = (a, t_a) if st % 2 == 0 else (b2, t_b2)
                lo = 2 * sh - 1
                C.TT("dve", dst[:, lo:W], src[:, lo:W], src[:, lo - sh:W - sh], ALU.add, [t_src], [t_dst])
                src, t_src = dst, t_dst
                sh *= 2
            wdw = float(POOL_W[g])
            d, t_d = db[s]
            C.STT("dve", d[:], src[:, HB:W], 1.0 / wdw, u[:, HB:W], ALU.mult, ALU.subtract, [t_src, t_u], [t_d])
            if i == 0:
                tt, t_tt = t16[s]
                C.TT("dve", tt[:], src[:, HB:HB + 16], cr[:, g, :], ALU.mult, [t_src, t_cr], [t_tt])
                C.TT("dve", d[:, 0:16], tt[:], u[:, HB:HB + 16], ALU.subtract, [t_tt, t_u], [t_d])
            bk, tb = C.bank()
            C.MM(bk[:], pw[:, g, :], d[:], True, True, [t_pw, t_d], [tb])
            yb, t_yb = ybb[s]
            C.ACT(yb[:], bk[:], AF.Copy, [tb, t_ps], [t_yb], scale=ps[:, g:g + 1])
            C.DMA(yT[(4 + g) * 128:(5 + g) * 128, c0:c0 + TN], yb[:], [t_yb], [ybst[s]], eng="act")


def st_proj(C, G, l, w_d):
    P = C.P
    x, tx = G["x"], G["tx"]
    scr = [C.tile([128, D], F32, f"scr{i}") for i in range(5)]
    mod, t_mod = C.mod_cols(G["cbc"], G["ada"][l][16:24], G["adab"][l][:, 16:24], 1, scr)
    if w_d is None:
        yb = [C.tile([128, TN], BF16, f"yrd{i}") for i in range(4)]
        q = 0
        for i in range(NTILE):
            c0 = i * TN
            for m in range(KC):
                y, t_y = yb[q % 4]
                q += 1
                C.DMA(y[:], G["yred"][i][m * 128:(m + 1) * 128, :], [G["t_yred"]], [t_y])
                C.STT("dve", x[:, m, c0:c0 + TN], y[:], mod[:, m:m + 1], x[:, m, c0:c0 + TN], ALU.mult, ALU.add,
                      [t_y, t_mod, tx[m][i]], [tx[m][i]])
        return
    wbf, t_wbf = C.tile([128, KC, D], BF16, "wbf")
    wst = [C.tile([128, D], F32, f"wst{i}") for i in range(2)]
    for k in range(KC):
        st, t_st = wst[k % 2]
        C.DMA(st[:], w_d[k * 128:(k + 1) * 128, :], [], [t_st])
        C.CP("pool", wbf[:, k, :], st[:], [t_st], [t_wbf])
    yst = [C.tile([128, TN], F32, f"yst{i}") for i in range(3)]
    ybf = [[C.tile([128, TN], BF16, f"ybf{k}_{s}") for s in range(2)] for k in range(KC)]
    q = 0
    for i in range(NTILE):
        c0 = i * TN
        for k in range(KC):
            st, t_st = yst[q % 3]
            q += 1
            C.DMA(st[:], G["ys"][k * 128:(k + 1) * 128, c0:c0 + TN], [G["t_ys"]], [t_st])
            C.CP("pool", ybf[k][i % 2][0][:], st[:], [t_st], [ybf[k][i % 2][1]])
        for m in range(KC):
            bk, tb = C.bank()
            for k in range(KC):
                C.MM(bk[:], wbf[:, k, m * 128:(m + 1) * 128], ybf[k][i % 2][0][:], k == 0, k == KC - 1,
                     [t_wbf, ybf[k][i % 2][1]], [tb], lazy=True)
            C.STT("dve", x[:, m, c0:c0 + TN], bk[:], mod[:, m:m + 1], x[:, m, c0:c0 + TN], ALU.mult, ALU.add,
                  [tb, t_mod, tx[m][i]], [tx[m][i]])


def st_moe(C, G, l):
    stg = [[C.tile([128, 1024], F32, f"stg{j}_{hh}") for hh in range(2)] for j in range(3)]
    mod, t_mod = C.mod_cols(G["cbc"], G["ada"][l][24:48], G["adab"][l][:, 24:48], 3,
                            [stg[0][0], stg[0][1], stg[1][0], stg[1][1], stg[2][0]])
    emit_moe(C, G["x"], G["tx"], 0, mod, t_mod, 0, G["g2"][l], G["wr"][l], G["br"][l], G["wg"][l], G["wu"][l],
             G["wd"][l], G["ident"], stg)


def st_attn(C, G, ia):
    P = C.P
    T = S
    NQ = T // TN
    NBK = T // 128
    g8 = G["g8"]
    GA = (9, 10, 11)
    t_g4a, t_g8a, t_g4b, t_g8b = P.tok("g4a"), P.tok("g8a"), P.tok("g4b"), P.tok("g8b")

    def g8tok(f0):
        return t_g8a if (f0 // 128) in GA else t_g8b

    def gathers():
        for j in GA:
            CC(C, "AllGather", ALU.bypass, G4, G["gsrc"][j], G["g4"][j], [G["t_gsrc"], t_g8a], t_g4a)
            CC(C, "AllGather", ALU.bypass, G2, G["g4"][j], g8[j], [t_g4a], t_g8a)
        for j in range(len(g8)):
            if j in GA:
                continue
            CC(C, "AllGather", ALU.bypass, G4, G["gsrc"][j], G["g4"][j], [G["t_gsrc"], t_g8a, t_g8b], t_g4b)
            CC(C, "AllGather", ALU.bypass, G2, G["g4"][j], g8[j], [t_g4b], t_g8b)

    def gsl(f0, n, t):
        r = t // 4
        lc = (t % 4) * TN
        j = f0 // 128
        rows = ATT_ROWS[j]
        o = f0 % 128
        assert o + n <= rows
        return g8[j][r * rows + o:r * rows + o + n, lc:lc + TN]

    accs = C.reserve(2)
    gathers()
    cols, t_cols = C.load(G["cols"][ia], [128, 16], F32, "cols")
    ng, t_ng = C.tile([128, 4], F32, "ng")
    C.TS("dve", ng[:96, 0:1], cols[:96, 7:8], 1.0, None, ALU.mult, None, [t_cols], [t_ng])
    C.STT("dve", ng[:96, 1:2], cols[:96, 8:9], 1.0, cols[:96, 12:13], ALU.mult, ALU.mult, [t_cols, t_ng], [t_ng])
    C.TS("dve", ng[:96, 2:3], cols[:96, 9:10], 1.0, None, ALU.mult, None, [t_cols, t_ng], [t_ng])
    C.STT("dve", ng[:96, 3:4], cols[:96, 10:11], 1.0, cols[:96, 12:13], ALU.mult, ALU.mult, [t_cols, t_ng], [t_ng])
    esk, t_esk = C.tile([128, 1], F32, "esk")
    C.ACT(esk[:], cols[:, 13:14], AF.Exp, [t_cols], [t_esk])

    wstage, t_wstage = C.tile([128, 1024], F32, "wstage")

    def wload(shape, name, view):
        n = int(np.prod(shape[1:]))
        p = shape[0]
        if len(shape) == 3:
            sv = wstage[:p, :n].rearrange("p (k f) -> p k f", k=shape[1])
        else:
            sv = wstage[:p, :n]
        C.DMA(sv, view, [], [t_wstage])
        b, t_b = C.tile(shape, BF16, name)
        C.CP("dve", b[:], sv, [t_wstage], [t_b])
        return b, t_b

    wuq_d, wuqs_d, wk_d, wks_d, wv_d, wo_d = (G[n][ia] for n in ("wuq", "wuqs", "wk", "wks", "wv", "wo"))
    wuq, t_wuq = wload([128, 3, 96], "wuq", wuq_d.rearrange("(k p) f -> p k f", p=128))
    wuqs, t_wuqs = wload([128, 3, 96], "wuqs", wuqs_d.rearrange("(k p) f -> p k f", p=128))
    wk, t_wk = wload([128, 2, 96], "wk", wk_d[0:256, :].rearrange("(k p) f -> p k f", p=128))
    wks, t_wks = wload([128, 2, 96], "wks", wks_d[0:256, :].rearrange("(k p) f -> p k f", p=128))
    wkr, t_wkr = wload([32, 96], "wkr", wk_d[256:288, :])
    wkrs, t_wkrs = wload([32, 96], "wkrs", wks_d[256:288, :])
    wv, t_wv = wload([128, 2, 64], "wv", wv_d.rearrange("(k p) f -> p k f", p=128))
    wos, t_wos = wload([64, D], "wos", wo_d[0:64, :])
    wom, t_wom = wload([64, D], "wom", wo_d[64:128, :])
    masks, t_masks = C.tile([128, 9, TN], BF16, "masks")
    C.DMA(masks[:], G["masks"].rearrange("j p q -> p j q"), [], [t_masks])
    selq, t_selq = C.load(G["selq"], [128, 4, 64], BF16, "selq")
    selkv, t_selkv = C.load(G["selkv"], [128, 64], BF16, "selkv")

    Kml = P.sbuf([96, T], BF16, "Kml")
    Vml = P.sbuf([128, NBK, 128], BF16, "Vml")
    tKml = [P.tok(f"Kml{i}") for i in range(NQ)]
    tVml = [P.tok(f"Vml{i}") for i in range(NQ)]
    t_on1 = P.tok("von1")
    C.MEMSET("dve", Vml[:, :, 64:128], 1.0, [t_on1])
    Kroll, t_Kroll = C.tile([64, 128 + TN], BF16, "Kroll")
    Vroll, t_Vroll = C.tile([128, 5, 128], BF16, "Vroll")
    C.MEMSET("dve", Vroll[:, :, 64:128], 1.0, [t_Vroll])

    F = lambda shape, name, d=F32: C.tile(shape, d, name)
    pos_t, t_pos = F([96, TN], "pos_t", I32)
    posf, t_posf = F([96, TN], "posf")
    a1, t_a1 = F([96, TN], "a1")
    a2, t_a2 = F([96, TN], "a2")
    Ct, t_Ct = F([96, TN], "Ct")
    St, t_St = F([96, TN], "St")
    rsA, t_rsA = F([128, TN], "rsA")
    rs96, t_rs96 = F([96, TN], "rs96")
    rs64, t_rs64 = F([64, TN], "rs64")
    u1, t_u1 = F([96, TN], "u1")
    u2, t_u2 = F([96, TN], "u2")
    xin, t_xin = F([128, 4, TN], "xin", BF16)
    xn, t_xn = F([128, 3, TN], "xn", BF16)
    krb, t_krb = F([32, TN], "krb", BF16)
    xk, t_xk = F([128, TN], "xk", BF16)
    xv, t_xv = F([128, TN], "xv", BF16)
    Q, t_Q = F([96, TN], "Q", BF16)
    Qs, t_Qs = F([64, TN], "Qs", BF16)
    pb = [F([128, TN], f"p{i}", BF16) for i in range(4)]
    rd, t_rd = F([128, TN], "rd")
    rd2, t_rd2 = F([128, TN], "rd2")
    o1, t_o1 = F([64, TN], "o1b", BF16)
    o2, t_o2 = F([64, TN], "o2b", BF16)
    yo = [F([128, TN], f"yo{i}", BF16) for i in range(4)]
    yq, t_yq = u2, t_u2
    qi, t_qi = pos_t, t_pos
    t_yp = G["t_yp"]

    def reduce_angle(a, t_a):
        C.TS("dve", yq[:], a[:], 1.0 / (2 * PI), None, ALU.mult, None, [t_a], [t_yq])
        C.CP("dve", qi[:], yq[:], [t_yq], [t_qi])
        C.CP("dve", yq[:], qi[:], [t_qi], [t_yq])
        C.STT("dve", a[:], yq[:], -2 * PI, a[:], ALU.mult, ALU.add, [t_yq, t_a], [t_a])
        C.TS("dve", yq[:], a[:], PI, None, ALU.is_ge, None, [t_a], [t_yq])
        C.STT("dve", a[:], yq[:], -2 * PI, a[:], ALU.mult, ALU.add, [t_yq, t_a], [t_a])
        C.TS("dve", a[:], a[:], PI, -PI, ALU.min, ALU.max, [t_a], [t_a])

    def rope_tabs(c0, ca, cb):
        C.DMA(pos_t[:], G["pos"][:, c0:c0 + TN], [], [t_pos])
        C.CP("dve", posf[:], pos_t[:], [t_pos], [t_posf])
        C.TS("dve", a1[:], posf[:], cols[:96, 11:12], None, ALU.mult, None, [t_posf, t_cols], [t_a1])
        C.TS("dve", a2[:], posf[:], cols[:96, 11:12], PI / 2, ALU.mult, ALU.add, [t_posf, t_cols], [t_a2])
        reduce_angle(a1, t_a1)
        reduce_angle(a2, t_a2)
        C.ACT(a1[:], a1[:], AF.Sin, [t_a1], [t_a1])
        C.ACT(a2[:], a2[:], AF.Sin, [t_a2], [t_a2])
        C.TS("dve", Ct[:], a2[:], ng[:96, ca:ca + 1], None, ALU.mult, None, [t_a2, t_ng], [t_Ct])
        C.TS("dve", St[:], a1[:], ng[:96, cb:cb + 1], None, ALU.mult, None, [t_a1, t_ng], [t_St])

    def norm_in(f0, nk, gcol0, t, inv_d):
        for k in range(nk):
            C.DMA(xin[:, k, :], gsl(f0 + k * 128, 128, t), [g8tok(f0 + k * 128)], [t_xin])
        C.rstd_bc([(xin[:, k, :], t_xin) for k in range(nk)], TN, inv_d, rsA[:], t_rsA, sq_eng=C.sq_eng)
        for k in range(nk):
            C.STT("dve", xn[:, k, :], xin[:, k, :], cols[:, gcol0 + k:gcol0 + k + 1], rsA[:], ALU.mult, ALU.mult,
                  [t_xin, t_cols, t_rsA], [t_xn])

    def rope_norm(bA, tA, bB, tB, out_ap, t_o):
        C.rstd_bc([(bA[:96, :], tA)], TN, 1.0 / 96, rs96[:], t_rs96, np_out=96)
        C.TT("dve", u1[:], bA[:96, :], Ct[:], ALU.mult, [tA, t_Ct], [t_u1])
        C.TT("dve", u2[:], bB[:96, :], St[:], ALU.mult, [tB, t_St], [t_u2])
        C.TT("dve", u1[:], u1[:], u2[:], ALU.add, [t_u1, t_u2], [t_u1])
        C.TT("dve", out_ap, u1[:], rs96[:], ALU.mult, [t_u1, t_rs96], [t_o])

    for i in range(NQ):
        c0 = i * TN
        norm_in(1152, 2, 5, i, 1.0 / 256)
        C.DMA(krb[:], gsl(1408, 32, i), [g8tok(1408)], [t_krb])
        bA, tA = C.bank()
        bB, tB = C.bank()
        for (bk, tb, w0, tw0, w1, tw1) in ((bA, tA, wk, t_wk, wkr, t_wkr), (bB, tB, wks, t_wks, wkrs, t_wkrs)):
            C.MM(bk[:96, :], w0[:, 0, :], xn[:, 0, :], True, False, [tw0, t_xn], [tb])
            C.MM(bk[:96, :], w0[:, 1, :], xn[:, 1, :], False, False, [tw0, t_xn], [tb])
            C.MM(bk[:96, :], w1[:, :], krb[:, :], False, True, [tw1, t_krb], [tb])
        rope_tabs(c0, 2, 3)
        rope_norm(bA, tA, bB, tB, Kml[:, c0:c0 + TN], tKml[i])
        bv, tbv = C.bank()
        for b in range(4):
            for k in range(2):
                C.MM(bv[:, b * 64:(b + 1) * 64], xn[:, k, b * 128:(b + 1) * 128], wv[:, k, :], k == 0, k == 1,
                     [t_xn, t_wv], [tbv])
        C.ACT(Vml[:, 4 * i:4 * i + 4, 0:64], bv[:, 0:256].rearrange("p (b d) -> p b d", d=64), AF.Copy,
              [tbv, t_on1], [tVml[i]])

    pre_ring = C.reserve(3)
    Qb = [(Q, t_Q), F([96, TN], "Q1", BF16)]
    Qsb = [(Qs, t_Qs), F([64, TN], "Qs1", BF16)]
    Krb = [(Kroll, t_Kroll), F([64, 128 + TN], "Kroll1", BF16)]
    Vr1, t_Vr1 = F([128, 5, 128], "Vroll1", BF16)
    C.MEMSET("dve", Vr1[:, :, 64:128], 1.0, [t_Vr1])
    Vrb = [(Vroll, t_Vroll), (Vr1, t_Vr1)]
    pi_ = 0
    yi_ = [0]

    def PRE(i):
        par = i % 2
        Qp, t_Qp = Qb[par]
        Qsp, t_Qsp = Qsb[par]
        Kr, t_Kr = Krb[par]
        Vr, t_Vr = Vrb[par]
        c0 = i * TN
        if i > 0:
            Kp, t_Kp = Krb[1 - par]
            Vp, t_Vp = Vrb[1 - par]
            C.CP("dve", Kr[:, 0:128], Kp[:, TN:TN + 128], [t_Kp], [t_Kr])
            C.CP("dve", Vr[:, 0, 0:64], Vp[:, 4, 0:64], [t_Vp], [t_Vr])
        norm_in(768, 3, 2, i, 1.0 / 384)
        bA, tA = C.bank()
        bB, tB = C.bank()
        for (bk, tb, w0, tw0) in ((bA, tA, wuq, t_wuq), (bB, tB, wuqs, t_wuqs)):
            for k in range(3):
                C.MM(bk[:96, :], w0[:, k, :], xn[:, k, :], k == 0, k == 2, [tw0, t_xn], [tb])
        rope_tabs(c0, 0, 1)
        rope_norm(bA, tA, bB, tB, Qp[:], t_Qp)
        for k in range(4):
            C.DMA(xin[:, k, :], gsl(k * 128, 128, i), [g8tok(k * 128)], [t_xin])
        bq, tbq = C.bank()
        for k in range(4):
            C.MM(bq[:64, :], selq[:, k, :], xin[:, k, :], k == 0, k == 3, [t_selq, t_xin], [tbq])
        C.rstd_bc([(bq[:64, :], tbq)], TN, 1.0 / 64, rs64[:], t_rs64, np_out=64)
        C.STT("dve", Qsp[:], bq[:64, :], cols[:64, 0:1], rs64[:], ALU.mult, ALU.mult, [tbq, t_cols, t_rs64], [t_Qsp])
        C.DMA(xk[:], gsl(512, 128, i), [g8tok(512)], [t_xk])
        bk2, tbk2 = C.bank()
        C.MM(bk2[:64, :], selkv[:], xk[:], True, True, [t_selkv, t_xk], [tbk2])
        C.rstd_bc([(bk2[:64, :], tbk2)], TN, 1.0 / 64, rs64[:], t_rs64, np_out=64)
        C.STT("dve", Kr[:, 128:128 + TN], bk2[:64, :], cols[:64, 1:2], rs64[:], ALU.mult, ALU.mult,
              [tbk2, t_cols, t_rs64], [t_Kr])
        C.DMA(xv[:], gsl(640, 128, i), [g8tok(640)], [t_xv])
        bv2, tbv2 = C.bank()
        for b in range(4):
            C.MM(bv2[:, b * 64:(b + 1) * 64], xv[:, b * 128:(b + 1) * 128], selkv[:], True, True, [t_xv, t_selkv], [tbv2])
        C.ACT(Vr[:, 1:5, 0:64], bv2[:, 0:256].rearrange("p (b d) -> p b d", d=64), AF.Copy, [tbv2], [t_Vr])

    def POSTb(i):
        r0 = (i // 4) * D
        lc = (i % 4) * TN
        for m in range(KC):
            by, tby = C.bank()
            C.MM(by[:], wos[:, m * 128:(m + 1) * 128], o2[:], True, False, [t_wos, t_o2], [tby])
            C.MM(by[:], wom[:, m * 128:(m + 1) * 128], o1[:], False, True, [t_wom, t_o1], [tby])
            yk = yi_[0] % len(yo)
            y, t_y = yo[yk]
            yi_[0] += 1
            if m % 4 == 0:
                C.ACT(y[:], by[:], AF.Copy, [tby], [t_y])
            else:
                C.CP("dve", y[:], by[:], [tby], [t_y])
            C.DMA(G["yp"][i % 4][r0 + m * 128:r0 + (m + 1) * 128, :], y[:], [t_y], [t_yst[yk]], eng="sp")

    bgq = []

    t_yst = [P.tok(f"yst{k}") for k in range(len(yo))]

    def reduce_quarter(q):
        CC(C, "ReduceScatter", ALU.add, G2, G["yp"][q], G["y2"][q], t_yst + [G["t_yred"]], G["t_y2"])
        CC(C, "ReduceScatter", ALU.add, G4, G["y2"][q], G["yred"][q], [G["t_y2"]], G["t_yred"])

    def record(fn, *a):
        P.rec = bgq
        C.pre = pre_ring
        C.sq_eng = "dve"
        fn(*a)
        P.rec = None
        C.pre = None
        C.sq_eng = "act"

    record(PRE, 0)
    P.replay(bgq, len(bgq))
    acc, t_acc = accs[0]
    acc2, t_acc2 = accs[1]
    for i in range(NQ):
        par = i % 2
        Qp, t_Qp = Qb[par]
        Qsp, t_Qsp = Qsb[par]
        Kr, t_Kr = Krb[par]
        Vr, t_Vr = Vrb[par]
        if i + 1 < NQ:
            record(PRE, i + 1)
        nkb = 4 * i + 4
        items = []
        for kb in range(nkb):
            items.append((Kml[:, kb * 128:(kb + 1) * 128], [tKml[kb // 4]], Qp, t_Qp, float(96 ** -0.5),
                          masks[:, kb - 4 * i, :] if kb >= 4 * i else None,
                          Vml[:, kb, :], [tVml[kb // 4], t_on1], acc, t_acc, kb == 0, kb == nkb - 1))
        rs_ = [r for r in range(5) if not (i == 0 and r == 0)]
        for n_, r in enumerate(rs_):
            items.append((Kr[:, r * 128:(r + 1) * 128], [t_Kr], Qsp, t_Qsp, 0.125, masks[:, 4 + r, :],
                          Vr[:, r, :], [t_Vr], acc2, t_acc2, n_ == 0, n_ == len(rs_) - 1))
        LA = 2
        pend = []
        nbg = len(bgq)
        done_bg = 0

        def emitA(it, pp):
            C.MM(it[8][:], it[6], pp[0][:], it[10], it[11], it[7] + [pp[1]], [it[9]])

        for n_it, it in enumerate(items):
            bs, tbs = C.bank()
            C.MM(bs[:], it[0], it[2][:], True, True, it[1] + [it[3]], [tbs])
            p, t_p = pb[pi_ % len(pb)]
            pi_ += 1
            C.ACT(p[:], bs[:], AF.Exp, [tbs], [t_p], scale=it[4])
            if it[5] is not None:
                C.TT("dve", p[:], p[:], it[5], ALU.mult, [t_p, t_masks], [t_p])
            pend.append((it, (p, t_p)))
            if len(pend) > LA:
                emitA(*pend.pop(0))
            want = ((n_it + 1) * nbg) // len(items)
            P.replay(bgq, want - done_bg)
            done_bg = want
        while pend:
            emitA(*pend.pop(0))
        P.replay(bgq, len(bgq))
        if i - 1 >= NQ - 4:
            reduce_quarter((i - 1) % 4)
        C.ACT(rd[64:128, :], acc[64:128, :], AF.Ln, [t_acc], [t_rd])
        C.ACT(rd[64:128, :], rd[64:128, :], AF.Exp, [t_rd], [t_rd], scale=-1.0)
        C.TT("dve", o1[:], acc[0:64, :], rd[64:128, :], ALU.mult, [t_acc, t_rd], [t_o1])
        C.ACT(rd2[64:128, :], acc2[64:128, :], AF.Ln, [t_acc2, t_esk], [t_rd2], bias=esk[64:128, :], scale=1.0)
        C.ACT(rd2[64:128, :], rd2[64:128, :], AF.Exp, [t_rd2], [t_rd2], scale=-1.0)
        C.TT("dve", o2[:], acc2[0:64, :], rd2[64:128, :], ALU.mult, [t_acc2, t_rd2], [t_o2])
        record(POSTb, i)
    P.replay(bgq, len(bgq))
    reduce_quarter((NQ - 1) % 4)
    C.banks = C.banks + pre_ring
    C.banks = C.banks + accs


def build_fused(nlayers=4):
    nc = bass.Bass("TRN2", target_bir_lowering=False)
    din = lambda name, shape, d=F32: nc.dram_tensor(name, list(shape), d, kind="ExternalInput").ap()
    dsc = lambda name, shape, d=F32: nc.dram_tensor(name, list(shape), d).ap()
    G = {}
    xT = din("xT", [D, NT])
    G["cbc"] = din("cbc", [128, D])
    G["ada"] = din("ada", [4, 48, 128, D])
    G["adab"] = din("adab", [4, 128, 48])
    G["g1"] = din("g1", [4, 128, KC])
    G["g2"] = din("g2", [4, 128, KC])
    cp_w_in = din("cp_w_in", [2, D, 2048])
    G["convw"] = din("convw", [2, 128, 4, 3])
    G["poolw"] = din("poolw", [2, 128, 4, 128])
    G["pscale"] = din("pscale", [2, 128, 4])
    cp_w_out = din("cp_w_out", [2, D, D])
    at_w_in = din("at_w_in", [2, D, NZ_ATT])
    G["corr"] = din("corr", [128, 4, 16])
    G["hsel"] = din("hsel", [128, 8])
    G["selq"] = din("selq", [128, 4, 64], BF16)
    G["selkv"] = din("selkv", [128, 64], BF16)
    G["wuq"] = din("wuq", [2, 384, 96])
    G["wuqs"] = din("wuqs", [2, 384, 96])
    G["wk"] = din("wk", [2, 288, 96])
    G["wks"] = din("wks", [2, 288, 96])
    G["wv"] = din("wv", [2, 256, 64])
    G["wo"] = din("wo", [2, 128, D])
    G["cols"] = din("cols", [2, 128, 16])
    G["masks"] = din("masks", [9, 128, TN], BF16)
    G["pos"] = din("pos", [96, S], I32)
    G["wr"] = din("wr", [4, 128, KC, 36])
    G["br"] = din("br", [4, 128, 36])
    G["wg"] = din("wg", [4, NE, D, FF])
    G["wu"] = din("wu", [4, NE, D, FF])
    G["wd"] = din("wd", [4, NE, FF, D])
    G["ident"] = din("ident", [128, 128])
    xo = nc.dram_tensor("xo", [D, NT], F32, kind="ExternalOutput").ap()
    G["zs"] = dsc("zs", [2048, NT])
    G["ys"] = dsc("ys", [D, NT])
    G["hsrc"] = dsc("hsrc", [128, 192])
    G["h4"] = dsc("h4", [512, 192])
    G["h8"] = dsc("h8", [1024, 192])
    G["gsrc"] = [dsc(f"gsrc{j}", [r, NT], BF16) for j, r in enumerate(ATT_ROWS)]
    G["g4"] = [dsc(f"g4_{j}", [4 * r, NT], BF16) for j, r in enumerate(ATT_ROWS)]
    G["g8"] = [dsc(f"g8_{j}", [8 * r, NT], BF16) for j, r in enumerate(ATT_ROWS)]
    G["yp"] = [dsc(f"yp{q}", [8 * D, TN], BF16) for q in range(NTILE)]
    G["y2"] = [dsc(f"y2_{q}", [4 * D, TN], BF16) for q in range(NTILE)]
    G["yred"] = [dsc(f"yred{q}", [D, TN], BF16) for q in range(NTILE)]
    with ExitStack() as es:
        C = Ctx(nc, es)
        P = C.P
        for n in ("zs", "ys", "hsrc", "h4", "h8", "gsrc", "g4", "g8", "yp", "y2", "yred"):
            G["t_" + n] = P.tok(n)
        G["t_ypq"] = [P.tok(f"ypq{q}") for q in range(NTILE)]
        x = P.sbuf([128, KC, NT], F32, "x")
        tx = [[P.tok(f"x{m}_{i}") for i in range(NTILE)] for m in range(KC)]
        G["x"], G["tx"] = x, tx
        for m in range(KC):
            C.DMA(x[:, m, :], xT[m * 128:(m + 1) * 128, :], [], tx[m])
        for l in range(nlayers):
            i = l // 2
            if l % 2 == 0:
                stage(C, lambda: st_normproj(C, G, l, cp_w_in[i], 2048, G["zs"], G["t_zs"], F32, True))
                stage(C, lambda: st_evenmix(C, G, i))
                stage(C, lambda: st_proj(C, G, l, cp_w_out[i]))
            else:
                stage(C, lambda: st_normproj(C, G, l, at_w_in[i], NZ_ATT, G["gsrc"], G["t_gsrc"], BF16, False))
                stage(C, lambda: st_attn(C, G, i))
                stage(C, lambda: st_proj(C, G, l, None))
            stage(C, lambda: st_moe(C, G, l))
        t_out = P.tok("xo")
        for m in range(KC):
            C.DMA(xo[m * 128:(m + 1) * 128, :], x[:, m, :], tx[m], [t_out], eng="sp")
        P.wait_all("sp", [t_out])
        P.emit()
    return nc


def _cols(v, n=KC):
    return np.ascontiguousarray(np.asarray(v, np.float32).reshape(n, 128).T)


def _attn_masks():
    k = np.arange(128)[:, None]
    q = np.arange(TN)[None, :]
    m = np.zeros((9, 128, TN), np.float32)
    for d in range(4):
        m[d] = (q >= d * 128 + k)
    for r in range(5):
        kp = (r - 1) * 128 + k
        m[4 + r] = (q >= kp) & (q < kp + 128)
    return m.astype(ml_dtypes.bfloat16)


_PROG = {}
NL = 4


def kernel(**inputs):
    inp = {k: np.asarray(v) for k, v in inputs.items()}
    nl = NL
    if ("fused", nl) not in _PROG:
        _PROG[("fused", nl)] = build_fused(nl)
    nc = _PROG[("fused", nl)]
    f32 = np.float32
    x = inp["x"][0]
    ada = np.ascontiguousarray(inp["ada_w"].transpose(0, 2, 1).reshape(4, 48, 128, D))
    adab = np.ascontiguousarray(inp["ada_b"].reshape(4, 48, 128).transpose(0, 2, 1))
    com = dict(
        cbc=np.ascontiguousarray(np.broadcast_to(inp["c"].reshape(1, D), (128, D))),
        ada=ada, adab=adab,
        g1=np.ascontiguousarray(inp["norm1_g"].reshape(4, KC, 128).transpose(0, 2, 1)),
        g2=np.ascontiguousarray(inp["norm2_g"].reshape(4, KC, 128).transpose(0, 2, 1)),
        cp_w_in=np.ascontiguousarray(inp["cp_w_in"]),
        convw=np.ascontiguousarray(inp["conv_w"].reshape(2, 3, 4, 128).transpose(0, 3, 2, 1)),
        poolw=np.ascontiguousarray(inp["pool_w"].transpose(0, 2, 1, 3)),
        pscale=np.ascontiguousarray(inp["pool_scale"].reshape(2, 4, 128).transpose(0, 2, 1)),
        cp_w_out=np.ascontiguousarray(inp["cp_w_out"]),
        at_w_in=np.ascontiguousarray(inp["at_w_in"]),
        masks=_attn_masks(),
        pos=np.ascontiguousarray(np.broadcast_to(inp["positions"].reshape(1, S).astype(np.int32), (96, S))),
        wr=np.ascontiguousarray(np.concatenate([inp["moe_w_group"], inp["moe_w_expert"]], axis=2)
                                .reshape(4, KC, 128, 36).transpose(0, 2, 1, 3)),
        br=np.ascontiguousarray(np.broadcast_to(
            np.concatenate([inp["moe_b_group"], inp["moe_b_expert"]], axis=1)[:, None, :], (4, 128, 36))),
        wg=np.ascontiguousarray(inp["moe_w_gate"]), wu=np.ascontiguousarray(inp["moe_w_up"]),
        wd=np.ascontiguousarray(inp["moe_w_down"]),
        ident=np.eye(128, dtype=f32),
    )
    inv = np.power(f32(10000.0), -np.arange(16, dtype=f32) / 16).astype(f32)
    maps = []
    for c in range(NCORE):
        h = c
        kvh = h // 4
        corr = np.zeros((128, 4, 16), f32)
        for g, wd_ in enumerate(POOL_W):
            t = np.arange(16, dtype=f32)
            corr[:, g, :] = (1.0 / np.minimum(t + 1.0, float(wd_))) if c == 0 else (1.0 / wd_)
        hsel = np.zeros((128, 8), f32)
        if c > 0:
            hsel[:, c - 1] = 1.0
        selq = np.zeros((128, 4, 64), f32)
        for j in range(64):
            f = h * 64 + j
            selq[f % 128, f // 128, j] = 1.0
        selkv = np.zeros((128, 64), f32)
        for j in range(64):
            selkv[kvh * 64 + j, j] = 1.0
        wuq = np.ascontiguousarray(inp["mla_w_uq"][:, :, h * 96:(h + 1) * 96])
        wkv = inp["mla_w_ukv"][:, :, h * 128:(h + 1) * 128]
        wk = np.zeros((2, 288, 96), f32)
        wk[:, 0:256, 0:64] = wkv[:, :, 0:64]
        wk[:, 256:288, 64:96] = np.eye(32, dtype=f32)
        cols = np.zeros((2, 128, 16), f32)
        for i in range(2):
            cols[i, 0:64, 0] = inp["swa_q_g"][i]
            cols[i, 0:64, 1] = inp["swa_k_g"][i]
            cols[i, :, 2:5] = _cols(inp["mla_q_norm_g"][i], 3)
            cols[i, :, 5:7] = _cols(inp["mla_kv_norm_g"][i], 2)
            cols[i, 0:96, 7] = inp["mla_q_g"][i]
            cols[i, 0:96, 8] = inp["mla_q_g"][i][PERM96]
            cols[i, 0:96, 9] = inp["mla_k_g"][i]
            cols[i, 0:96, 10] = inp["mla_k_g"][i][PERM96]
            cols[i, 64:80, 11] = inv
            cols[i, 80:96, 11] = inv
            cols[i, 64:80, 12] = -1.0
            cols[i, 80:96, 12] = 1.0
            cols[i, :, 13] = inp["swa_sinks"][i][h]
        wo = np.ascontiguousarray(np.concatenate(
            [inp["at_w_out"][:, h * 64:(h + 1) * 64, :], inp["at_w_out"][:, 512 + h * 64:512 + (h + 1) * 64, :]], axis=1))
        maps.append(dict(
            com, xT=np.ascontiguousarray(x[c * NT:(c + 1) * NT].T), corr=corr, hsel=hsel,
            selq=selq.astype(ml_dtypes.bfloat16), selkv=selkv.astype(ml_dtypes.bfloat16),
            wuq=wuq, wuqs=np.ascontiguousarray(wuq[:, :, PERM96]), wk=wk, wks=np.ascontiguousarray(wk[:, :, PERM96]),
            wv=np.ascontiguousarray(wkv[:, :, 64:128]), wo=wo, cols=cols))
    res = run_bass_kernel_spmd(nc, maps, core_ids=list(range(NCORE)))
    out = np.concatenate([res.results[c]["xo"].T for c in range(NCORE)], axis=0)[None]
    return np.ascontiguousarray(out.astype(np.float32))
```
